# Optimizing a Trainium2 kernel written in Bass

```python
import jax
import jax.numpy as jnp
from jax import lax
import numpy as np

D_MODEL = 2048
BATCH = 4
SEQ = 2048
DEPTH = 1

CTX_LEN = 256
GRID_W = 64
EPS = 1e-6
N_MOD = 6

CONV_DIM = 2048
CONV_WIDTH = 31

DN_HEADS = 16
DN_DK = 128
DN_DV = 128
DN_QK = DN_HEADS * DN_DK
DN_V = DN_HEADS * DN_DV
SHORT_CONV = 5
CHUNK = 64

PEER_HEADS = 8
PEER_KEYS = 128
PEER_EXPERTS = PEER_KEYS * PEER_KEYS
PEER_QDIM = 256
PEER_HALF = PEER_QDIM // 2
PEER_TOPK = 16
PEER_BLOCK = 128

COL_GLU = 2 * CONV_DIM
COL_QKV = 2 * DN_QK + DN_V
COL_Z = DN_V
COL_AB = 4 * DN_HEADS
COL_BR = 2 * D_MODEL
IN_COLS = COL_GLU + COL_QKV + COL_Z + COL_AB + COL_BR

kernel_name = 'hybrid_conv_gdn_peer_prefix_dit'


def rms_norm(x, g):
    xf = x.astype(jnp.float32)
    y = xf * lax.rsqrt(jnp.mean(xf * xf, axis=-1, keepdims=True) + EPS)
    return (y * g.astype(jnp.float32)).astype(x.dtype)


def layer_norm(x, g, b):
    xf = x.astype(jnp.float32)
    mu = jnp.mean(xf, axis=-1, keepdims=True)
    var = jnp.mean(jnp.square(xf - mu), axis=-1, keepdims=True)
    y = (xf - mu) * lax.rsqrt(var + EPS)
    return (y * g.astype(jnp.float32) + b.astype(jnp.float32)).astype(x.dtype)


def l2norm(x):
    xf = x.astype(jnp.float32)
    return xf * lax.rsqrt(jnp.sum(xf * xf, axis=-1, keepdims=True) + EPS)


def modulate(h, shift, scale):
    return h * (1.0 + scale[:, None, :]) + shift[:, None, :]


def dw_conv(x, w):
    pad = w.shape[0] // 2
    return lax.conv_general_dilated(x, w[:, None, :].astype(x.dtype), (1,), [(pad, pad)],
                                    dimension_numbers=('NWC', 'WIO', 'NWC'),
                                    feature_group_count=x.shape[-1])


def split_in_proj(p):
    s1 = COL_GLU
    s2 = s1 + COL_QKV
    s3 = s2 + COL_Z
    s4 = s3 + COL_AB
    return jnp.split(p, [s1, s2, s3, s4], axis=-1)


def conformer_conv(glu_in, conv_w, conv_b, ln_g, ln_b, w_conv_out, rows):
    a, gate = jnp.split(glu_in, 2, axis=-1)
    u = a * jax.nn.sigmoid(gate)
    B, L, C = u.shape
    if rows is not None:
        u = u.reshape(B * rows, GRID_W, C)
    u = (dw_conv(u, conv_w) + conv_b).reshape(B, L, C)
    u = jax.nn.silu(layer_norm(u, ln_g, ln_b))
    return u @ w_conv_out


def dn_qkv(qkv, short_w):
    B, L, _ = qkv.shape
    qkv = jax.nn.silu(dw_conv(qkv, short_w))
    q, k, v = jnp.split(qkv, [DN_QK, 2 * DN_QK], axis=-1)
    q = l2norm(q.reshape(B, L, DN_HEADS, DN_DK)) * (DN_DK ** -0.5)
    k = l2norm(k.reshape(B, L, DN_HEADS, DN_DK))
    v = v.reshape(B, L, DN_HEADS, DN_DV)
    return q, k, v


def dn_gates(ab, a_log, dt_bias):
    a, b = jnp.split(ab.astype(jnp.float32), 2, axis=-1)
    g = -jnp.exp(a_log.astype(jnp.float32)) * jax.nn.softplus(a + dt_bias.astype(jnp.float32))
    beta = jax.nn.sigmoid(b)
    return g, beta


def gated_delta_chunked(q, k, v, g, beta, s0):
    B, L, H, DK = q.shape
    DV = v.shape[-1]
    n = L // CHUNK

    def chunks(t):
        t = t.astype(jnp.float32).reshape((B, n, CHUNK) + t.shape[2:])
        return jnp.moveaxis(t, 1, 0).swapaxes(2, 3)

    qc, kc, vc, gc, bc = chunks(q), chunks(k), chunks(v), chunks(g), chunks(beta)
    gcum = jnp.cumsum(gc, axis=-1)
    tri = jnp.tril(jnp.ones((CHUNK, CHUNK), dtype=bool))
    strict = jnp.tril(jnp.ones((CHUNK, CHUNK), dtype=bool), -1)
    decay = jnp.exp(jnp.where(tri, gcum[..., :, None] - gcum[..., None, :], -jnp.inf))
    kb = kc * bc[..., None]
    m = jnp.where(strict, jnp.einsum('nbhik,nbhjk->nbhij', kb, kc) * decay, 0.0)
    eye = jnp.eye(CHUNK, dtype=jnp.float32)
    t_inv = lax.linalg.triangular_solve(m + eye, jnp.broadcast_to(eye, m.shape), left_side=True,
                                        lower=True, unit_diagonal=True)
    u = jnp.einsum('nbhij,nbhjv->nbhiv', t_inv, vc * bc[..., None])
    w = jnp.einsum('nbhij,nbhjk->nbhik', t_inv, kb * jnp.exp(gcum)[..., None])
    attn = jnp.einsum('nbhik,nbhjk->nbhij', qc, kc) * decay

    def step(S, inp):
        q_i, k_i, u_i, w_i, g_i, a_i = inp
        v_new = u_i - jnp.einsum('bhck,bhkv->bhcv', w_i, S)
        o = (jnp.einsum('bhck,bhkv->bhcv', q_i * jnp.exp(g_i)[..., None], S)
             + jnp.einsum('bhij,bhjv->bhiv', a_i, v_new))
        g_last = g_i[..., -1:]
        S = (S * jnp.exp(g_last)[..., None]
             + jnp.einsum('bhck,bhcv->bhkv', k_i * jnp.exp(g_last - g_i)[..., None], v_new))
        return S, o

    s_final, o = lax.scan(step, s0.astype(jnp.float32), (qc, kc, u, w, gcum, attn))
    o = jnp.moveaxis(o.swapaxes(2, 3), 0, 1).reshape(B, L, H, DV)
    return s_final, o


def bidirectional_deltanet(qkv_l, qkv_c, ab_l, ab_c, short_w, a_log, dt_bias):
    ql, kl, vl = dn_qkv(qkv_l, short_w)
    qc, kc, vc = dn_qkv(qkv_c, short_w)
    B = ql.shape[0]
    s0 = jnp.zeros((B, DN_HEADS, DN_DK, DN_DV), jnp.float32)
    o_l = jnp.zeros(vl.shape, jnp.float32)
    o_c = jnp.zeros(vc.shape, jnp.float32)
    for d in range(2):
        cols = slice(2 * d * DN_HEADS, (2 * d + 2) * DN_HEADS)
        gl, bl = dn_gates(ab_l[..., cols], a_log[d], dt_bias[d])
        gc, bc = dn_gates(ab_c[..., cols], a_log[d], dt_bias[d])
        seq_l = [ql, kl, vl, gl, bl]
        seq_c = [qc, kc, vc, gc, bc]
        if d == 1:
            seq_l = [jnp.flip(t, axis=1) for t in seq_l]
            seq_c = [jnp.flip(t, axis=1) for t in seq_c]
        s_ctx, oc = gated_delta_chunked(*seq_c, s0)
        _, ol = gated_delta_chunked(*seq_l, s_ctx)
        if d == 1:
            oc = jnp.flip(oc, axis=1)
            ol = jnp.flip(ol, axis=1)
        o_l = o_l + ol
        o_c = o_c + oc
    return o_l, o_c


def token_mixer(h, hc, w_in, conv_w, conv_b, conv_ln_g, conv_ln_b, w_conv_out, dn_short_w,
                dn_a_log, dn_dt_bias, dn_norm_g, w_dn_out, w_out, with_ctx_out):
    B, L, _ = h.shape
    rows = L // GRID_W
    glu_l, qkv_l, z_l, ab_l, br_l = split_in_proj(h @ w_in)
    glu_c, qkv_c, z_c, ab_c, br_c = split_in_proj(hc @ w_in)
    o_l, o_c = bidirectional_deltanet(qkv_l, qkv_c, ab_l, ab_c, dn_short_w, dn_a_log, dn_dt_bias)

    def merge(glu, z, br, o, rows_):
        Bq, Lq, _ = glu.shape
        y_conv = conformer_conv(glu, conv_w, conv_b, conv_ln_g, conv_ln_b, w_conv_out, rows_)
        o = rms_norm(o, dn_norm_g).astype(glu.dtype) * jax.nn.silu(z.reshape(Bq, Lq, DN_HEADS, DN_DV))
        y_dn = o.reshape(Bq, Lq, DN_V) @ w_dn_out
        g_conv, g_dn = jnp.split(jax.nn.sigmoid(br), 2, axis=-1)
        return (g_conv * y_conv + g_dn * y_dn) @ w_out

    y = merge(glu_l, z_l, br_l, o_l, rows)
    yc = merge(glu_c, z_c, br_c, o_c, None) if with_ctx_out else None
    return y, yc


def peer(h, w_q, key1, key2, u_tab, v_tab):
    B, L, D = h.shape
    T = B * L
    t = h.reshape(T, D)
    q = (t @ w_q).reshape(T, PEER_HEADS, PEER_QDIM).astype(jnp.float32)
    s1 = jnp.einsum('thd,hkd->thk', q[..., :PEER_HALF], key1.astype(jnp.float32))
    s2 = jnp.einsum('thd,hkd->thk', q[..., PEER_HALF:], key2.astype(jnp.float32))
    v1, i1 = lax.top_k(s1, PEER_TOPK)
    v2, i2 = lax.top_k(s2, PEER_TOPK)
    cand = (v1[..., :, None] + v2[..., None, :]).reshape(T, PEER_HEADS, PEER_TOPK * PEER_TOPK)
    cidx = (i1[..., :, None] * PEER_KEYS + i2[..., None, :]).reshape(T, PEER_HEADS, PEER_TOPK * PEER_TOPK)
    best, pos = lax.top_k(cand, PEER_TOPK)
    idx = jnp.take_along_axis(cidx, pos, axis=-1)
    wgt = jax.nn.softmax(best, axis=-1).astype(h.dtype)
    nb = T // PEER_BLOCK

    def expert_block(args):
        tb, ib, wb = args
        act = jax.nn.gelu(jnp.einsum('td,thkd->thk', tb, u_tab[ib]))
        return jnp.einsum('thk,thkd->td', wb * act, v_tab[ib])

    y = lax.map(expert_block, (t.reshape(nb, PEER_BLOCK, D),
                               idx.reshape(nb, PEER_BLOCK, PEER_HEADS, PEER_TOPK),
                               wgt.reshape(nb, PEER_BLOCK, PEER_HEADS, PEER_TOPK)))
    return y.reshape(B, L, D)


def setup_inputs(seed: int = 0) -> dict:
    key = jax.random.key(seed)
    ks = jax.random.split(key, 28)
    f32 = jnp.float32
    D = D_MODEL

    def nrm(k, shape, scale):
        return jax.random.normal(k, shape, f32) * scale

    dt = jnp.exp(jax.random.uniform(ks[14], (DEPTH, 2, DN_HEADS), f32,
                                    float(np.log(1e-3)), float(np.log(1e-1))))
    return {
        'x': nrm(ks[0], (BATCH, SEQ, D), 1.0),
        'c': nrm(ks[1], (BATCH, D), 1.0),
        'ctx': nrm(ks[2], (BATCH, CTX_LEN, D), 1.0),
        'c_ctx': nrm(ks[3], (D,), 1.0),
        'w_mod': nrm(ks[4], (DEPTH, D, N_MOD * D), 0.5 * D ** -0.5),
        'b_mod': nrm(ks[5], (DEPTH, N_MOD * D), 0.02),
        'norm1_g': 1.0 + nrm(ks[6], (DEPTH, D), 0.05),
        'norm2_g': 1.0 + nrm(ks[7], (DEPTH, D), 0.05),
        'w_in': nrm(ks[8], (DEPTH, D, IN_COLS), D ** -0.5),
        'conv_w': nrm(ks[9], (DEPTH, CONV_WIDTH, CONV_DIM), CONV_WIDTH ** -0.5),
        'conv_b': nrm(ks[10], (DEPTH, CONV_DIM), 0.02),
        'conv_ln_g': 1.0 + nrm(ks[11], (DEPTH, CONV_DIM), 0.05),
        'conv_ln_b': nrm(ks[12], (DEPTH, CONV_DIM), 0.02),
        'w_conv_out': nrm(ks[13], (DEPTH, CONV_DIM, D), CONV_DIM ** -0.5),
        'dn_short_w': nrm(ks[15], (DEPTH, SHORT_CONV, COL_QKV), SHORT_CONV ** -0.5),
        'dn_a_log': jnp.log(jax.random.uniform(ks[16], (DEPTH, 2, DN_HEADS), f32, 1.0, 16.0)),
        'dn_dt_bias': dt + jnp.log(-jnp.expm1(-dt)),
        'dn_norm_g': 1.0 + nrm(ks[17], (DEPTH, DN_DV), 0.05),
        'w_dn_out': nrm(ks[18], (DEPTH, DN_V, D), DN_V ** -0.5),
        'w_out': nrm(ks[19], (DEPTH, D, D), D ** -0.5),
        'peer_w_q': nrm(ks[20], (DEPTH, D, PEER_HEADS * PEER_QDIM), D ** -0.5),
        'peer_key1': nrm(ks[21], (DEPTH, PEER_HEADS, PEER_KEYS, PEER_HALF), PEER_HALF ** -0.5),
        'peer_key2': nrm(ks[22], (DEPTH, PEER_HEADS, PEER_KEYS, PEER_HALF), PEER_HALF ** -0.5),
        'peer_u': nrm(ks[23], (DEPTH, PEER_EXPERTS, D), D ** -0.5),
        'peer_v': nrm(ks[24], (DEPTH, PEER_EXPERTS, D), PEER_HEADS ** -0.5),
        'final_g': 1.0 + nrm(ks[25], (D,), 0.05),
    }


def reference(x, c, ctx, c_ctx, w_mod, b_mod, norm1_g, norm2_g, w_in, conv_w, conv_b, conv_ln_g,
              conv_ln_b, w_conv_out, dn_short_w, dn_a_log, dn_dt_bias, dn_norm_g, w_dn_out, w_out,
              peer_w_q, peer_key1, peer_key2, peer_u, peer_v, final_g):
    xc = ctx
    sc = jax.nn.silu(c)
    scc = jax.nn.silu(c_ctx)[None, :]
    for l in range(DEPTH):
        last = l == DEPTH - 1
        mod = jnp.split(sc @ w_mod[l] + b_mod[l], N_MOD, axis=-1)
        modc = jnp.split(scc @ w_mod[l] + b_mod[l], N_MOD, axis=-1)
        h = modulate(rms_norm(x, norm1_g[l]), mod[0], mod[1])
        hc = modulate(rms_norm(xc, norm1_g[l]), modc[0], modc[1])
        y, yc = token_mixer(h, hc, w_in[l], conv_w[l], conv_b[l], conv_ln_g[l], conv_ln_b[l],
                            w_conv_out[l], dn_short_w[l], dn_a_log[l], dn_dt_bias[l], dn_norm_g[l],
                            w_dn_out[l], w_out[l], not last)
        x = x + mod[2][:, None, :] * y
        h = modulate(rms_norm(x, norm2_g[l]), mod[3], mod[4])
        x = x + mod[5][:, None, :] * peer(h, peer_w_q[l], peer_key1[l], peer_key2[l], peer_u[l], peer_v[l])
        if not last:
            xc = xc + modc[2][:, None, :] * yc
            hc = modulate(rms_norm(xc, norm2_g[l]), modc[3], modc[4])
            xc = xc + modc[5][:, None, :] * peer(hc, peer_w_q[l], peer_key1[l], peer_key2[l],
                                                 peer_u[l], peer_v[l])
    return rms_norm(x, final_g)
```

```python
import os
from contextlib import ExitStack

import numpy as np
import concourse.bass as bass
import concourse.mybir as mybir
from concourse.bass_utils import run_bass_kernel_spmd

F32 = mybir.dt.float32
BF16 = mybir.dt.bfloat16
I32 = mybir.dt.int32
U32 = mybir.dt.uint32
AF = mybir.ActivationFunctionType
ALU = mybir.AluOpType

D = 2048
NT = 1024
NCX = 256
EPS = 1e-6
NEG = -30000.0


class Sched:
    def __init__(self, nc):
        self.nc = nc
        self.engs = {}
        self.state = {}
        self.dma_sems = {}
        self._ctx = []
        for name, obj in [('pe', nc.tensor), ('act', nc.scalar), ('dve', nc.vector),
                          ('pool', nc.gpsimd), ('sp', nc.sync)]:
            cm = nc.semaphore('s_' + name)
            sem = cm.__enter__()
            self._ctx.append(cm)
            self.engs[name] = dict(name=name, obj=obj, sem=sem, count=0, known={})

    def _st(self, k):
        if k not in self.state:
            self.state[k] = dict(w=None, r=[])
        return self.state[k]

    def _wait(self, eng, need):
        for key, (sem, val) in need.items():
            if sem is eng['sem'] and eng['name'] == 'pe':
                continue
            if eng['known'].get(key, 0) < val:
                eng['obj'].wait_ge(sem, val)
                eng['known'][key] = val

    def _deps(self, eng, reads, writes):
        need = {}

        def add(tok):
            if tok is None:
                return
            sem, val = tok
            key = id(sem)
            if key not in need or need[key][1] < val:
                need[key] = (sem, val)
        for k in reads:
            add(self._st(k)['w'])
        for k in writes:
            st = self._st(k)
            add(st['w'])
            for r in st['r']:
                add(r)
        self._wait(eng, need)

    def _mark(self, tok, reads, writes):
        for k in reads:
            st = self._st(k)
            st['r'] = [r for r in st['r'] if r[0] is not tok[0]] + [tok]
        for k in writes:
            st = self._st(k)
            st['w'] = tok
            st['r'] = []

    def op(self, engname, fn, reads=(), writes=()):
        eng = self.engs[engname]
        self._deps(eng, reads, writes)
        inst = fn(eng['obj'])
        eng['count'] += 1
        inst.then_inc(eng['sem'], 1)
        self._mark((eng['sem'], eng['count']), reads, writes)
        return inst

    def dma(self, engname, slot, fn, reads=(), writes=()):
        eng = self.engs[engname]
        self._deps(eng, reads, writes)
        if slot not in self.dma_sems:
            cm = self.nc.semaphore('d_' + slot)
            sem = cm.__enter__()
            self._ctx.append(cm)
            self.dma_sems[slot] = [sem, 0]
        ent = self.dma_sems[slot]
        inst = fn(eng['obj'])
        ent[1] += 16
        inst.then_inc(ent[0], 16)
        self._mark((ent[0], ent[1]), reads, writes)
        return inst

    def barrier(self):
        need = {}
        for e in self.engs.values():
            if e['count'] > 0:
                need[id(e['sem'])] = (e['sem'], e['count'])
        for sem, val in self.dma_sems.values():
            if val > 0:
                need[id(sem)] = (sem, val)
        for e in self.engs.values():
            n2 = {k: v for k, v in need.items() if v[0] is not e['sem']}
            for key, (sem, val) in n2.items():
                if e['known'].get(key, 0) < val:
                    e['obj'].wait_ge(sem, val)
                    e['known'][key] = val
        self.state = {}
        self._gen = getattr(self, '_gen', 0) + 1
        for e in self.engs.values():
            if e['count'] > 2000:
                cm = self.nc.semaphore('s_%s_%d' % (e['name'], self._gen))
                e['sem'] = cm.__enter__()
                self._ctx.append(cm)
                e['count'] = 0


C_GLU_A, C_GLU_G, C_Q, C_K, C_V, C_Z, C_AB, C_BRC, C_BRD = 0, 16, 32, 48, 64, 80, 96, 97, 113
N_WIN_CH = 129


def build_program(debug=0, DN_BLOCKS=4, PEER_TILES=8, dn_stop=99):
    nc = bass.Bass("TRN2", target_bir_lowering=False)

    def di(name, shape, dt=F32):
        if debug == 7 and name != 'cst':
            shape = [1, 2]
        return nc.dram_tensor(name, list(shape), dt, kind="ExternalInput").ap()

    dbg_outs = []

    def scratch(name, shape, dt):
        if debug:
            dbg_outs.append(name)
            return nc.dram_tensor(name, list(shape), dt, kind="ExternalOutput").ap()
        return nc.dram_tensor(name, list(shape), dt, kind="Internal").ap()

    xo = di("xo", [NT, D]); xp = di("xp", [NT, D]); xh = di("xh", [2, D]); cx = di("cx", [NCX, D])
    ccol = di("ccol", [128, 16, 2])
    wmod = di("wmod", [48, 128, 16, 256]); bmod = di("bmod", [128, 96])
    g1 = di("g1", [128, 16]); g2 = di("g2", [128, 16]); gf = di("gf", [128, 16])
    win = di("win", [N_WIN_CH, 128, 16, 128])
    convw = di("convw", [128, 16, 31]); convb = di("convb", [128, 16])
    lng = di("lng", [128, 16]); lnb = di("lnb", [128, 16])
    wco = di("wco", [16, 128, 16, 128]); wdn = di("wdn", [16, 128, 16, 128])
    wout = di("wout", [16, 128, 16, 128]); wq = di("wq", [16, 128, 16, 128])
    shw = di("shw", [128, 48, 5]); shwf = di("shwf", [128, 48, 5])
    gpar = di("gpar", [128, 2])
    dnng = di("dnng", [128, 1])
    pk1 = di("pk1", [8, 128, 128]); pk2 = di("pk2", [8, 128, 128])
    PUR = 16384 if debug in (0, 6) else 128
    pu = di("pu", [PUR, D]); pv = di("pv", [PUR, D])
    cst = di("cst", [128, 1024])
    out = nc.dram_tensor("out", [NT, D], F32, kind="ExternalOutput").ap()

    XT = scratch("XT", [16, 128, NT], F32)
    X1 = scratch("X1", [16, 128, NT], F32)
    UC = scratch("UC", [16, 128, NT], BF16)
    QTo = scratch("QTo", [16, 128, NT], BF16)
    KTo = scratch("KTo", [16, 128, NT], BF16)
    VTo = scratch("VTo", [16, 128, NT], BF16)
    KTp = scratch("KTp", [16, 128, NT], BF16)
    VTp = scratch("VTp", [16, 128, NT], BF16)
    KTc = scratch("KTc", [16, 128, NCX], BF16)
    VTc = scratch("VTc", [16, 128, NCX], BF16)
    ZT = scratch("ZT", [16, 128, NT], BF16)
    BR = scratch("BR", [32, 128, NT], BF16)
    PUB = nc.dram_tensor("PUB", [PUR, D], BF16, kind="Internal").ap()
    PVB = nc.dram_tensor("PVB", [PUR, D], BF16, kind="Internal").ap()
    if debug:
        DBG = scratch("DBG", [128, 4096], F32)
        OTD = scratch("OTD", [128, 16, NT], BF16)

    S = Sched(nc)
    es = ExitStack()

    def sb(stack, name, shape, dt):
        return stack.enter_context(nc.sbuf_tensor(name, list(shape), dt))

    pb = [es.enter_context(nc.psum_tensor("pb%d" % i, [128, 512], F32)) for i in range(8)]
    bank_ctr = [0]
    bank_pool = [list(range(8))]

    def nb():
        pool = bank_pool[0]
        b = pool[bank_ctr[0] % len(pool)]
        bank_ctr[0] += 1
        return b

    def pk(b):
        return 'pb%d' % b

    cs = sb(es, "cs", [128, 1024], F32)
    S.dma('sp', 'cs', lambda e: e.dma_start(out=cs[:], in_=cst), writes=['cs'])
    ident = cs[:, 0:128]
    ones = cs[:, 128:256]
    tri = cs[0:64, 256:320]
    negones = cs[0:64, 320:384]
    nmL = cs[0:64, 384:448]
    nmU = cs[0:64, 448:512]
    eye64 = cs[0:64, 0:64]
    identb = sb(es, "identb", [128, 128], BF16)
    onesb = sb(es, "onesb", [128, 128], BF16)
    S.op('dve', lambda e: e.tensor_copy(out=identb[:], in_=ident), reads=['cs'], writes=['identb'])
    S.op('dve', lambda e: e.tensor_copy(out=onesb[:], in_=ones), reads=['cs'], writes=['onesb'])

    S_dma_prm = S.dma if debug != 7 else (lambda *a, **k: None)
    prm = sb(es, "prm", [128, 16 * 6 + 96 + 31 * 16 + 48 * 10 + 4], F32)
    o_ = [0]

    def prm_alloc(n):
        a = o_[0]
        o_[0] += n
        return a
    pofs = {}
    for nm_, src, n in [('g1', g1, 16), ('g2', g2, 16), ('gf', gf, 16), ('convb', convb, 16),
                        ('lng', lng, 16), ('lnb', lnb, 16), ('bmod', bmod, 96)]:
        a = prm_alloc(n)
        pofs[nm_] = a
        S_dma_prm('sp', 'prm_' + nm_, lambda e, a=a, n=n, src=src: e.dma_start(out=prm[:, a:a + n], in_=src),
              writes=['prm'])
    a = prm_alloc(31 * 16); pofs['convw'] = a
    S_dma_prm('sp', 'prm_convw', lambda e: e.dma_start(out=prm[:, a:a + 496], in_=convw.rearrange("p c k -> p (c k)")), writes=['prm'])
    a2 = prm_alloc(240); pofs['shw'] = a2
    S_dma_prm('sp', 'prm_shw', lambda e: e.dma_start(out=prm[:, a2:a2 + 240], in_=shw.rearrange("p c k -> p (c k)")), writes=['prm'])
    a3 = prm_alloc(240); pofs['shwf'] = a3
    S_dma_prm('sp', 'prm_shwf', lambda e: e.dma_start(out=prm[:, a3:a3 + 240], in_=shwf.rearrange("p c k -> p (c k)")), writes=['prm'])
    a4 = prm_alloc(2); pofs['gpar'] = a4
    S_dma_prm('sp', 'prm_gpar', lambda e: e.dma_start(out=prm[:, a4:a4 + 2], in_=gpar), writes=['prm'])
    a5 = prm_alloc(1); pofs['dnng'] = a5
    S_dma_prm('sp', 'prm_dnng', lambda e: e.dma_start(out=prm[:, a5:a5 + 1], in_=dnng), writes=['prm'])

    def pcol(name, j):
        a = pofs[name] + j
        return prm[:, a:a + 1]

    md = sb(es, "md", [128, 16 * 10 + 4], F32)
    MD = dict(A1=0, B1=16, A1c=32, B1c=48, A2=64, B2=80, G2=96, G5=112, NEA=160)

    def mdc(name, j):
        a = MD[name] + j
        return md[:, a:a + 1]

    wf = [sb(es, "wf%d" % i, [128, 16, 128], F32) for i in range(2)]
    wb = [sb(es, "wb%d" % i, [128, 16, 128], BF16) for i in range(2)]
    wctr = [0]

    cvt_jobs = []
    if debug != 7:
        for src_, dst_ in ((pu, PUB), (pv, PVB)):
            for r0 in range(0, PUR, 128):
                cvt_jobs.append((src_, dst_, r0))
    cvt_state = dict(call=0, bufs=None)

    def cvt_step(n=1):
        if cvt_state['bufs'] is None:
            return
        cvf, cvb = cvt_state['bufs']
        N = len(cvt_jobs)
        for _ in range(n):
            c = cvt_state['call']
            if 2 * c - 4 >= N:
                return
            cvt_state['call'] += 1
            for j in (2 * c, 2 * c + 1):
                if 0 <= j < N:
                    src_, dst_, r0 = cvt_jobs[j]
                    sl = j % 4
                    S.dma('sp', 'cvl%d' % sl, lambda e: e.dma_start(out=cvf[sl][:], in_=src_[r0:r0 + 128, :]), writes=['cvf%d' % sl])
            for j in (2 * c - 2, 2 * c - 1):
                if 0 <= j < N:
                    sl = j % 4
                    S.op('pool', lambda e: e.tensor_copy(out=cvb[sl][:], in_=cvf[sl][:]), reads=['cvf%d' % sl], writes=['cvb%d' % sl])
            for j in (2 * c - 4, 2 * c - 3):
                if 0 <= j < N:
                    src_, dst_, r0 = cvt_jobs[j]
                    sl = j % 4
                    S.dma('sp', 'cvs%d' % sl, lambda e: e.dma_start(out=dst_[r0:r0 + 128, :], in_=cvb[sl][:]), reads=['cvb%d' % sl])

    def load_w(chunk_ap):
        slot = wctr[0] % 2
        wctr[0] += 1
        cvt_step(1)
        S.dma('sp', 'wf%d' % slot, lambda e: e.dma_start(out=wf[slot][:], in_=chunk_ap), writes=['wf%d' % slot])
        S.op('pool', lambda e: e.tensor_copy(out=wb[slot][:], in_=wf[slot][:]), reads=['wf%d' % slot], writes=['wb%d' % slot])
        return slot

    def mm16(slot, actfn, N, actkeys):
        b = nb()
        for kc in range(16):
            S.op('pe', lambda e: e.matmul(pb[b][:, 0:N], lhsT=wb[slot][:, kc, :], rhs=actfn(kc),
                                          start=(kc == 0), stop=(kc == 15)),
                 reads=['wb%d' % slot] + list(actkeys), writes=[pk(b)])
        return b

    gT = sb(es, "gT", [128, 2304], F32)

    for ph in ([ExitStack()] if debug != 7 else []):
        cc = sb(ph, "cc", [128, 16, 2], F32)
        sc = sb(ph, "sc", [128, 16, 2], F32)
        wm = [sb(ph, "wm%d" % i, [128, 16, 256], F32) for i in range(2)]
        modT = sb(ph, "modT", [128, 96, 2], F32)
        S.dma('sp', 'cc', lambda e: e.dma_start(out=cc[:], in_=ccol), writes=['cc'])
        S.op('act', lambda e: e.activation(out=sc[:], in_=cc[:], func=AF.Silu), reads=['cc'], writes=['sc'])
        bm = nb()
        for blk in range(48):
            s_ = blk % 2
            S.dma('sp', 'wm%d' % s_, lambda e: e.dma_start(out=wm[s_][:], in_=wmod[blk]), writes=['wm%d' % s_])
            for c2 in range(2):
                ci = blk * 2 + c2
                for kc in range(16):
                    S.op('pe', lambda e: e.matmul(pb[bm][:, ci * 2:ci * 2 + 2], lhsT=wm[s_][:, kc, c2 * 128:(c2 + 1) * 128],
                                                  rhs=sc[:, kc, :], start=(kc == 0), stop=(kc == 15)),
                         reads=['wm%d' % s_, 'sc'], writes=[pk(bm)])
        bmo = pofs['bmod']
        S.op('dve', lambda e: e.tensor_tensor(out=modT[:], in0=pb[bm][:, 0:192].rearrange("p (c t) -> p c t", t=2),
                                              in1=prm[:, bmo:bmo + 96].unsqueeze(2).to_broadcast([128, 96, 2]), op=ALU.add),
             reads=[pk(bm), 'prm'], writes=['modT'])
        for (An, Bn, gname, msc, msh, col) in [('A1', 'B1', 'g1', 1, 0, 0), ('A1c', 'B1c', 'g1', 1, 0, 1), ('A2', 'B2', 'g2', 4, 3, 0)]:
            ga = pofs[gname]
            S.op('dve', lambda e: e.scalar_tensor_tensor(out=md[:, MD[An]:MD[An] + 16], in0=modT[:, msc * 16:(msc + 1) * 16, col],
                                                         scalar=1.0, in1=prm[:, ga:ga + 16], op0=ALU.add, op1=ALU.mult),
                 reads=['modT', 'prm'], writes=['md'])
            S.op('dve', lambda e: e.tensor_copy(out=md[:, MD[Bn]:MD[Bn] + 16], in_=modT[:, msh * 16:(msh + 1) * 16, col]),
                 reads=['modT'], writes=['md'])
        S.op('dve', lambda e: e.tensor_copy(out=md[:, MD['G2']:MD['G2'] + 16], in_=modT[:, 32:48, 0]), reads=['modT'], writes=['md'])
        S.op('dve', lambda e: e.tensor_copy(out=md[:, MD['G5']:MD['G5'] + 16], in_=modT[:, 80:96, 0]), reads=['modT'], writes=['md'])
        gp = pofs['gpar']
        S.op('act', lambda e: e.activation(out=md[:, 160:161], in_=prm[:, gp:gp + 1], func=AF.Exp), reads=['prm'], writes=['md'])
        S.op('dve', lambda e: e.tensor_scalar(out=md[:, 160:161], in0=md[:, 160:161], scalar1=-1.0, scalar2=None, op0=ALU.mult),
             reads=['md'], writes=['md'])
        if debug == 1:
            S.dma('sp', 'dbg', lambda e: e.dma_start(out=DBG[:, 0:164], in_=md[:]), reads=['md'], writes=['DBG'])
        S.barrier()
        ph.close()

    def fm_norm(ph, srcT, N, Acol, Bcol, dst, dstkey, srckey, tmp, tmpkey, out_f32=False):
        b = nb()
        for kc in range(16):
            S.op('act', lambda e: e.activation(out=tmp[:, kc, 0:N], in_=srcT[:, kc, 0:N], func=AF.Square),
                 reads=[srckey], writes=[tmpkey + str(kc)])
            S.op('pe', lambda e: e.matmul(pb[b][:, 0:N], lhsT=onesb[:], rhs=tmp[:, kc, 0:N], start=(kc == 0), stop=(kc == 15)),
                 reads=[tmpkey + str(kc), 'onesb'], writes=[pk(b)])
        rs = fm_rs
        S.op('act', lambda e: e.activation(out=rs[:, 0:N], in_=pb[b][:, 0:N], func=AF.Sqrt, scale=1.0 / D, bias=epsc[:, 0:1]),
             reads=[pk(b), 'epsc'], writes=['fm_rs'])
        S.op('dve', lambda e: e.reciprocal(out=rs[:, 0:N], in_=rs[:, 0:N]), reads=['fm_rs'], writes=['fm_rs'])
        for kc in range(16):
            S.op('dve', lambda e: e.tensor_tensor(out=fm_t[:, 0:N], in0=srcT[:, kc, 0:N], in1=rs[:, 0:N], op=ALU.mult),
                 reads=[srckey, 'fm_rs'], writes=['fm_t'])
            if Bcol is not None:
                S.op('act', lambda e: e.activation(out=dst[:, kc, 0:N], in_=fm_t[:, 0:N], func=AF.Identity,
                                                   scale=Acol(kc), bias=Bcol(kc)),
                     reads=['fm_t', 'md', 'prm'], writes=[dstkey])
            else:
                S.op('act', lambda e: e.activation(out=dst[:, kc, 0:N], in_=fm_t[:, 0:N], func=AF.Identity, scale=Acol(kc), bias=epsc[:, 2:3]),
                     reads=['fm_t', 'md', 'prm', 'epsc'], writes=[dstkey])

    fm_rs = sb(es, "fm_rs", [128, 512], F32)
    fm_t = sb(es, "fm_t", [128, 512], F32)
    epsc = sb(es, "epsc", [128, 4], F32)
    S.op('dve', lambda e: e.memset(epsc[:, 0:1], EPS), writes=['epsc'])
    S.op('dve', lambda e: e.memset(epsc[:, 1:2], 1.0), writes=['epsc'])
    S.op('dve', lambda e: e.memset(epsc[:, 2:3], 0.0), writes=['epsc'])

    for ph in ([ExitStack()] if debug != 7 else []):
        hTo = sb(ph, "hTo", [128, 16, NT + 2], BF16)
        hTp = sb(ph, "hTp", [128, 16, NT], BF16)
        hTc = sb(ph, "hTc", [128, 16, NCX], BF16)
        phB = ExitStack()
        xtm = [sb(phB, "xtm%d" % i, [128, D], F32) for i in range(2)]
        xTt = sb(phB, "xTt", [128, 16, 512], F32)
        sqt = sb(phB, "sqt", [128, 16, 512], BF16)

        class _V:
            def __init__(self, t, o):
                self.t, self.o = t, o

            def __getitem__(self, key):
                p, kc, sl = key
                return self.t[p, kc, self.o + sl.start:self.o + sl.stop]

        def stageB2(src, ntok, dstT, dstkey, off, Aname, Bname, save_xt):
            t0 = 0
            while t0 < ntok:
                n = min(512, ntok - t0)
                for s0 in range(0, n, 128):
                    m = min(128, n - s0)
                    sl = ((t0 + s0) // 128) % 2
                    S.dma('sp', 'xtm%d' % sl, lambda e: e.dma_start(out=xtm[sl][0:m, :], in_=src[t0 + s0:t0 + s0 + m, :]),
                          writes=['xtm%d' % sl])
                    for q4 in range(4):
                        b = nb()
                        for i in range(4):
                            kc = q4 * 4 + i
                            S.op('pe', lambda e: e.transpose(out=pb[b][:, i * 128:i * 128 + m], in_=xtm[sl][0:m, kc * 128:(kc + 1) * 128],
                                                             identity=ident[0:m, 0:m]),
                                 reads=['xtm%d' % sl, 'cs'], writes=[pk(b)])
                        S.op('act', lambda e: e.activation(out=xTt[:, q4 * 4:q4 * 4 + 4, s0:s0 + m],
                                                           in_=pb[b][:, :].rearrange("p (i t) -> p i t", t=128)[:, :, 0:m], func=AF.Copy),
                             reads=[pk(b)], writes=['xTt'])
                if save_xt:
                    S.dma('sp', 'XTst', lambda e: e.dma_start(out=XT.rearrange("c p t -> p c t")[:, :, t0:t0 + n], in_=xTt[:, :, 0:n]),
                          reads=['xTt'], writes=['XT'])
                fm_norm(ph, xTt, n, lambda kc: mdc(Aname, kc), lambda kc: mdc(Bname, kc),
                        _V(dstT, off + t0), dstkey, 'xTt', sqt, 'sqt')
                t0 += n

        stageB2(xo, NT, hTo, 'hTo', 0, 'A1', 'B1', True)
        stageB2(xh, 2, hTo, 'hTo', NT, 'A1', 'B1', False)
        stageB2(xp, NT, hTp, 'hTp', 0, 'A1', 'B1', False)
        stageB2(cx, NCX, hTc, 'hTc', 0, 'A1c', 'B1c', False)
        if debug == 2:
            S.barrier()
            S.op('act', lambda e: e.activation(out=xTt[:, :, 0:64], in_=hTo[:, :, 0:64], func=AF.Copy), reads=['hTo'], writes=['xTt'])
            S.dma('sp', 'dbg', lambda e: e.dma_start(out=DBG[:, 0:1024], in_=xTt[:, :, 0:64]), reads=['xTt'], writes=['DBG'])

        S.barrier()
        phB.close()
        if cvt_jobs:
            cvt_state['bufs'] = ([sb(ph, "cvf%d" % i, [128, D], F32) for i in range(4)],
                                 [sb(ph, "cvb%d" % i, [128, D], BF16) for i in range(4)])
        upad = sb(ph, "upad", [128, 16, 94], BF16)
        S.op('dve', lambda e: e.memset(upad[:], 0.0), writes=['upad'])
        dg = sb(ph, "dg", [128, 31, 128], BF16)
        dg5 = sb(ph, "dg5", [128, 5, 128], BF16)
        dg5f = sb(ph, "dg5f", [128, 5, 128], BF16)
        po = sb(ph, "po", [128, NT + 6], BF16)
        pp = sb(ph, "pp", [128, NT + 4], BF16)
        pc = sb(ph, "pc", [128, NCX + 4], BF16)
        for t_, k_ in [(po, 'po'), (pp, 'pp'), (pc, 'pc')]:
            S.op('dve', lambda e: e.memset(t_[:], 0.0), writes=[k_])
        sig = sb(ph, "sig", [128, 512], F32)
        stg = [sb(ph, "stg%d" % i, [128, NT], BF16) for i in range(2)]
        stgc = [0]
        sl5s = [sb(ph, "sl5_%d" % i, [128, 512], F32) for i in range(2)]
        sq5s = [sb(ph, "sq5_%d" % i, [128, 512], BF16) for i in range(2)]
        rs5s = [sb(ph, "rs5_%d" % i, [128, 512], F32) for i in range(2)]
        c5ctr = [0]

        own_tiles = [(0, 512), (512, 512)]

        def act_o(t0, n):
            return lambda kc: hTo[:, kc, t0:t0 + n]

        def act_p(t0, n):
            return lambda kc: hTp[:, kc, t0:t0 + n]

        def act_c(t0, n):
            return lambda kc: hTc[:, kc, t0:t0 + n]

        def next_stg():
            i = stgc[0] % 2
            stgc[0] += 1
            return i

        for j in range(16):
            sa = load_w(win[C_GLU_A + j])
            bas = [mm16(sa, act_o(t0, n), n, ['hTo']) for (t0, n) in own_tiles]
            sg = load_w(win[C_GLU_G + j])
            for ti, (t0, n) in enumerate(own_tiles):
                bg = mm16(sg, act_o(t0, n), n, ['hTo'])
                S.op('act', lambda e: e.activation(out=sig[:, 0:n], in_=pb[bg][:, 0:n], func=AF.Sigmoid), reads=[pk(bg)], writes=['sig'])
                S.op('dve', lambda e: e.tensor_tensor(out=upad[:, 8 * ti:8 * ti + 8, 15:79],
                                                      in0=pb[bas[ti]][:, 0:512].rearrange("p (r c) -> p r c", c=64),
                                                      in1=sig[:, 0:512].rearrange("p (r c) -> p r c", c=64), op=ALU.mult),
                     reads=[pk(bas[ti]), 'sig'], writes=['upad'])
            cw = pofs['convw'] + j * 31
            S.op('dve', lambda e: e.tensor_tensor(out=dg[:], in0=identb[:].unsqueeze(1).to_broadcast([128, 31, 128]),
                                                  in1=prm[:, cw:cw + 31].unsqueeze(2).to_broadcast([128, 31, 128]), op=ALU.mult),
                 reads=['identb', 'prm'], writes=['dg'])
            si = next_stg()
            for hf in range(2):
                b = nb()
                for k in range(31):
                    S.op('pe', lambda e: e.matmul(pb[b][:, :], lhsT=dg[:, k, :], rhs=upad[:, 8 * hf:8 * hf + 8, k:k + 64],
                                                  start=(k == 0), stop=(k == 30)),
                         reads=['dg', 'upad'], writes=[pk(b)])
                S.op('act', lambda e: e.activation(out=stg[si][:, hf * 512:(hf + 1) * 512], in_=pb[b][:, :], func=AF.Identity,
                                                   bias=pcol('convb', j), scale=1.0),
                     reads=[pk(b), 'prm'], writes=['stg%d' % si])
            S.dma('sp', 'stgo%d' % si, lambda e: e.dma_start(out=UC[j], in_=stg[si][:]), reads=['stg%d' % si])

        def conv5_store(kind, j, pad, padkey, ntok, taps, dst, dstj):
            si = next_stg()
            t0 = 0
            while t0 < ntok:
                n = min(512, ntok - t0)
                pz = c5ctr[0] % 2
                c5ctr[0] += 1
                sl5, sq5, rs5 = sl5s[pz], sq5s[pz], rs5s[pz]
                b = nb()
                for k in range(5):
                    S.op('pe', lambda e: e.matmul(pb[b][:, 0:n], lhsT=taps[:, k, :], rhs=pad[:, t0 + k:t0 + k + n],
                                                  start=(k == 0), stop=(k == 4)),
                         reads=[padkey, 'dg5', 'dg5f'], writes=[pk(b)])
                if kind == 'v':
                    S.op('act', lambda e: e.activation(out=stg[si][:, t0:t0 + n], in_=pb[b][:, 0:n], func=AF.Silu),
                         reads=[pk(b)], writes=['stg%d' % si])
                else:
                    S.op('act', lambda e: e.activation(out=sl5[:, 0:n], in_=pb[b][:, 0:n], func=AF.Silu), reads=[pk(b)], writes=['sl5_%d' % pz])
                    S.op('act', lambda e: e.activation(out=sq5[:, 0:n], in_=sl5[:, 0:n], func=AF.Square), reads=['sl5_%d' % pz], writes=['sq5_%d' % pz])
                    b2 = nb()
                    S.op('pe', lambda e: e.matmul(pb[b2][:, 0:n], lhsT=onesb[:], rhs=sq5[:, 0:n], start=True, stop=True),
                         reads=['sq5_%d' % pz, 'onesb'], writes=[pk(b2)])
                    S.op('act', lambda e: e.activation(out=rs5[:, 0:n], in_=pb[b2][:, 0:n], func=AF.Sqrt, scale=1.0, bias=epsc[:, 0:1]),
                         reads=[pk(b2), 'epsc'], writes=['rs5_%d' % pz])
                    S.op('dve', lambda e: e.reciprocal(out=rs5[:, 0:n], in_=rs5[:, 0:n]), reads=['rs5_%d' % pz], writes=['rs5_%d' % pz])
                    sc_ = (128.0 ** -0.5) if kind == 'q' else 1.0
                    S.op('dve', lambda e: e.scalar_tensor_tensor(out=stg[si][:, t0:t0 + n], in0=sl5[:, 0:n], scalar=sc_, in1=rs5[:, 0:n],
                                                                 op0=ALU.mult, op1=ALU.mult),
                         reads=['sl5_%d' % pz, 'rs5_%d' % pz], writes=['stg%d' % si])
                t0 += n
            S.dma('sp', 'stgo%d' % si, lambda e: e.dma_start(out=dst[dstj][:, 0:ntok], in_=stg[si][:, 0:ntok]),
                  reads=['stg%d' % si])

        def build_taps(idx):
            a_ = pofs['shw'] + idx * 5
            f_ = pofs['shwf'] + idx * 5
            for (t_, o_k, key_) in ((dg5, a_, 'dg5'), (dg5f, f_, 'dg5f')):
                S.op('dve', lambda e: e.tensor_tensor(out=t_[:], in0=identb[:].unsqueeze(1).to_broadcast([128, 5, 128]),
                                                      in1=prm[:, o_k:o_k + 5].unsqueeze(2).to_broadcast([128, 5, 128]), op=ALU.mult),
                     reads=['identb', 'prm'], writes=[key_])

        for j in range(16):
            for kind, cbase, kidx in [('q', C_Q, 0), ('k', C_K, 16), ('v', C_V, 32)]:
                s_ = load_w(win[cbase + j])
                build_taps(kidx + j)
                for (t0, n) in own_tiles + [(NT, 2)]:
                    b = mm16(s_, act_o(t0, n), n, ['hTo'])
                    S.op('act', lambda e: e.activation(out=po[:, 2 + t0:2 + t0 + n], in_=pb[b][:, 0:n], func=AF.Copy), reads=[pk(b)], writes=['po'])
                conv5_store(kind, j, po, 'po', NT, dg5, {'q': QTo, 'k': KTo, 'v': VTo}[kind], j)
                if kind != 'q':
                    for (t0, n) in own_tiles:
                        b = mm16(s_, act_p(t0, n), n, ['hTp'])
                        S.op('act', lambda e: e.activation(out=pp[:, 2 + t0:2 + t0 + n], in_=pb[b][:, 0:n], func=AF.Copy), reads=[pk(b)], writes=['pp'])
                    S.op('act', lambda e: e.activation(out=pp[:, 2 + NT:3 + NT], in_=po[:, 2 + NT - 1:2 + NT], func=AF.Copy), reads=['po'], writes=['pp'])
                    S.op('act', lambda e: e.activation(out=pp[:, 3 + NT:4 + NT], in_=po[:, 2 + NT - 2:2 + NT - 1], func=AF.Copy), reads=['po'], writes=['pp'])
                    conv5_store(kind, j, pp, 'pp', NT, dg5f, {'k': KTp, 'v': VTp}[kind], j)
                    b = mm16(s_, act_c(0, NCX), NCX, ['hTc'])
                    S.op('act', lambda e: e.activation(out=pc[:, 2:2 + NCX], in_=pb[b][:, 0:NCX], func=AF.Copy), reads=[pk(b)], writes=['pc'])
                    conv5_store(kind, j, pc, 'pc', NCX, dg5, {'k': KTc, 'v': VTc}[kind], j)

        for j in range(16):
            s_ = load_w(win[C_Z + j])
            si = next_stg()
            for (t0, n) in own_tiles:
                b = mm16(s_, act_o(t0, n), n, ['hTo'])
                S.op('act', lambda e: e.activation(out=stg[si][:, t0:t0 + n], in_=pb[b][:, 0:n], func=AF.Silu), reads=[pk(b)], writes=['stg%d' % si])
            S.dma('sp', 'stgo%d' % si, lambda e: e.dma_start(out=ZT[j], in_=stg[si][:]), reads=['stg%d' % si])

        s_ = load_w(win[C_AB])
        gp = pofs['gpar']
        for (actf, keys, t0, n, goff) in ([(act_o(t0, n), ['hTo'], t0, n, t0) for (t0, n) in own_tiles] +
                                          [(act_p(t0, n), ['hTp'], t0, n, NT + t0) for (t0, n) in own_tiles] +
                                          [(act_c(0, NCX), ['hTc'], 0, NCX, 2 * NT)]):
            b = mm16(s_, actf, n, keys)
            for g0 in (0, 64):
                S.op('act', lambda e: e.activation(out=sig[g0:g0 + 32, 0:n], in_=pb[b][g0:g0 + 32, 0:n], func=AF.Exp,
                                                   bias=prm[g0:g0 + 32, gp + 1:gp + 2], scale=1.0), reads=[pk(b), 'prm'], writes=['sig'])
                S.op('act', lambda e: e.activation(out=sig[g0:g0 + 32, 0:n], in_=sig[g0:g0 + 32, 0:n], func=AF.Ln,
                                                   bias=epsc[g0:g0 + 32, 1:2], scale=1.0), reads=['sig', 'epsc'], writes=['sig'])
                S.op('dve', lambda e: e.tensor_scalar(out=gT[g0:g0 + 32, goff:goff + n], in0=sig[g0:g0 + 32, 0:n],
                                                      scalar1=md[g0:g0 + 32, 160:161], scalar2=None, op0=ALU.mult),
                     reads=['sig', 'md'], writes=['gT'])
                S.op('act', lambda e: e.activation(out=gT[g0 + 32:g0 + 64, goff:goff + n], in_=pb[b][g0 + 32:g0 + 64, 0:n], func=AF.Sigmoid),
                     reads=[pk(b)], writes=['gT'])

        for j in range(32):
            s_ = load_w(win[C_BRC + j])
            si = next_stg()
            for (t0, n) in own_tiles:
                b = mm16(s_, act_o(t0, n), n, ['hTo'])
                S.op('act', lambda e: e.activation(out=stg[si][:, t0:t0 + n], in_=pb[b][:, 0:n], func=AF.Sigmoid), reads=[pk(b)], writes=['stg%d' % si])
            S.dma('sp', 'stgo%d' % si, lambda e: e.dma_start(out=BR[j], in_=stg[si][:]), reads=['stg%d' % si])
        if debug == 3:
            S.dma('sp', 'dbg', lambda e: e.dma_start(out=DBG[:, 0:2304], in_=gT[:]), reads=['gT'], writes=['DBG'])
        cvt_step(10 ** 6)
        cvt_state['bufs'] = None
        S.barrier()
        ph.close()

    if debug in (1, 2, 3):
        S.barrier()
        es.close()
        return nc, dbg_outs

    def r3(ap, inner):
        return ap.rearrange("p (a b) -> p a b", b=inner)

    def pbb(b):
        return pb[b][:, :].bitcast(BF16)

    phDE = ExitStack()
    OT = sb(phDE, "OT", [128, 16, NT], BF16)

    with ExitStack() as ph:
        Sf = sb(ph, "Sf", [128, 16, 128], F32)
        Sb = sb(ph, "Sb", [128, 16, 128], BF16)
        kblk = sb(ph, "kblk", [128, 16, 256], BF16)
        vblk = sb(ph, "vblk", [128, 16, 256], BF16)
        qblk = sb(ph, "qblk", [128, 16, 256], BF16)
        gtm = sb(ph, "gtm", [64, 128], F32)
        sm = sb(ph, "sm", [64, 96], F32)
        egl = sb(ph, "egl", [128, 16], F32)
        Gm = sb(ph, "Gm", [64, 1024], F32)
        gb = sb(ph, "gb", [64, 1024], F32)
        dl = sb(ph, "dl", [64, 512], F32)
        du = sb(ph, "du", [64, 512], F32)
        decS = [sb(ph, "decS%d" % i, [64, 512], F32) for i in range(2)]
        decT = [sb(ph, "decT%d" % i, [64, 512], F32) for i in range(2)]
        tN = sb(ph, "tN", [64, 512], F32)
        Nm = [sb(ph, "Nm%d" % i, [64, 512], F32) for i in range(2)]
        Qm = [sb(ph, "Qm%d" % i, [64, 512], F32) for i in range(2)]
        Nn = [[sb(ph, "Nn%d_%d" % (i, k), [64, 512], F32) for k in range(2)] for i in range(2)]
        Qn = [[sb(ph, "Qn%d_%d" % (i, k), [64, 512], F32) for k in range(2)] for i in range(2)]
        N2I = [sb(ph, "N2I%d" % i, [64, 512], F32) for i in range(2)]
        Rm = [sb(ph, "Rm%d" % i, [64, 512], F32) for i in range(2)]
        TinvT = [sb(ph, "TinvT%d" % i, [64, 512], BF16) for i in range(2)]
        kd = [sb(ph, "kd%d" % i, [64, 1024], BF16) for i in range(2)]
        vb = [sb(ph, "vb%d" % i, [64, 1024], F32) for i in range(2)]
        attnT = [sb(ph, "attnT%d" % i, [64, 512], BF16) for i in range(2)]
        Ed = sb(ph, "Ed", [64, 1024], F32)
        qs = [sb(ph, "qs%d" % i, [128, 8, 64], BF16) for i in range(2)]
        tr = [sb(ph, "tr%d" % i, [64, 512], F32) for i in range(2)]
        rr = [sb(ph, "rr%d" % i, [64, 512], BF16) for i in range(2)]
        vn = [sb(ph, "vn%d" % i, [64, 512], BF16) for i in range(2)]
        rk = sb(ph, "rk", [128, 16, 64], BF16)
        rv = sb(ph, "rv", [128, 16, 64], BF16)
        rq = sb(ph, "rq", [128, 16, 64], BF16)
        rg = sb(ph, "rg", [128, 64], F32)

        gcum_sb, egc, dd, ekd, cc_, nbeta = (sm[:, 0:16], sm[:, 16:32], sm[:, 32:48], sm[:, 48:64], sm[:, 64:80], sm[:, 80:96])

        def bc(ap, axis, shape):
            return ap.unsqueeze(axis).to_broadcast(shape)

        def dn_chunk(kT, vT, qT, qT8, gview, goff, boff, omode, otv, keys=('kblk', 'vblk', 'qblk', 'gT')):
            KK_, VK_, QK_, GK_ = keys
            if dn_stop <= 0:
                return
            b = nb()
            S.op('pe', lambda e: e.transpose(out=pb[b][0:64, 0:128], in_=gview, identity=ident), reads=[GK_, 'cs'], writes=[pk(b)])
            S.op('act', lambda e: e.activation(out=gtm[:], in_=pb[b][0:64, 0:128], func=AF.Copy), reads=[pk(b)], writes=['gtm'])
            g = gtm[:, goff:goff + 16]
            beta = gtm[:, boff:boff + 16]
            b = nb()
            S.op('pe', lambda e: e.matmul(pb[b][0:64, 0:16], lhsT=tri, rhs=g, start=True, stop=True), reads=['gtm', 'cs'], writes=[pk(b)])
            S.op('pe', lambda e: e.matmul(pb[b][0:64, 16:32], lhsT=ones[0:64, 0:64], rhs=g, start=True, stop=True), reads=['gtm', 'cs'], writes=[pk(b)])
            S.op('pe', lambda e: e.matmul(pb[b][:, 32:48], lhsT=ones[0:64, :], rhs=g, start=True, stop=True), reads=['gtm', 'cs'], writes=[pk(b)])
            S.op('act', lambda e: e.activation(out=gcum_sb, in_=pb[b][0:64, 0:16], func=AF.Copy), reads=[pk(b)], writes=['sm'])
            S.op('act', lambda e: e.activation(out=egc, in_=pb[b][0:64, 0:16], func=AF.Exp), reads=[pk(b)], writes=['sm'])
            S.op('act', lambda e: e.activation(out=dd, in_=pb[b][0:64, 16:32], func=AF.Copy), reads=[pk(b)], writes=['sm'])
            S.op('dve', lambda e: e.tensor_tensor(out=dd, in0=dd, in1=gcum_sb, op=ALU.subtract), reads=['sm'], writes=['sm'])
            S.op('act', lambda e: e.activation(out=ekd, in_=dd, func=AF.Exp), reads=['sm'], writes=['sm'])
            S.op('act', lambda e: e.activation(out=egl[:], in_=pb[b][:, 32:48], func=AF.Exp), reads=[pk(b)], writes=['egl'])
            S.op('dve', lambda e: e.scalar_tensor_tensor(out=cc_, in0=beta, scalar=-1.0, in1=egc, op0=ALU.mult, op1=ALU.mult),
                 reads=['gtm', 'sm'], writes=['sm'])
            S.op('dve', lambda e: e.tensor_scalar(out=nbeta, in0=beta, scalar1=-1.0, scalar2=None, op0=ALU.mult), reads=['gtm'], writes=['sm'])
            if dn_stop <= 1:
                return
            S.op('dve', lambda e: e.tensor_tensor(out=r3(Gm[:], 64), in0=bc(tri, 1, [64, 16, 64]), in1=bc(g, 2, [64, 16, 64]), op=ALU.mult),
                 reads=['gtm', 'cs'], writes=['Gm'])
            S.op('dve', lambda e: e.tensor_copy(out=r3(gb[:], 64), in_=bc(g, 2, [64, 16, 64])), reads=['gtm'], writes=['gb'])
            for hh in range(2):
                b = nb()
                S.op('pe', lambda e: e.matmul(pb[b][0:64, :], lhsT=tri, rhs=gb[:, hh * 512:(hh + 1) * 512], start=True, stop=False),
                     reads=['gb', 'cs'], writes=[pk(b)])
                S.op('pe', lambda e: e.matmul(pb[b][0:64, :], lhsT=negones, rhs=Gm[:, hh * 512:(hh + 1) * 512], start=False, stop=True),
                     reads=['Gm', 'cs'], writes=[pk(b)])
                S.op('dve', lambda e: e.tensor_tensor(out=r3(dl[:], 64), in0=r3(pb[b][0:64, :], 64), in1=bc(nmL, 1, [64, 8, 64]), op=ALU.add),
                     reads=[pk(b), 'cs'], writes=['dl'])
                S.op('act', lambda e: e.activation(out=decS[hh][:], in_=dl[:], func=AF.Exp), reads=['dl'], writes=['decS%d' % hh])
                S.op('dve', lambda e: e.scalar_tensor_tensor(out=r3(du[:], 64), in0=r3(pb[b][0:64, :], 64), scalar=-1.0, in1=bc(nmU, 1, [64, 8, 64]),
                                                             op0=ALU.mult, op1=ALU.add), reads=[pk(b), 'cs'], writes=['du'])
                S.op('act', lambda e: e.activation(out=decT[hh][:], in_=du[:], func=AF.Exp), reads=['du'], writes=['decT%d' % hh])
            if dn_stop <= 2:
                return
            for hh in range(2):
                b = nb()
                for hl in range(8):
                    h = hh * 8 + hl
                    S.op('pe', lambda e: e.matmul(pb[b][0:64, hl * 64:(hl + 1) * 64], lhsT=kT(h), rhs=kT(h), start=True, stop=True),
                         reads=[KK_], writes=[pk(b)])
                S.op('dve', lambda e: e.tensor_tensor(out=tN[:], in0=pb[b][0:64, :], in1=decS[hh][:], op=ALU.mult),
                     reads=[pk(b), 'decS%d' % hh], writes=['tN'])
                S.op('dve', lambda e: e.tensor_tensor(out=r3(Nm[hh][:], 64), in0=r3(tN[:], 64), in1=bc(nbeta[:, hh * 8:hh * 8 + 8], 2, [64, 8, 64]), op=ALU.mult),
                     reads=['tN', 'sm'], writes=['Nm%d' % hh])
                if dn_stop <= 2.3:
                    continue
                b2 = nb()
                for hl in range(8):
                    S.op('pe', lambda e: e.transpose(out=pb[b2][0:64, hl * 64:(hl + 1) * 64], in_=Nm[hh][:, hl * 64:(hl + 1) * 64], identity=eye64),
                         reads=['Nm%d' % hh, 'cs'], writes=[pk(b2)])
                if dn_stop <= 2.6:
                    continue
                S.op('act', lambda e: e.activation(out=Qm[hh][:], in_=pb[b2][0:64, :], func=AF.Copy), reads=[pk(b2)], writes=['Qm%d' % hh])
                if dn_stop <= 2.7:
                    continue
                S.op('dve', lambda e: e.tensor_tensor(out=r3(Rm[hh][:], 64), in0=r3(Qm[hh][:], 64), in1=bc(eye64, 1, [64, 8, 64]), op=ALU.add),
                     reads=['Qm%d' % hh, 'cs'], writes=['Rm%d' % hh])
            if dn_stop <= 3:
                return
            cur = [(Nm[0], 'Nm0', Qm[0], 'Qm0'), (Nm[1], 'Nm1', Qm[1], 'Qm1')]
            for lvl in range(5):
                bNs, bQs = [], []
                for hh in range(2):
                    Nc, Nk, Qc, Qk = cur[hh]
                    bN = nb()
                    for hl in range(8):
                        sl = slice(hl * 64, (hl + 1) * 64)
                        S.op('pe', lambda e: e.matmul(pb[bN][0:64, sl], lhsT=Qc[:, sl], rhs=Nc[:, sl], start=True, stop=True),
                             reads=[Nk, Qk], writes=[pk(bN)])
                    bNs.append(bN)
                    if lvl < 4:
                        bQ = nb()
                        for hl in range(8):
                            sl = slice(hl * 64, (hl + 1) * 64)
                            S.op('pe', lambda e: e.matmul(pb[bQ][0:64, sl], lhsT=Nc[:, sl], rhs=Qc[:, sl], start=True, stop=True),
                                 reads=[Nk, Qk], writes=[pk(bQ)])
                        bQs.append(bQ)
                for hh in range(2):
                    bN = bNs[hh]
                    nk, qk = 'Nn%d_%d' % (hh, lvl % 2), 'Qn%d_%d' % (hh, lvl % 2)
                    S.op('act', lambda e: e.activation(out=Nn[hh][lvl % 2][:], in_=pb[bN][0:64, :], func=AF.Copy), reads=[pk(bN)], writes=[nk])
                    S.op('dve', lambda e: e.tensor_tensor(out=r3(N2I[hh][:], 64), in0=r3(Nn[hh][lvl % 2][:], 64), in1=bc(eye64, 1, [64, 8, 64]), op=ALU.add),
                         reads=[nk, 'cs'], writes=['N2I%d' % hh])
                    if lvl < 4:
                        S.op('act', lambda e: e.activation(out=Qn[hh][lvl % 2][:], in_=pb[bQs[hh]][0:64, :], func=AF.Copy), reads=[pk(bQs[hh])], writes=[qk])
                        cur[hh] = (Nn[hh][lvl % 2], nk, Qn[hh][lvl % 2], qk)
                for hh in range(2):
                    bR = nb()
                    for hl in range(8):
                        sl = slice(hl * 64, (hl + 1) * 64)
                        S.op('pe', lambda e: e.matmul(pb[bR][0:64, sl], lhsT=N2I[hh][:, sl], rhs=Rm[hh][:, sl], start=True, stop=True),
                             reads=['N2I%d' % hh, 'Rm%d' % hh], writes=[pk(bR)])
                    if lvl < 4:
                        S.op('act', lambda e: e.activation(out=Rm[hh][:], in_=pb[bR][0:64, :], func=AF.Copy), reads=[pk(bR)], writes=['Rm%d' % hh])
                    else:
                        S.op('act', lambda e: e.activation(out=TinvT[hh][:], in_=pb[bR][0:64, :], func=AF.Copy), reads=[pk(bR)], writes=['TinvT%d' % hh])
            if dn_stop <= 4:
                return
            for hh in range(2):
                b = nb()
                for hl in range(8):
                    S.op('pe', lambda e: e.transpose(out=pbb(b)[0:64, hl * 128:(hl + 1) * 128], in_=kT(hh * 8 + hl), identity=identb[:]),
                         reads=[KK_, 'identb'], writes=[pk(b)])
                S.op('dve', lambda e: e.tensor_tensor(out=r3(kd[hh][:], 128), in0=r3(pbb(b)[0:64, :], 128), in1=bc(ekd[:, hh * 8:hh * 8 + 8], 2, [64, 8, 128]), op=ALU.mult),
                     reads=[pk(b), 'sm'], writes=['kd%d' % hh])
                b = nb()
                for hl in range(8):
                    S.op('pe', lambda e: e.transpose(out=pbb(b)[0:64, hl * 128:(hl + 1) * 128], in_=vT(hh * 8 + hl), identity=identb[:]),
                         reads=[VK_, 'identb'], writes=[pk(b)])
                S.op('dve', lambda e: e.tensor_tensor(out=r3(vb[hh][:], 128), in0=r3(pbb(b)[0:64, :], 128), in1=bc(beta[:, hh * 8:hh * 8 + 8], 2, [64, 8, 128]), op=ALU.mult),
                     reads=[pk(b), 'gtm'], writes=['vb%d' % hh])
            if dn_stop <= 5:
                return
            if omode:
                S.op('dve', lambda e: e.tensor_tensor(out=r3(Ed[:], 64), in0=bc(eye64, 1, [64, 16, 64]), in1=bc(egc, 2, [64, 16, 64]), op=ALU.mult),
                     reads=['sm', 'cs'], writes=['Ed'])
                for hh in range(2):
                    b = nb()
                    for hl in range(8):
                        h = hh * 8 + hl
                        S.op('pe', lambda e: e.matmul(pb[b][0:64, hl * 64:(hl + 1) * 64], lhsT=kT(h), rhs=qT(h), start=True, stop=True),
                             reads=[KK_, QK_], writes=[pk(b)])
                    S.op('dve', lambda e: e.tensor_tensor(out=attnT[hh][:], in0=pb[b][0:64, :], in1=decT[hh][:], op=ALU.mult),
                         reads=[pk(b), 'decT%d' % hh], writes=['attnT%d' % hh])
                    b = nb()
                    S.op('pe', lambda e: e.matmul(pb[b][:, :], lhsT=ones[0:64, :], rhs=Ed[:, hh * 512:(hh + 1) * 512], start=True, stop=True),
                         reads=['Ed', 'cs'], writes=[pk(b)])
                    S.op('dve', lambda e: e.tensor_tensor(out=qs[hh][:], in0=qT8(hh), in1=r3(pb[b][:, :], 64), op=ALU.mult),
                         reads=[QK_, pk(b)], writes=['qs%d' % hh])
            if dn_stop <= 6:
                return
            for qd in range(4):
                hh, hb, par = qd // 2, (qd % 2) * 4, qd % 2
                b = nb()
                for hl in range(4):
                    h = 4 * qd + hl
                    S.op('pe', lambda e: e.matmul(pb[b][0:64, hl * 128:(hl + 1) * 128], lhsT=kT(h), rhs=Sb[:, h, :], start=True, stop=True),
                         reads=[KK_, 'Sb%d' % qd], writes=[pk(b)])
                S.op('dve', lambda e: e.tensor_tensor(out=r3(tr[par][:], 128), in0=r3(pb[b][0:64, :], 128), in1=bc(cc_[:, 4 * qd:4 * qd + 4], 2, [64, 4, 128]), op=ALU.mult),
                     reads=[pk(b), 'sm'], writes=['tr%d' % par])
                S.op('dve', lambda e: e.tensor_tensor(out=rr[par][:], in0=tr[par][:], in1=vb[hh][:, hb * 128:(hb + 4) * 128], op=ALU.add),
                     reads=['tr%d' % par, 'vb%d' % hh], writes=['rr%d' % par])
                b2 = nb()
                for hl in range(4):
                    S.op('pe', lambda e: e.matmul(pb[b2][0:64, hl * 128:(hl + 1) * 128], lhsT=TinvT[hh][:, (hb + hl) * 64:(hb + hl + 1) * 64],
                                                  rhs=rr[par][:, hl * 128:(hl + 1) * 128], start=True, stop=True),
                         reads=['TinvT%d' % hh, 'rr%d' % par], writes=[pk(b2)])
                S.op('act', lambda e: e.activation(out=vn[par][:], in_=pb[b2][0:64, :], func=AF.Copy), reads=[pk(b2)], writes=['vn%d' % par])
                if omode:
                    b3 = nb()
                    for hl in range(4):
                        h = 4 * qd + hl
                        S.op('pe', lambda e: e.matmul(pb[b3][:, hl * 64:(hl + 1) * 64], lhsT=Sb[:, h, :], rhs=qs[hh][:, hb + hl, :], start=True, stop=False),
                             reads=['Sb%d' % qd, 'qs%d' % hh], writes=[pk(b3)])
                        S.op('pe', lambda e: e.matmul(pb[b3][:, hl * 64:(hl + 1) * 64], lhsT=vn[par][:, hl * 128:(hl + 1) * 128],
                                                      rhs=attnT[hh][:, (hb + hl) * 64:(hb + hl + 1) * 64], start=False, stop=True),
                             reads=['vn%d' % par, 'attnT%d' % hh], writes=[pk(b3)])
                    if omode == 'set':
                        S.op('act', lambda e: e.activation(out=otv(qd), in_=r3(pb[b3][:, 0:256], 64), func=AF.Copy), reads=[pk(b3)], writes=['OT'])
                    else:
                        S.op('dve', lambda e: e.tensor_tensor(out=otv(qd), in0=r3(pb[b3][:, 0:256], 64), in1=otv(qd), op=ALU.add),
                             reads=[pk(b3), 'OT'], writes=['OT'])
                b4 = nb()
                for hl in range(4):
                    S.op('pe', lambda e: e.matmul(pb[b4][:, hl * 128:(hl + 1) * 128], lhsT=kd[hh][:, (hb + hl) * 128:(hb + hl + 1) * 128],
                                                  rhs=vn[par][:, hl * 128:(hl + 1) * 128], start=True, stop=True),
                         reads=['kd%d' % hh, 'vn%d' % par], writes=[pk(b4)])
                for hl in range(4):
                    h = 4 * qd + hl
                    S.op('dve', lambda e: e.scalar_tensor_tensor(out=Sf[:, h, :], in0=Sf[:, h, :], scalar=egl[:, h:h + 1], in1=pb[b4][:, hl * 128:(hl + 1) * 128],
                                                                 op0=ALU.mult, op1=ALU.add), reads=['Sf%d' % qd, 'egl', pk(b4)], writes=['Sf%d' % qd])
                S.op('act', lambda e: e.activation(out=Sb[:, 4 * qd:4 * qd + 4, :], in_=Sf[:, 4 * qd:4 * qd + 4, :], func=AF.Copy),
                     reads=['Sf%d' % qd], writes=['Sb%d' % qd])

        def fwd(t, h, c0):
            return t[:, h, c0:c0 + 64]

        def rev(t, h, c0):
            if c0 == 0:
                return t[:, h, 63::-1]
            return t[:, h, c0 + 63:c0 - 1:-1]

        def gfwd(c0):
            return gT[:, c0:c0 + 64]

        def grev(c0):
            if c0 == 0:
                return gT[:, 63::-1]
            return gT[:, c0 + 63:c0 - 1:-1]

        def reset_state():
            for qd in range(4):
                S.op('dve', lambda e: e.memset(Sf[:, 4 * qd:4 * qd + 4, :], 0.0), writes=['Sf%d' % qd])
                S.op('dve', lambda e: e.memset(Sb[:, 4 * qd:4 * qd + 4, :], 0.0), writes=['Sb%d' % qd])

        def load_blk(Ksrc, Vsrc, Qsrc, t0, n):
            S.dma('sp', 'kblk', lambda e: e.dma_start(out=kblk[:, :, 0:n], in_=Ksrc.rearrange("h p t -> p h t")[:, :, t0:t0 + n]), writes=['kblk'])
            S.dma('sp', 'vblk', lambda e: e.dma_start(out=vblk[:, :, 0:n], in_=Vsrc.rearrange("h p t -> p h t")[:, :, t0:t0 + n]), writes=['vblk'])
            if Qsrc is not None:
                S.dma('sp', 'qblk', lambda e: e.dma_start(out=qblk[:, :, 0:n], in_=Qsrc.rearrange("h p t -> p h t")[:, :, t0:t0 + n]), writes=['qblk'])

        def run_stream(Ksrc, Vsrc, Qsrc, nblk, gbase, goff, boff, reverse, omode):
            blks = range(nblk - 1, -1, -1) if reverse else range(nblk)
            for bi in blks:
                load_blk(Ksrc, Vsrc, Qsrc, bi * 256, 256)
                chs = range(3, -1, -1) if reverse else range(4)
                for ci in chs:
                    c0 = ci * 64
                    view = rev if reverse else fwd
                    gv = (grev if reverse else gfwd)(gbase + bi * 256 + c0)
                    tok = bi * 256 + c0

                    def otv(qd, tok=tok):
                        if reverse:
                            if tok == 0:
                                return OT[:, 4 * qd:4 * qd + 4, 63::-1]
                            return OT[:, 4 * qd:4 * qd + 4, tok + 63:tok - 1:-1]
                        return OT[:, 4 * qd:4 * qd + 4, tok:tok + 64]

                    def qT8(hh, c0=c0):
                        if reverse:
                            if c0 == 0:
                                return qblk[:, hh * 8:hh * 8 + 8, 63::-1]
                            return qblk[:, hh * 8:hh * 8 + 8, c0 + 63:c0 - 1:-1]
                        return qblk[:, hh * 8:hh * 8 + 8, c0:c0 + 64]
                    if reverse:
                        def rsl(t, c0=c0):
                            if c0 == 0:
                                return t[:, :, 63::-1]
                            return t[:, :, c0 + 63:c0 - 1:-1]
                        S.op('act', lambda e: e.activation(out=rk[:], in_=rsl(kblk), func=AF.Copy), reads=['kblk'], writes=['rk'])
                        S.op('dve', lambda e: e.tensor_copy(out=rv[:], in_=rsl(vblk)), reads=['vblk'], writes=['rv'])
                        if omode:
                            S.op('act', lambda e: e.activation(out=rq[:], in_=rsl(qblk), func=AF.Copy), reads=['qblk'], writes=['rq'])
                        S.op('dve', lambda e: e.tensor_copy(out=rg[:], in_=gv), reads=['gT'], writes=['rg'])
                        dn_chunk(lambda h: rk[:, h, :], lambda h: rv[:, h, :], lambda h: rq[:, h, :],
                                 lambda hh: rq[:, hh * 8:hh * 8 + 8, :], rg[:], goff, boff, omode, otv,
                                 keys=('rk', 'rv', 'rq', 'rg'))
                    else:
                        dn_chunk(lambda h, c0=c0: view(kblk, h, c0), lambda h, c0=c0: view(vblk, h, c0),
                                 lambda h, c0=c0: view(qblk, h, c0), qT8, gv, goff, boff, omode, otv)

        NDB = DN_BLOCKS
        reset_state()
        run_stream(KTc, VTc, None, 1, 2 * NT, 0, 32, False, None)
        S.barrier()
        run_stream(KTo, VTo, QTo, NDB, 0, 0, 32, False, 'set')
        S.barrier()
        def dn_dump():
            def san(dst, src, rk_, wk_):
                S.op('dve', lambda e: e.tensor_scalar(out=dst, in0=src, scalar1=1e30, scalar2=-1e30, op0=ALU.min, op1=ALU.max), reads=rk_, writes=wk_)
            S.barrier()
            san(OT[:], OT[:], ['OT'], ['OT'])
            S.dma('sp', 'dbg', lambda e: e.dma_start(out=OTD, in_=OT[:]), reads=['OT'], writes=['OTD'])
            san(Sf[:], Sf[:], ['Sf0'], ['Sf0'])
            S.dma('sp', 'dbg', lambda e: e.dma_start(out=DBG[:, 1024:3072], in_=Sf[:].rearrange("p h v -> p (h v)")), reads=['Sf0'], writes=['DBG'])
            for (src, c0, n) in [(decS[0][:], 0, 512), (Nm[0][:], 512, 512), (TinvT[0][:], 3072, 512), (vb[0][:, 0:512], 3584, 512)]:
                san(Gm[:, 0:n], src, [], ['Gm'])
                S.dma('sp', 'dbg', lambda e: e.dma_start(out=DBG[0:64, c0:c0 + n], in_=Gm[:, 0:n]), reads=['Gm'], writes=['DBG'])
            for (src, c0, n) in [(sm[:], 0, 96), (gtm[:], 128, 128), (kd[0][:, 0:512], 256, 512), (vn[0][:], 768, 512)]:
                san(gb[:, 0:n], src, [], ['gb'])
                S.dma('sp', 'dbg', lambda e: e.dma_start(out=DBG[64:128, c0:c0 + n], in_=gb[:, 0:n]), reads=['gb'], writes=['DBG'])
            S.barrier()
        if debug == 8:
            dn_dump()
        for _ in ([0] if debug != 8 else []):
          reset_state()
          run_stream(KTc, VTc, None, 1, 2 * NT, 64, 96, True, None)
          S.barrier()
          if NDB == 4:
              run_stream(KTp, VTp, None, 4, NT, 64, 96, False, None)
              S.barrier()
          run_stream(KTo, VTo, QTo, NDB, 0, 64, 96, True, 'add')
          S.barrier()
        if debug in (4, 7):
            dn_dump()

    if debug in (4, 7, 8):
        S.barrier()
        phDE.close()
        es.close()
        return nc, dbg_outs

    own_tiles = [(0, 512), (512, 512)]
    with ExitStack() as ph:
        convact = sb(ph, "convact", [128, 16, NT], BF16)
        mT = sb(ph, "mT", [128, 16, NT], BF16)
        zt = [sb(ph, "zt%d" % i, [128, NT], BF16) for i in range(2)]
        osq = [sb(ph, "osq%d" % i, [128, 512], BF16) for i in range(2)]
        lt = sb(ph, "lt", [128, 4, 512], F32)
        xj = [sb(ph, "xj%d" % i, [128, NT], F32) for i in range(2)]
        for h in range(16):
            s_ = h % 2
            S.dma('sp', 'zt%d' % s_, lambda e: e.dma_start(out=zt[s_][:], in_=ZT[h]), writes=['zt%d' % s_])
            for (t0, n) in own_tiles:
                S.op('act', lambda e: e.activation(out=osq[0][:], in_=OT[:, h, t0:t0 + n], func=AF.Square), reads=['OT'], writes=['osq0'])
                b = nb()
                S.op('pe', lambda e: e.matmul(pb[b][:, :], lhsT=onesb[:], rhs=osq[0][:], start=True, stop=True), reads=['osq0', 'onesb'], writes=[pk(b)])
                S.op('act', lambda e: e.activation(out=fm_rs[:], in_=pb[b][:, :], func=AF.Sqrt, scale=1.0 / 128, bias=epsc[:, 0:1]),
                     reads=[pk(b), 'epsc'], writes=['fm_rs'])
                S.op('dve', lambda e: e.reciprocal(out=fm_rs[:], in_=fm_rs[:]), reads=['fm_rs'], writes=['fm_rs'])
                S.op('dve', lambda e: e.tensor_tensor(out=fm_t[:], in0=OT[:, h, t0:t0 + n], in1=fm_rs[:], op=ALU.mult), reads=['OT', 'fm_rs'], writes=['fm_t'])
                S.op('dve', lambda e: e.scalar_tensor_tensor(out=OT[:, h, t0:t0 + n], in0=fm_t[:], scalar=pcol('dnng', 0), in1=zt[s_][:, t0:t0 + n],
                                                             op0=ALU.mult, op1=ALU.mult), reads=['fm_t', 'prm', 'zt%d' % s_], writes=['OT'])
        for kc in range(16):
            S.dma('sp', 'cact%d' % (kc % 4), lambda e: e.dma_start(out=convact[:, kc, :], in_=UC[kc]), writes=['convact'])
        for (t0, n) in own_tiles:
            bs, bq = nb(), nb()
            for kc in range(16):
                S.op('pe', lambda e: e.matmul(pb[bs][:, :], lhsT=onesb[:], rhs=convact[:, kc, t0:t0 + n], start=(kc == 0), stop=(kc == 15)),
                     reads=['convact', 'onesb'], writes=[pk(bs)])
                S.op('act', lambda e: e.activation(out=osq[kc % 2][:], in_=convact[:, kc, t0:t0 + n], func=AF.Square), reads=['convact'], writes=['osq%d' % (kc % 2)])
                S.op('pe', lambda e: e.matmul(pb[bq][:, :], lhsT=onesb[:], rhs=osq[kc % 2][:], start=(kc == 0), stop=(kc == 15)),
                     reads=['osq%d' % (kc % 2), 'onesb'], writes=[pk(bq)])
            mean, msq, rs_, nmr = lt[:, 0, :], lt[:, 1, :], lt[:, 2, :], lt[:, 3, :]
            S.op('act', lambda e: e.activation(out=mean, in_=pb[bs][:, :], func=AF.Copy, scale=1.0 / D), reads=[pk(bs)], writes=['lt'])
            S.op('dve', lambda e: e.tensor_tensor(out=msq, in0=mean, in1=mean, op=ALU.mult), reads=['lt'], writes=['lt'])
            S.op('dve', lambda e: e.scalar_tensor_tensor(out=rs_, in0=pb[bq][:, :], scalar=1.0 / D, in1=msq, op0=ALU.mult, op1=ALU.subtract),
                 reads=[pk(bq), 'lt'], writes=['lt'])
            S.op('act', lambda e: e.activation(out=rs_, in_=rs_, func=AF.Sqrt, scale=1.0, bias=epsc[:, 0:1]), reads=['lt', 'epsc'], writes=['lt'])
            S.op('dve', lambda e: e.reciprocal(out=rs_, in_=rs_), reads=['lt'], writes=['lt'])
            S.op('dve', lambda e: e.scalar_tensor_tensor(out=nmr, in0=mean, scalar=-1.0, in1=rs_, op0=ALU.mult, op1=ALU.mult), reads=['lt'], writes=['lt'])
            for kc in range(16):
                S.op('dve', lambda e: e.tensor_tensor(out=fm_t[:], in0=convact[:, kc, t0:t0 + n], in1=rs_, op=ALU.mult), reads=['convact', 'lt'], writes=['fm_t'])
                S.op('dve', lambda e: e.tensor_tensor(out=fm_t[:], in0=fm_t[:], in1=nmr, op=ALU.add), reads=['fm_t', 'lt'], writes=['fm_t'])
                S.op('act', lambda e: e.activation(out=convact[:, kc, t0:t0 + n], in_=fm_t[:], func=AF.Silu, scale=pcol('lng', kc), bias=pcol('lnb', kc)),
                     reads=['fm_t', 'prm'], writes=['convact'])
        for j in range(16):
            s_ = load_w(wco[j])
            z_ = j % 2
            S.dma('sp', 'zt%d' % z_, lambda e: e.dma_start(out=zt[z_][:], in_=BR[j]), writes=['zt%d' % z_])
            for (t0, n) in own_tiles:
                b = mm16(s_, lambda kc: convact[:, kc, t0:t0 + n], n, ['convact'])
                S.op('dve', lambda e: e.tensor_tensor(out=mT[:, j, t0:t0 + n], in0=pb[b][:, 0:n], in1=zt[z_][:, t0:t0 + n], op=ALU.mult),
                     reads=[pk(b), 'zt%d' % z_], writes=['mT'])
        for j in range(16):
            s_ = load_w(wdn[j])
            z_ = j % 2
            S.dma('sp', 'zt%d' % z_, lambda e: e.dma_start(out=zt[z_][:], in_=BR[16 + j]), writes=['zt%d' % z_])
            for (t0, n) in own_tiles:
                b = mm16(s_, lambda kc: OT[:, kc, t0:t0 + n], n, ['OT'])
                S.op('dve', lambda e: e.tensor_tensor(out=fm_t[:], in0=pb[b][:, 0:n], in1=zt[z_][:, t0:t0 + n], op=ALU.mult),
                     reads=[pk(b), 'zt%d' % z_], writes=['fm_t'])
                S.op('dve', lambda e: e.tensor_tensor(out=mT[:, j, t0:t0 + n], in0=fm_t[:], in1=mT[:, j, t0:t0 + n], op=ALU.add),
                     reads=['fm_t', 'mT'], writes=['mT'])
        for j in range(16):
            s_ = load_w(wout[j])
            z_ = j % 2
            S.dma('sp', 'xj%d' % z_, lambda e: e.dma_start(out=xj[z_][:], in_=XT[j]), writes=['xj%d' % z_])
            for (t0, n) in own_tiles:
                b = mm16(s_, lambda kc: mT[:, kc, t0:t0 + n], n, ['mT'])
                S.op('dve', lambda e: e.scalar_tensor_tensor(out=xj[z_][:, t0:t0 + n], in0=pb[b][:, 0:n], scalar=mdc('G2', j), in1=xj[z_][:, t0:t0 + n],
                                                             op0=ALU.mult, op1=ALU.add), reads=[pk(b), 'md', 'xj%d' % z_], writes=['xj%d' % z_])
            S.dma('sp', 'xjo%d' % z_, lambda e: e.dma_start(out=X1[j], in_=xj[z_][:]), reads=['xj%d' % z_], writes=['X1'])
        S.barrier()
    phDE.close()
    if debug == 5:
        S.barrier()
        es.close()
        return nc, dbg_outs

    S.barrier()
    with ExitStack() as ph:
        h2T = sb(ph, "h2T", [128, 16, NT], BF16)
        qpT = sb(ph, "qpT", [128, 16, NT], BF16)
        with ExitStack() as ph2:
            x1t = sb(ph2, "x1t", [128, 16, 512], F32)
            sqt2 = sb(ph2, "sqt2", [128, 16, 512], BF16)

            class _V2:
                def __init__(self, t, o):
                    self.t, self.o = t, o

                def __getitem__(self, key):
                    p, kc, sl = key
                    return self.t[p, kc, self.o + sl.start:self.o + sl.stop]
            for (t0, n) in own_tiles:
                S.dma('sp', 'x1t', lambda e: e.dma_start(out=x1t[:], in_=X1.rearrange("c p t -> p c t")[:, :, t0:t0 + n]), writes=['x1t'])
                fm_norm(ph2, x1t, n, lambda kc: mdc('A2', kc), lambda kc: mdc('B2', kc), _V2(h2T, t0), 'h2T', 'x1t', sqt2, 'sqt2')
            S.barrier()
        for j in range(16):
            s_ = load_w(wq[j])
            for (t0, n) in own_tiles:
                b = mm16(s_, lambda kc: h2T[:, kc, t0:t0 + n], n, ['h2T'])
                S.op('act', lambda e: e.activation(out=qpT[:, j, t0:t0 + n], in_=pb[b][:, 0:n], func=AF.Copy), reads=[pk(b)], writes=['qpT'])
        kT12 = sb(ph, "kT12", [128, 16, 128], BF16)
        ktmp = [sb(ph, "ktmp%d" % i, [128, 128], F32) for i in range(2)]
        for i in range(16):
            src = (pk1 if i < 8 else pk2)[i % 8]
            s_ = i % 2
            S.dma('sp', 'ktmp%d' % s_, lambda e: e.dma_start(out=ktmp[s_][:], in_=src), writes=['ktmp%d' % s_])
            b = nb()
            S.op('pe', lambda e: e.transpose(out=pb[b][:, 0:128], in_=ktmp[s_][:], identity=ident), reads=['ktmp%d' % s_, 'cs'], writes=[pk(b)])
            S.op('act', lambda e: e.activation(out=kT12[:, i, :], in_=pb[b][:, 0:128], func=AF.Copy), reads=[pk(b)], writes=['kT12'])

        h2tm = sb(ph, "h2tm", [128, D], BF16)
        gsl = [sb(ph, "gsl%d" % i, [128, D], BF16) for i in range(4)]
        ysb = sb(ph, "ysb", [128, D], F32)
        x1tile = sb(ph, "x1tile", [128, 16, 128], F32)
        xnt = sb(ph, "xnt", [128, 16, 128], F32)
        sq3 = sb(ph, "sq3", [128, 16, 128], BF16)
        junkb2 = [sb(ph, "junkb%d" % i, [128, D], BF16) for i in range(2)]
        v12 = sb(ph, "v12", [128, 32], F32)
        i12 = sb(ph, "i12", [128, 32], U32)
        i12f = sb(ph, "i12f", [128, 32], F32)
        scr = sb(ph, "scr", [128, 128], F32)
        cand = sb(ph, "cand", [128, 256], F32)
        cidx = sb(ph, "cidx", [128, 256], F32)
        scr2 = sb(ph, "scr2", [128, 256], F32)
        junk = sb(ph, "junk", [128, 256], F32)
        best = sb(ph, "best", [128, 16], F32)
        e16 = sb(ph, "e16", [128, 16], F32)
        sml = sb(ph, "sml", [128, 4], F32)
        idxf = sb(ph, "idxf", [128, 128], F32)
        idxi = sb(ph, "idxi", [128, 128], I32)
        wgt = sb(ph, "wgt", [128, 128], F32)
        pre = sb(ph, "pre", [128, 128], F32)
        gtmp = sb(ph, "gtmp", [128, 3, 128], F32)
        coef = sb(ph, "coef", [128, 128], F32)
        dgs = [sb(ph, "dgs%d" % i, [128, 128], BF16) for i in range(2)]
        bank_pool[0] = [4, 5, 6, 7]
        yb = [0, 1, 2, 3]
        gctr = [0]

        for tt in range(PEER_TILES):
            tok = tt * 128
            for q4 in range(4):
                b = nb()
                for i in range(4):
                    kc = q4 * 4 + i
                    S.op('pe', lambda e: e.transpose(out=pbb(b)[:, i * 128:(i + 1) * 128], in_=h2T[:, kc, tok:tok + 128], identity=identb[:]),
                         reads=['h2T', 'identb'], writes=[pk(b)])
                S.op('act', lambda e: e.activation(out=h2tm[:, q4 * 512:(q4 + 1) * 512], in_=pbb(b)[:, 0:512], func=AF.Copy), reads=[pk(b)], writes=['h2tm'])
            for h in range(8):
                b = nb()
                S.op('pe', lambda e: e.matmul(pb[b][:, 0:128], lhsT=qpT[:, 2 * h, tok:tok + 128], rhs=kT12[:, h, :], start=True, stop=True),
                     reads=['qpT', 'kT12'], writes=[pk(b)])
                S.op('pe', lambda e: e.matmul(pb[b][:, 128:256], lhsT=qpT[:, 2 * h + 1, tok:tok + 128], rhs=kT12[:, 8 + h, :], start=True, stop=True),
                     reads=['qpT', 'kT12'], writes=[pk(b)])
                for (c0, vo) in ((0, 0), (128, 16)):
                    src = pb[b][:, c0:c0 + 128]
                    S.op('dve', lambda e: e.max(out=v12[:, vo:vo + 8], in_=src), reads=[pk(b)], writes=['v12'])
                    S.op('dve', lambda e: e.max_index(out=i12[:, vo:vo + 8], in_max=v12[:, vo:vo + 8], in_values=src), reads=[pk(b), 'v12'], writes=['i12'])
                    S.op('dve', lambda e: e.match_replace(out=scr[:], in_to_replace=v12[:, vo:vo + 8], in_values=src, imm_value=-1e30),
                         reads=[pk(b), 'v12'], writes=['scr'])
                    S.op('dve', lambda e: e.max(out=v12[:, vo + 8:vo + 16], in_=scr[:]), reads=['scr'], writes=['v12'])
                    S.op('dve', lambda e: e.max_index(out=i12[:, vo + 8:vo + 16], in_max=v12[:, vo + 8:vo + 16], in_values=scr[:]),
                         reads=['scr', 'v12'], writes=['i12'])
                S.op('dve', lambda e: e.tensor_copy(out=i12f[:], in_=i12[:]), reads=['i12'], writes=['i12f'])
                S.op('dve', lambda e: e.tensor_tensor(out=r3(cand[:], 16), in0=bc(v12[:, 0:16], 2, [128, 16, 16]), in1=bc(v12[:, 16:32], 1, [128, 16, 16]), op=ALU.add),
                     reads=['v12'], writes=['cand'])
                S.op('dve', lambda e: e.scalar_tensor_tensor(out=r3(cidx[:], 16), in0=bc(i12f[:, 0:16], 2, [128, 16, 16]), scalar=128.0,
                                                             in1=bc(i12f[:, 16:32], 1, [128, 16, 16]), op0=ALU.mult, op1=ALU.add),
                     reads=['i12f'], writes=['cidx'])
                S.op('dve', lambda e: e.max(out=best[:, 0:8], in_=cand[:]), reads=['cand'], writes=['best'])
                S.op('dve', lambda e: e.match_replace(out=scr2[:], in_to_replace=best[:, 0:8], in_values=cand[:], imm_value=-1e30),
                     reads=['cand', 'best'], writes=['scr2'])
                S.op('dve', lambda e: e.max(out=best[:, 8:16], in_=scr2[:]), reads=['scr2'], writes=['best'])
                for k in range(16):
                    S.op('dve', lambda e: e.scalar_tensor_tensor(out=junk[:], in0=cand[:], scalar=best[:, k:k + 1], in1=cidx[:], op0=ALU.is_equal, op1=ALU.mult,
                                                                 accum_out=idxf[:, h * 16 + k:h * 16 + k + 1]),
                         reads=['cand', 'best', 'cidx'], writes=['junk', 'idxf'])
                S.op('dve', lambda e: e.tensor_scalar(out=sml[:, 0:1], in0=best[:, 0:1], scalar1=-1.0, scalar2=None, op0=ALU.mult), reads=['best'], writes=['sml'])
                S.op('act', lambda e: e.activation(out=e16[:], in_=best[:], func=AF.Exp, bias=sml[:, 0:1], scale=1.0, accum_out=sml[:, 1:2]),
                     reads=['best', 'sml'], writes=['e16', 'sml'])
                S.op('dve', lambda e: e.reciprocal(out=sml[:, 2:3], in_=sml[:, 1:2]), reads=['sml'], writes=['sml'])
                S.op('dve', lambda e: e.tensor_scalar(out=wgt[:, h * 16:(h + 1) * 16], in0=e16[:], scalar1=sml[:, 2:3], scalar2=None, op0=ALU.mult),
                     reads=['e16', 'sml'], writes=['wgt'])
            S.op('dve', lambda e: e.tensor_scalar(out=idxf[:], in0=idxf[:], scalar1=float(PUR - 1), scalar2=0.0, op0=ALU.min, op1=ALU.max),
                 reads=['idxf'], writes=['idxf'])
            S.op('dve', lambda e: e.tensor_copy(out=idxi[:], in_=idxf[:]), reads=['idxf'], writes=['idxi'])
            for s in range(128):
                g_ = gctr[0] % 4
                gctr[0] += 1
                S.dma('pool', 'gsl%d' % g_, lambda e: e.indirect_dma_start(out=gsl[g_][:], out_offset=None, in_=PUB,
                      in_offset=bass.IndirectOffsetOnAxis(ap=idxi[:, s:s + 1], axis=0)), reads=['idxi'], writes=['gsl%d' % g_])
                jb_ = s % 2
                S.op('dve', lambda e: e.tensor_tensor(out=junkb2[jb_][:], in0=gsl[g_][:], in1=h2tm[:], op=ALU.mult),
                     reads=['gsl%d' % g_, 'h2tm'], writes=['junkb%d' % jb_])
                S.op('act', lambda e: e.activation(out=junkb2[jb_][:], in_=junkb2[jb_][:], func=AF.Copy, accum_out=pre[:, s:s + 1]),
                     reads=['junkb%d' % jb_], writes=['junkb%d' % jb_, 'pre%d' % (s % 8)])
            S.op('dve', lambda e: e.tensor_tensor(out=gtmp[:, 0, :], in0=pre[:], in1=pre[:], op=ALU.mult), reads=['pre%d' % i for i in range(8)], writes=['gtmp'])
            S.op('dve', lambda e: e.tensor_scalar(out=gtmp[:, 0, :], in0=gtmp[:, 0, :], scalar1=0.044715, scalar2=1.0, op0=ALU.mult, op1=ALU.add),
                 reads=['gtmp'], writes=['gtmp'])
            S.op('dve', lambda e: e.tensor_tensor(out=gtmp[:, 1, :], in0=gtmp[:, 0, :], in1=pre[:], op=ALU.mult), reads=['gtmp'] + ['pre%d' % i for i in range(8)], writes=['gtmp'])
            S.op('act', lambda e: e.activation(out=gtmp[:, 2, :], in_=gtmp[:, 1, :], func=AF.Tanh, scale=0.7978845608028654), reads=['gtmp'], writes=['gtmp'])
            S.op('dve', lambda e: e.scalar_tensor_tensor(out=gtmp[:, 0, :], in0=gtmp[:, 2, :], scalar=1.0, in1=pre[:], op0=ALU.add, op1=ALU.mult),
                 reads=['gtmp'] + ['pre%d' % i for i in range(8)], writes=['gtmp'])
            S.op('dve', lambda e: e.scalar_tensor_tensor(out=coef[:], in0=gtmp[:, 0, :], scalar=0.5, in1=wgt[:], op0=ALU.mult, op1=ALU.mult),
                 reads=['gtmp', 'wgt'], writes=['coef'])
            for s in range(128):
                g_ = gctr[0] % 4
                gctr[0] += 1
                S.dma('pool', 'gsl%d' % g_, lambda e: e.indirect_dma_start(out=gsl[g_][:], out_offset=None, in_=PVB,
                      in_offset=bass.IndirectOffsetOnAxis(ap=idxi[:, s:s + 1], axis=0)), reads=['idxi'], writes=['gsl%d' % g_])
                d_ = s % 2
                S.op('dve', lambda e: e.tensor_scalar(out=dgs[d_][:], in0=identb[:], scalar1=coef[:, s:s + 1], scalar2=None, op0=ALU.mult),
                     reads=['identb', 'coef'], writes=['dgs%d' % d_])
                for n4 in range(4):
                    S.op('pe', lambda e: e.matmul(pb[yb[n4]][:, :], lhsT=dgs[d_][:], rhs=gsl[g_][:, n4 * 512:(n4 + 1) * 512], start=(s == 0), stop=(s == 127)),
                         reads=['dgs%d' % d_, 'gsl%d' % g_], writes=[pk(yb[n4])])
            for n4 in range(4):
                S.op('act', lambda e: e.activation(out=ysb[:, n4 * 512:(n4 + 1) * 512], in_=pb[yb[n4]][:, :], func=AF.Copy), reads=[pk(yb[n4])], writes=['ysb'])
            S.dma('sp', 'x1tile', lambda e: e.dma_start(out=x1tile[:], in_=X1.rearrange("c p t -> p c t")[:, :, tok:tok + 128]), writes=['x1tile'])
            for q4 in range(4):
                b = nb()
                for i in range(4):
                    kc = q4 * 4 + i
                    S.op('pe', lambda e: e.transpose(out=pb[b][:, i * 128:(i + 1) * 128], in_=ysb[:, kc * 128:(kc + 1) * 128], identity=ident),
                         reads=['ysb', 'cs'], writes=[pk(b)])
                for i in range(4):
                    kc = q4 * 4 + i
                    S.op('dve', lambda e: e.scalar_tensor_tensor(out=x1tile[:, kc, :], in0=pb[b][:, i * 128:(i + 1) * 128], scalar=mdc('G5', kc), in1=x1tile[:, kc, :],
                                                                 op0=ALU.mult, op1=ALU.add), reads=[pk(b), 'md', 'x1tile'], writes=['x1tile'])
            fm_norm(ph, x1tile, 128, lambda kc: pcol('gf', kc), None, xnt, 'xnt', 'x1tile', sq3, 'sq3')
            for q4 in range(4):
                b = nb()
                for i in range(4):
                    kc = q4 * 4 + i
                    S.op('pe', lambda e: e.transpose(out=pb[b][:, i * 128:(i + 1) * 128], in_=xnt[:, kc, :], identity=ident), reads=['xnt', 'cs'], writes=[pk(b)])
                S.op('act', lambda e: e.activation(out=ysb[:, q4 * 512:(q4 + 1) * 512], in_=pb[b][:, :], func=AF.Copy), reads=[pk(b)], writes=['ysb'])
            S.dma('sp', 'outst', lambda e: e.dma_start(out=out[tok:tok + 128, :], in_=ysb[:]), reads=['ysb'], writes=['out'])
        if debug == 6:
            S.dma('sp', 'dbg', lambda e: e.dma_start(out=DBG[:, 0:128], in_=idxf[:]), reads=['idxf'], writes=['DBG'])
            S.dma('sp', 'dbg', lambda e: e.dma_start(out=DBG[:, 128:256], in_=wgt[:]), reads=['wgt'], writes=['DBG'])
            S.dma('sp', 'dbg', lambda e: e.dma_start(out=DBG[:, 256:384], in_=pre[:]), reads=['pre'], writes=['DBG'])
            S.dma('sp', 'dbg', lambda e: e.dma_start(out=DBG[:, 384:512], in_=coef[:]), reads=['coef'], writes=['DBG'])
        S.barrier()
    S.barrier()
    es.close()
    return nc, dbg_outs


def _prep_core(inp, core):
    b, half = core // 2, core % 2
    f = np.ascontiguousarray
    x = inp['x'][b]
    ctx = inp['ctx'][b]
    if half == 0:
        xo, xp, xh, cxx = x[0:NT], x[2047:1023:-1], x[NT:NT + 2], ctx
        p1, p2 = 0, 1
    else:
        xo, xp, xh, cxx = x[2047:1023:-1], x[0:NT], x[1023:1021:-1], ctx[::-1]
        p1, p2 = 1, 0

    def colform(v):
        return f(v.reshape(-1, 128).T)

    ccol = np.stack([colform(inp['c'][b]), colform(inp['c_ctx'])], axis=-1)
    wm = inp['w_mod'][0].reshape(16, 128, 48, 256).transpose(2, 1, 0, 3)
    w_in = inp['w_in'][0]
    ab = w_in[:, 12288:12352]
    abp = np.zeros((D, 128), np.float32)
    for gi, d in enumerate((p1, p2)):
        abp[:, gi * 64:gi * 64 + 16] = ab[:, 32 * d:32 * d + 16]
        abp[:, gi * 64 + 32:gi * 64 + 48] = ab[:, 32 * d + 16:32 * d + 32]
    wsel = np.concatenate([w_in[:, 0:12288], abp, w_in[:, 12352:16448]], axis=1)

    def chunked(w):
        n = w.shape[1] // 128
        return f(w.reshape(16, 128, n, 128).transpose(2, 1, 0, 3))

    cw = inp['conv_w'][0]
    sw = inp['dn_short_w'][0]
    if half == 1:
        cw = cw[::-1]
        sw = sw[::-1]
    convw = f(cw.reshape(31, 16, 128).transpose(2, 1, 0))
    shw = f(sw.reshape(5, 48, 128).transpose(2, 1, 0))
    shwf = f(sw[::-1].reshape(5, 48, 128).transpose(2, 1, 0))
    gpar = np.zeros((128, 2), np.float32)
    for gi, d in enumerate((p1, p2)):
        gpar[gi * 64:gi * 64 + 16, 0] = inp['dn_a_log'][0, d]
        gpar[gi * 64:gi * 64 + 16, 1] = inp['dn_dt_bias'][0, d]
    cst = np.zeros((128, 1024), np.float32)
    cst[:, 0:128] = np.eye(128)
    cst[:, 128:256] = 1.0
    i_ = np.arange(64)
    cst[0:64, 256:320] = (i_[:, None] <= i_[None, :])
    cst[0:64, 320:384] = -1.0
    cst[0:64, 384:448] = np.where(i_[:, None] > i_[None, :], 0.0, NEG)
    cst[0:64, 448:512] = np.where(i_[:, None] <= i_[None, :], 0.0, NEG)
    m = dict(
        xo=f(xo), xp=f(xp), xh=f(xh), cx=f(cxx), ccol=f(ccol), wmod=f(wm), bmod=colform(inp['b_mod'][0]),
        g1=colform(inp['norm1_g'][0]), g2=colform(inp['norm2_g'][0]), gf=colform(inp['final_g']),
        win=chunked(wsel), convw=convw, convb=colform(inp['conv_b'][0]), lng=colform(inp['conv_ln_g'][0]),
        lnb=colform(inp['conv_ln_b'][0]), wco=chunked(inp['w_conv_out'][0]), wdn=chunked(inp['w_dn_out'][0]),
        wout=chunked(inp['w_out'][0]), wq=chunked(inp['peer_w_q'][0]), shw=shw, shwf=shwf, gpar=gpar,
        dnng=f(inp['dn_norm_g'][0].reshape(128, 1)), pk1=f(inp['peer_key1'][0]), pk2=f(inp['peer_key2'][0]),
        pu=f(inp['peer_u'][0]), pv=f(inp['peer_v'][0]), cst=cst,
    )
    return {k: np.ascontiguousarray(v, dtype=np.float32) for k, v in m.items()}


def kernel(**inputs):
    inp = {k: np.asarray(v) for k, v in inputs.items()}
    nc, _ = build_program(0)
    in_maps = [_prep_core(inp, c) for c in range(8)]
    res = run_bass_kernel_spmd(nc, in_maps, core_ids=list(range(8)))
    outp = np.zeros((4, 2048, D), np.float32)
    for c in range(8):
        b, half = c // 2, c % 2
        o = np.asarray(res.results[c]['out'])
        if half == 0:
            outp[b, 0:NT] = o
        else:
            outp[b, NT:] = o[::-1]
    return outp
```

```python
import os
from contextlib import ExitStack

import numpy as np
import concourse.bass as bass
import concourse.mybir as mybir
from concourse.bass_utils import run_bass_kernel_spmd

F32 = mybir.dt.float32
BF16 = mybir.dt.bfloat16
I32 = mybir.dt.int32
U32 = mybir.dt.uint32
AF = mybir.ActivationFunctionType
ALU = mybir.AluOpType

D = 2048
NT = 1024
NCX = 256
EPS = 1e-6
NEG = -30000.0


class Sched:
    def __init__(self, nc):
        self.nc = nc
        self.engs = {}
        self.state = {}
        self.dma_sems = {}
        self._ctx = []
        for name, obj in [('pe', nc.tensor), ('act', nc.scalar), ('dve', nc.vector),
                          ('pool', nc.gpsimd), ('sp', nc.sync)]:
            cm = nc.semaphore('s_' + name)
            sem = cm.__enter__()
            self._ctx.append(cm)
            self.engs[name] = dict(name=name, obj=obj, sem=sem, count=0, known={})

    def _st(self, k):
        if k not in self.state:
            self.state[k] = dict(w=None, r=[])
        return self.state[k]

    def _wait(self, eng, need):
        for key, (sem, val) in need.items():
            if sem is eng['sem'] and eng['name'] == 'pe':
                continue
            if eng['known'].get(key, 0) < val:
                eng['obj'].wait_ge(sem, val)
                eng['known'][key] = val

    def _deps(self, eng, reads, writes):
        need = {}

        def add(tok):
            if tok is None:
                return
            sem, val = tok
            key = id(sem)
            if key not in need or need[key][1] < val:
                need[key] = (sem, val)
        for k in reads:
            add(self._st(k)['w'])
        for k in writes:
            st = self._st(k)
            add(st['w'])
            for r in st['r']:
                add(r)
        self._wait(eng, need)

    def _mark(self, tok, reads, writes):
        for k in reads:
            st = self._st(k)
            st['r'] = [r for r in st['r'] if r[0] is not tok[0]] + [tok]
        for k in writes:
            st = self._st(k)
            st['w'] = tok
            st['r'] = []

    def op(self, engname, fn, reads=(), writes=()):
        eng = self.engs[engname]
        self._deps(eng, reads, writes)
        inst = fn(eng['obj'])
        eng['count'] += 1
        inst.then_inc(eng['sem'], 1)
        self._mark((eng['sem'], eng['count']), reads, writes)
        return inst

    def dma(self, engname, slot, fn, reads=(), writes=()):
        eng = self.engs[engname]
        self._deps(eng, reads, writes)
        if slot not in self.dma_sems:
            cm = self.nc.semaphore('d_' + slot)
            sem = cm.__enter__()
            self._ctx.append(cm)
            self.dma_sems[slot] = [sem, 0]
        ent = self.dma_sems[slot]
        inst = fn(eng['obj'])
        ent[1] += 16
        inst.then_inc(ent[0], 16)
        self._mark((ent[0], ent[1]), reads, writes)
        return inst

    def barrier(self):
        need = {}
        for e in self.engs.values():
            if e['count'] > 0:
                need[id(e['sem'])] = (e['sem'], e['count'])
        for sem, val in self.dma_sems.values():
            if val > 0:
                need[id(sem)] = (sem, val)
        for e in self.engs.values():
            n2 = {k: v for k, v in need.items() if v[0] is not e['sem']}
            for key, (sem, val) in n2.items():
                if e['known'].get(key, 0) < val:
                    e['obj'].wait_ge(sem, val)
                    e['known'][key] = val
        self.state = {}
        self._gen = getattr(self, '_gen', 0) + 1
        for e in self.engs.values():
            if e['count'] > 2000:
                cm = self.nc.semaphore('s_%s_%d' % (e['name'], self._gen))
                e['sem'] = cm.__enter__()
                self._ctx.append(cm)
                e['count'] = 0


C_GLU_A, C_GLU_G, C_Q, C_K, C_V, C_Z, C_AB, C_BRC, C_BRD = 0, 16, 32, 48, 64, 80, 96, 97, 113
N_WIN_CH = 129


def build_program(debug=0, DN_BLOCKS=4, PEER_TILES=8, dn_stop=99):
    nc = bass.Bass("TRN2", target_bir_lowering=False)

    def di(name, shape, dt=F32):
        if debug == 7 and name != 'cst':
            shape = [1, 2]
        return nc.dram_tensor(name, list(shape), dt, kind="ExternalInput").ap()

    dbg_outs = []

    def scratch(name, shape, dt):
        if debug:
            dbg_outs.append(name)
            return nc.dram_tensor(name, list(shape), dt, kind="ExternalOutput").ap()
        return nc.dram_tensor(name, list(shape), dt, kind="Internal").ap()

    xo = di("xo", [NT, D]); xp = di("xp", [NT, D]); xh = di("xh", [2, D]); cx = di("cx", [NCX, D])
    ccol = di("ccol", [128, 16, 2])
    wmod = di("wmod", [48, 128, 16, 256]); bmod = di("bmod", [128, 96])
    g1 = di("g1", [128, 16]); g2 = di("g2", [128, 16]); gf = di("gf", [128, 16])
    win = di("win", [N_WIN_CH, 128, 16, 128])
    convw = di("convw", [128, 16, 31]); convb = di("convb", [128, 16])
    lng = di("lng", [128, 16]); lnb = di("lnb", [128, 16])
    wco = di("wco", [16, 128, 16, 128]); wdn = di("wdn", [16, 128, 16, 128])
    wout = di("wout", [16, 128, 16, 128]); wq = di("wq", [16, 128, 16, 128])
    shw = di("shw", [128, 48, 5]); shwf = di("shwf", [128, 48, 5])
    gpar = di("gpar", [128, 2])
    dnng = di("dnng", [128, 1])
    pk1 = di("pk1", [8, 128, 128]); pk2 = di("pk2", [8, 128, 128])
    PUR = 16384 if debug in (0, 6) else 128
    pu = di("pu", [PUR, D]); pv = di("pv", [PUR, D])
    cst = di("cst", [128, 1024])
    out = nc.dram_tensor("out", [NT, D], F32, kind="ExternalOutput").ap()

    XT = scratch("XT", [16, 128, NT], F32)
    X1 = scratch("X1", [16, 128, NT], F32)
    UC = scratch("UC", [16, 128, NT], BF16)
    QTo = scratch("QTo", [16, 128, NT], BF16)
    KTo = scratch("KTo", [16, 128, NT], BF16)
    VTo = scratch("VTo", [16, 128, NT], BF16)
    KTp = scratch("KTp", [16, 128, NT], BF16)
    VTp = scratch("VTp", [16, 128, NT], BF16)
    KTc = scratch("KTc", [16, 128, NCX], BF16)
    VTc = scratch("VTc", [16, 128, NCX], BF16)
    ZT = scratch("ZT", [16, 128, NT], BF16)
    BR = scratch("BR", [32, 128, NT], BF16)
    PUB = nc.dram_tensor("PUB", [PUR, D], BF16, kind="Internal").ap()
    PVB = nc.dram_tensor("PVB", [PUR, D], BF16, kind="Internal").ap()
    if debug:
        DBG = scratch("DBG", [128, 4096], F32)
        OTD = scratch("OTD", [128, 16, NT], BF16)

    S = Sched(nc)
    es = ExitStack()

    def sb(stack, name, shape, dt):
        return stack.enter_context(nc.sbuf_tensor(name, list(shape), dt))

    pb = [es.enter_context(nc.psum_tensor("pb%d" % i, [128, 512], F32)) for i in range(8)]
    bank_ctr = [0]
    bank_pool = [list(range(8))]

    def nb():
        pool = bank_pool[0]
        b = pool[bank_ctr[0] % len(pool)]
        bank_ctr[0] += 1
        return b

    def pk(b):
        return 'pb%d' % b

    cs = sb(es, "cs", [128, 1024], F32)
    S.dma('sp', 'cs', lambda e: e.dma_start(out=cs[:], in_=cst), writes=['cs'])
    ident = cs[:, 0:128]
    ones = cs[:, 128:256]
    tri = cs[0:64, 256:320]
    negones = cs[0:64, 320:384]
    nmL = cs[0:64, 384:448]
    nmU = cs[0:64, 448:512]
    eye64 = cs[0:64, 0:64]
    identb = sb(es, "identb", [128, 128], BF16)
    onesb = sb(es, "onesb", [128, 128], BF16)
    S.op('dve', lambda e: e.tensor_copy(out=identb[:], in_=ident), reads=['cs'], writes=['identb'])
    S.op('dve', lambda e: e.tensor_copy(out=onesb[:], in_=ones), reads=['cs'], writes=['onesb'])

    S_dma_prm = S.dma if debug != 7 else (lambda *a, **k: None)
    prm = sb(es, "prm", [128, 16 * 6 + 96 + 31 * 16 + 48 * 10 + 4], F32)
    o_ = [0]

    def prm_alloc(n):
        a = o_[0]
        o_[0] += n
        return a
    pofs = {}
    for nm_, src, n in [('g1', g1, 16), ('g2', g2, 16), ('gf', gf, 16), ('convb', convb, 16),
                        ('lng', lng, 16), ('lnb', lnb, 16), ('bmod', bmod, 96)]:
        a = prm_alloc(n)
        pofs[nm_] = a
        S_dma_prm('sp', 'prm_' + nm_, lambda e, a=a, n=n, src=src: e.dma_start(out=prm[:, a:a + n], in_=src),
              writes=['prm'])
    a = prm_alloc(31 * 16); pofs['convw'] = a
    S_dma_prm('sp', 'prm_convw', lambda e: e.dma_start(out=prm[:, a:a + 496], in_=convw.rearrange("p c k -> p (c k)")), writes=['prm'])
    a2 = prm_alloc(240); pofs['shw'] = a2
    S_dma_prm('sp', 'prm_shw', lambda e: e.dma_start(out=prm[:, a2:a2 + 240], in_=shw.rearrange("p c k -> p (c k)")), writes=['prm'])
    a3 = prm_alloc(240); pofs['shwf'] = a3
    S_dma_prm('sp', 'prm_shwf', lambda e: e.dma_start(out=prm[:, a3:a3 + 240], in_=shwf.rearrange("p c k -> p (c k)")), writes=['prm'])
    a4 = prm_alloc(2); pofs['gpar'] = a4
    S_dma_prm('sp', 'prm_gpar', lambda e: e.dma_start(out=prm[:, a4:a4 + 2], in_=gpar), writes=['prm'])
    a5 = prm_alloc(1); pofs['dnng'] = a5
    S_dma_prm('sp', 'prm_dnng', lambda e: e.dma_start(out=prm[:, a5:a5 + 1], in_=dnng), writes=['prm'])

    def pcol(name, j):
        a = pofs[name] + j
        return prm[:, a:a + 1]

    md = sb(es, "md", [128, 16 * 10 + 4], F32)
    MD = dict(A1=0, B1=16, A1c=32, B1c=48, A2=64, B2=80, G2=96, G5=112, NEA=160)

    def mdc(name, j):
        a = MD[name] + j
        return md[:, a:a + 1]

    wf = [sb(es, "wf%d" % i, [128, 16, 128], F32) for i in range(2)]
    wb = [sb(es, "wb%d" % i, [128, 16, 128], BF16) for i in range(2)]
    wctr = [0]

    cvt_jobs = []
    if debug != 7:
        for src_, dst_ in ((pu, PUB), (pv, PVB)):
            for r0 in range(0, PUR, 128):
                cvt_jobs.append((src_, dst_, r0))
    cvt_state = dict(call=0, bufs=None)

    def cvt_step(n=1):
        if cvt_state['bufs'] is None:
            return
        cvf, cvb = cvt_state['bufs']
        N = len(cvt_jobs)
        for _ in range(n):
            c = cvt_state['call']
            if 2 * c - 4 >= N:
                return
            cvt_state['call'] += 1
            for j in (2 * c, 2 * c + 1):
                if 0 <= j < N:
                    src_, dst_, r0 = cvt_jobs[j]
                    sl = j % 4
                    S.dma('sp', 'cvl%d' % sl, lambda e: e.dma_start(out=cvf[sl][:], in_=src_[r0:r0 + 128, :]), writes=['cvf%d' % sl])
            for j in (2 * c - 2, 2 * c - 1):
                if 0 <= j < N:
                    sl = j % 4
                    S.op('pool', lambda e: e.tensor_copy(out=cvb[sl][:], in_=cvf[sl][:]), reads=['cvf%d' % sl], writes=['cvb%d' % sl])
            for j in (2 * c - 4, 2 * c - 3):
                if 0 <= j < N:
                    src_, dst_, r0 = cvt_jobs[j]
                    sl = j % 4
                    S.dma('sp', 'cvs%d' % sl, lambda e: e.dma_start(out=dst_[r0:r0 + 128, :], in_=cvb[sl][:]), reads=['cvb%d' % sl])

    def load_w(chunk_ap):
        slot = wctr[0] % 2
        wctr[0] += 1
        S.dma('sp', 'wf%d' % slot, lambda e: e.dma_start(out=wf[slot][:], in_=chunk_ap), writes=['wf%d' % slot])
        S.op('pool', lambda e: e.tensor_copy(out=wb[slot][:], in_=wf[slot][:]), reads=['wf%d' % slot], writes=['wb%d' % slot])
        return slot

    def mm16(slot, actfn, N, actkeys):
        b = nb()
        for kc in range(16):
            S.op('pe', lambda e: e.matmul(pb[b][:, 0:N], lhsT=wb[slot][:, kc, :], rhs=actfn(kc),
                                          start=(kc == 0), stop=(kc == 15)),
                 reads=['wb%d' % slot] + list(actkeys), writes=[pk(b)])
        return b

    gT = sb(es, "gT", [128, 2304], F32)

    for ph in ([ExitStack()] if debug != 7 else []):
        cc = sb(ph, "cc", [128, 16, 2], F32)
        sc = sb(ph, "sc", [128, 16, 2], F32)
        wm = [sb(ph, "wm%d" % i, [128, 16, 256], F32) for i in range(2)]
        modT = sb(ph, "modT", [128, 96, 2], F32)
        S.dma('sp', 'cc', lambda e: e.dma_start(out=cc[:], in_=ccol), writes=['cc'])
        S.op('act', lambda e: e.activation(out=sc[:], in_=cc[:], func=AF.Silu), reads=['cc'], writes=['sc'])
        bm = nb()
        for blk in range(48):
            s_ = blk % 2
            S.dma('sp', 'wm%d' % s_, lambda e: e.dma_start(out=wm[s_][:], in_=wmod[blk]), writes=['wm%d' % s_])
            for c2 in range(2):
                ci = blk * 2 + c2
                for kc in range(16):
                    S.op('pe', lambda e: e.matmul(pb[bm][:, ci * 2:ci * 2 + 2], lhsT=wm[s_][:, kc, c2 * 128:(c2 + 1) * 128],
                                                  rhs=sc[:, kc, :], start=(kc == 0), stop=(kc == 15)),
                         reads=['wm%d' % s_, 'sc'], writes=[pk(bm)])
        bmo = pofs['bmod']
        S.op('dve', lambda e: e.tensor_tensor(out=modT[:], in0=pb[bm][:, 0:192].rearrange("p (c t) -> p c t", t=2),
                                              in1=prm[:, bmo:bmo + 96].unsqueeze(2).to_broadcast([128, 96, 2]), op=ALU.add),
             reads=[pk(bm), 'prm'], writes=['modT'])
        for (An, Bn, gname, msc, msh, col) in [('A1', 'B1', 'g1', 1, 0, 0), ('A1c', 'B1c', 'g1', 1, 0, 1), ('A2', 'B2', 'g2', 4, 3, 0)]:
            ga = pofs[gname]
            S.op('dve', lambda e: e.scalar_tensor_tensor(out=md[:, MD[An]:MD[An] + 16], in0=modT[:, msc * 16:(msc + 1) * 16, col],
                                                         scalar=1.0, in1=prm[:, ga:ga + 16], op0=ALU.add, op1=ALU.mult),
                 reads=['modT', 'prm'], writes=['md'])
            S.op('dve', lambda e: e.tensor_copy(out=md[:, MD[Bn]:MD[Bn] + 16], in_=modT[:, msh * 16:(msh + 1) * 16, col]),
                 reads=['modT'], writes=['md'])
        S.op('dve', lambda e: e.tensor_copy(out=md[:, MD['G2']:MD['G2'] + 16], in_=modT[:, 32:48, 0]), reads=['modT'], writes=['md'])
        S.op('dve', lambda e: e.tensor_copy(out=md[:, MD['G5']:MD['G5'] + 16], in_=modT[:, 80:96, 0]), reads=['modT'], writes=['md'])
        gp = pofs['gpar']
        S.op('act', lambda e: e.activation(out=md[:, 160:161], in_=prm[:, gp:gp + 1], func=AF.Exp), reads=['prm'], writes=['md'])
        S.op('dve', lambda e: e.tensor_scalar(out=md[:, 160:161], in0=md[:, 160:161], scalar1=-1.0, scalar2=None, op0=ALU.mult),
             reads=['md'], writes=['md'])
        if debug == 1:
            S.dma('sp', 'dbg', lambda e: e.dma_start(out=DBG[:, 0:164], in_=md[:]), reads=['md'], writes=['DBG'])
        S.barrier()
        ph.close()

    def fm_norm(ph, srcT, N, Acol, Bcol, dst, dstkey, srckey, tmp, tmpkey, out_f32=False):
        b = nb()
        for kc in range(16):
            S.op('act', lambda e: e.activation(out=tmp[:, kc, 0:N], in_=srcT[:, kc, 0:N], func=AF.Square),
                 reads=[srckey], writes=[tmpkey + str(kc)])
            S.op('pe', lambda e: e.matmul(pb[b][:, 0:N], lhsT=onesb[:], rhs=tmp[:, kc, 0:N], start=(kc == 0), stop=(kc == 15)),
                 reads=[tmpkey + str(kc), 'onesb'], writes=[pk(b)])
        rs = fm_rs
        S.op('act', lambda e: e.activation(out=rs[:, 0:N], in_=pb[b][:, 0:N], func=AF.Sqrt, scale=1.0 / D, bias=epsc[:, 0:1]),
             reads=[pk(b), 'epsc'], writes=['fm_rs'])
        S.op('dve', lambda e: e.reciprocal(out=rs[:, 0:N], in_=rs[:, 0:N]), reads=['fm_rs'], writes=['fm_rs'])
        for kc in range(16):
            S.op('dve', lambda e: e.tensor_tensor(out=fm_t[:, 0:N], in0=srcT[:, kc, 0:N], in1=rs[:, 0:N], op=ALU.mult),
                 reads=[srckey, 'fm_rs'], writes=['fm_t'])
            if Bcol is not None:
                S.op('act', lambda e: e.activation(out=dst[:, kc, 0:N], in_=fm_t[:, 0:N], func=AF.Identity,
                                                   scale=Acol(kc), bias=Bcol(kc)),
                     reads=['fm_t', 'md', 'prm'], writes=[dstkey])
            else:
                S.op('act', lambda e: e.activation(out=dst[:, kc, 0:N], in_=fm_t[:, 0:N], func=AF.Identity, scale=Acol(kc), bias=epsc[:, 2:3]),
                     reads=['fm_t', 'md', 'prm', 'epsc'], writes=[dstkey])

    fm_rs = sb(es, "fm_rs", [128, 512], F32)
    fm_t = sb(es, "fm_t", [128, 512], F32)
    epsc = sb(es, "epsc", [128, 4], F32)
    S.op('dve', lambda e: e.memset(epsc[:, 0:1], EPS), writes=['epsc'])
    S.op('dve', lambda e: e.memset(epsc[:, 1:2], 1.0), writes=['epsc'])
    S.op('dve', lambda e: e.memset(epsc[:, 2:3], 0.0), writes=['epsc'])

    for ph in ([ExitStack()] if debug != 7 else []):
        hTo = sb(ph, "hTo", [128, 16, NT + 2], BF16)
        hTp = sb(ph, "hTp", [128, 16, NT], BF16)
        hTc = sb(ph, "hTc", [128, 16, NCX], BF16)
        phB = ExitStack()
        xtm = [sb(phB, "xtm%d" % i, [128, D], F32) for i in range(2)]
        xTt = sb(phB, "xTt", [128, 16, 512], F32)
        sqt = sb(phB, "sqt", [128, 16, 512], BF16)

        class _V:
            def __init__(self, t, o):
                self.t, self.o = t, o

            def __getitem__(self, key):
                p, kc, sl = key
                return self.t[p, kc, self.o + sl.start:self.o + sl.stop]

        def stageB2(src, ntok, dstT, dstkey, off, Aname, Bname, save_xt):
            t0 = 0
            while t0 < ntok:
                n = min(512, ntok - t0)
                for s0 in range(0, n, 128):
                    m = min(128, n - s0)
                    sl = ((t0 + s0) // 128) % 2
                    S.dma('sp', 'xtm%d' % sl, lambda e: e.dma_start(out=xtm[sl][0:m, :], in_=src[t0 + s0:t0 + s0 + m, :]),
                          writes=['xtm%d' % sl])
                    for q4 in range(4):
                        b = nb()
                        for i in range(4):
                            kc = q4 * 4 + i
                            S.op('pe', lambda e: e.transpose(out=pb[b][:, i * 128:i * 128 + m], in_=xtm[sl][0:m, kc * 128:(kc + 1) * 128],
                                                             identity=ident[0:m, 0:m]),
                                 reads=['xtm%d' % sl, 'cs'], writes=[pk(b)])
                        S.op('act', lambda e: e.activation(out=xTt[:, q4 * 4:q4 * 4 + 4, s0:s0 + m],
                                                           in_=pb[b][:, :].rearrange("p (i t) -> p i t", t=128)[:, :, 0:m], func=AF.Copy),
                             reads=[pk(b)], writes=['xTt'])
                if save_xt:
                    S.dma('sp', 'XTst', lambda e: e.dma_start(out=XT.rearrange("c p t -> p c t")[:, :, t0:t0 + n], in_=xTt[:, :, 0:n]),
                          reads=['xTt'], writes=['XT'])
                fm_norm(ph, xTt, n, lambda kc: mdc(Aname, kc), lambda kc: mdc(Bname, kc),
                        _V(dstT, off + t0), dstkey, 'xTt', sqt, 'sqt')
                t0 += n

        stageB2(xo, NT, hTo, 'hTo', 0, 'A1', 'B1', True)
        stageB2(xh, 2, hTo, 'hTo', NT, 'A1', 'B1', False)
        stageB2(xp, NT, hTp, 'hTp', 0, 'A1', 'B1', False)
        stageB2(cx, NCX, hTc, 'hTc', 0, 'A1c', 'B1c', False)
        if debug == 2:
            S.barrier()
            S.op('act', lambda e: e.activation(out=xTt[:, :, 0:64], in_=hTo[:, :, 0:64], func=AF.Copy), reads=['hTo'], writes=['xTt'])
            S.dma('sp', 'dbg', lambda e: e.dma_start(out=DBG[:, 0:1024], in_=xTt[:, :, 0:64]), reads=['xTt'], writes=['DBG'])

        S.barrier()
        phB.close()
        if cvt_jobs:
            cvt_state['bufs'] = ([sb(ph, "cvf%d" % i, [128, D], F32) for i in range(4)],
                                 [sb(ph, "cvb%d" % i, [128, D], BF16) for i in range(4)])
        upad = sb(ph, "upad", [128, 16, 94], BF16)
        S.op('dve', lambda e: e.memset(upad[:], 0.0), writes=['upad'])
        dg = sb(ph, "dg", [128, 31, 128], BF16)
        dg5 = sb(ph, "dg5", [128, 5, 128], BF16)
        dg5f = sb(ph, "dg5f", [128, 5, 128], BF16)
        po = sb(ph, "po", [128, NT + 6], BF16)
        pp = sb(ph, "pp", [128, NT + 4], BF16)
        pc = sb(ph, "pc", [128, NCX + 4], BF16)
        for t_, k_ in [(po, 'po'), (pp, 'pp'), (pc, 'pc')]:
            S.op('dve', lambda e: e.memset(t_[:], 0.0), writes=[k_])
        sig = sb(ph, "sig", [128, 512], F32)
        stg = [sb(ph, "stg%d" % i, [128, NT], BF16) for i in range(2)]
        stgc = [0]
        sl5s = [sb(ph, "sl5_%d" % i, [128, 512], F32) for i in range(2)]
        sq5s = [sb(ph, "sq5_%d" % i, [128, 512], BF16) for i in range(2)]
        rs5s = [sb(ph, "rs5_%d" % i, [128, 512], F32) for i in range(2)]
        c5ctr = [0]

        own_tiles = [(0, 512), (512, 512)]

        def act_o(t0, n):
            return lambda kc: hTo[:, kc, t0:t0 + n]

        def act_p(t0, n):
            return lambda kc: hTp[:, kc, t0:t0 + n]

        def act_c(t0, n):
            return lambda kc: hTc[:, kc, t0:t0 + n]

        def next_stg():
            i = stgc[0] % 2
            stgc[0] += 1
            return i

        for j in range(16):
            sa = load_w(win[C_GLU_A + j])
            bas = [mm16(sa, act_o(t0, n), n, ['hTo']) for (t0, n) in own_tiles]
            sg = load_w(win[C_GLU_G + j])
            for ti, (t0, n) in enumerate(own_tiles):
                bg = mm16(sg, act_o(t0, n), n, ['hTo'])
                S.op('act', lambda e: e.activation(out=sig[:, 0:n], in_=pb[bg][:, 0:n], func=AF.Sigmoid), reads=[pk(bg)], writes=['sig'])
                S.op('dve', lambda e: e.tensor_tensor(out=upad[:, 8 * ti:8 * ti + 8, 15:79],
                                                      in0=pb[bas[ti]][:, 0:512].rearrange("p (r c) -> p r c", c=64),
                                                      in1=sig[:, 0:512].rearrange("p (r c) -> p r c", c=64), op=ALU.mult),
                     reads=[pk(bas[ti]), 'sig'], writes=['upad'])
            cw = pofs['convw'] + j * 31
            S.op('dve', lambda e: e.tensor_tensor(out=dg[:], in0=identb[:].unsqueeze(1).to_broadcast([128, 31, 128]),
                                                  in1=prm[:, cw:cw + 31].unsqueeze(2).to_broadcast([128, 31, 128]), op=ALU.mult),
                 reads=['identb', 'prm'], writes=['dg'])
            si = next_stg()
            for hf in range(2):
                b = nb()
                for k in range(31):
                    S.op('pe', lambda e: e.matmul(pb[b][:, :], lhsT=dg[:, k, :], rhs=upad[:, 8 * hf:8 * hf + 8, k:k + 64],
                                                  start=(k == 0), stop=(k == 30)),
                         reads=['dg', 'upad'], writes=[pk(b)])
                S.op('act', lambda e: e.activation(out=stg[si][:, hf * 512:(hf + 1) * 512], in_=pb[b][:, :], func=AF.Identity,
                                                   bias=pcol('convb', j), scale=1.0),
                     reads=[pk(b), 'prm'], writes=['stg%d' % si])
            S.dma('sp', 'stgo%d' % si, lambda e: e.dma_start(out=UC[j], in_=stg[si][:]), reads=['stg%d' % si])

        def conv5_store(kind, j, pad, padkey, ntok, taps, dst, dstj):
            si = next_stg()
            t0 = 0
            while t0 < ntok:
                n = min(512, ntok - t0)
                pz = c5ctr[0] % 2
                c5ctr[0] += 1
                sl5, sq5, rs5 = sl5s[pz], sq5s[pz], rs5s[pz]
                b = nb()
                for k in range(5):
                    S.op('pe', lambda e: e.matmul(pb[b][:, 0:n], lhsT=taps[:, k, :], rhs=pad[:, t0 + k:t0 + k + n],
                                                  start=(k == 0), stop=(k == 4)),
                         reads=[padkey, 'dg5', 'dg5f'], writes=[pk(b)])
                if kind == 'v':
                    S.op('act', lambda e: e.activation(out=stg[si][:, t0:t0 + n], in_=pb[b][:, 0:n], func=AF.Silu),
                         reads=[pk(b)], writes=['stg%d' % si])
                else:
                    S.op('act', lambda e: e.activation(out=sl5[:, 0:n], in_=pb[b][:, 0:n], func=AF.Silu), reads=[pk(b)], writes=['sl5_%d' % pz])
                    S.op('act', lambda e: e.activation(out=sq5[:, 0:n], in_=sl5[:, 0:n], func=AF.Square), reads=['sl5_%d' % pz], writes=['sq5_%d' % pz])
                    b2 = nb()
                    S.op('pe', lambda e: e.matmul(pb[b2][:, 0:n], lhsT=onesb[:], rhs=sq5[:, 0:n], start=True, stop=True),
                         reads=['sq5_%d' % pz, 'onesb'], writes=[pk(b2)])
                    S.op('act', lambda e: e.activation(out=rs5[:, 0:n], in_=pb[b2][:, 0:n], func=AF.Sqrt, scale=1.0, bias=epsc[:, 0:1]),
                         reads=[pk(b2), 'epsc'], writes=['rs5_%d' % pz])
                    S.op('dve', lambda e: e.reciprocal(out=rs5[:, 0:n], in_=rs5[:, 0:n]), reads=['rs5_%d' % pz], writes=['rs5_%d' % pz])
                    sc_ = (128.0 ** -0.5) if kind == 'q' else 1.0
                    S.op('dve', lambda e: e.scalar_tensor_tensor(out=stg[si][:, t0:t0 + n], in0=sl5[:, 0:n], scalar=sc_, in1=rs5[:, 0:n],
                                                                 op0=ALU.mult, op1=ALU.mult),
                         reads=['sl5_%d' % pz, 'rs5_%d' % pz], writes=['stg%d' % si])
                t0 += n
            S.dma('sp', 'stgo%d' % si, lambda e: e.dma_start(out=dst[dstj][:, 0:ntok], in_=stg[si][:, 0:ntok]),
                  reads=['stg%d' % si])

        def build_taps(idx):
            a_ = pofs['shw'] + idx * 5
            f_ = pofs['shwf'] + idx * 5
            for (t_, o_k, key_) in ((dg5, a_, 'dg5'), (dg5f, f_, 'dg5f')):
                S.op('dve', lambda e: e.tensor_tensor(out=t_[:], in0=identb[:].unsqueeze(1).to_broadcast([128, 5, 128]),
                                                      in1=prm[:, o_k:o_k + 5].unsqueeze(2).to_broadcast([128, 5, 128]), op=ALU.mult),
                     reads=['identb', 'prm'], writes=[key_])

        for j in range(16):
            for kind, cbase, kidx in [('q', C_Q, 0), ('k', C_K, 16), ('v', C_V, 32)]:
                s_ = load_w(win[cbase + j])
                cvt_step(3)
                build_taps(kidx + j)
                for (t0, n) in own_tiles + [(NT, 2)]:
                    b = mm16(s_, act_o(t0, n), n, ['hTo'])
                    S.op('act', lambda e: e.activation(out=po[:, 2 + t0:2 + t0 + n], in_=pb[b][:, 0:n], func=AF.Copy), reads=[pk(b)], writes=['po'])
                conv5_store(kind, j, po, 'po', NT, dg5, {'q': QTo, 'k': KTo, 'v': VTo}[kind], j)
                if kind != 'q':
                    for (t0, n) in own_tiles:
                        b = mm16(s_, act_p(t0, n), n, ['hTp'])
                        S.op('act', lambda e: e.activation(out=pp[:, 2 + t0:2 + t0 + n], in_=pb[b][:, 0:n], func=AF.Copy), reads=[pk(b)], writes=['pp'])
                    S.op('act', lambda e: e.activation(out=pp[:, 2 + NT:3 + NT], in_=po[:, 2 + NT - 1:2 + NT], func=AF.Copy), reads=['po'], writes=['pp'])
                    S.op('act', lambda e: e.activation(out=pp[:, 3 + NT:4 + NT], in_=po[:, 2 + NT - 2:2 + NT - 1], func=AF.Copy), reads=['po'], writes=['pp'])
                    conv5_store(kind, j, pp, 'pp', NT, dg5f, {'k': KTp, 'v': VTp}[kind], j)
                    b = mm16(s_, act_c(0, NCX), NCX, ['hTc'])
                    S.op('act', lambda e: e.activation(out=pc[:, 2:2 + NCX], in_=pb[b][:, 0:NCX], func=AF.Copy), reads=[pk(b)], writes=['pc'])
                    conv5_store(kind, j, pc, 'pc', NCX, dg5, {'k': KTc, 'v': VTc}[kind], j)

        for j in range(16):
            s_ = load_w(win[C_Z + j])
            si = next_stg()
            for (t0, n) in own_tiles:
                b = mm16(s_, act_o(t0, n), n, ['hTo'])
                S.op('act', lambda e: e.activation(out=stg[si][:, t0:t0 + n], in_=pb[b][:, 0:n], func=AF.Silu), reads=[pk(b)], writes=['stg%d' % si])
            S.dma('sp', 'stgo%d' % si, lambda e: e.dma_start(out=ZT[j], in_=stg[si][:]), reads=['stg%d' % si])

        s_ = load_w(win[C_AB])
        gp = pofs['gpar']
        for (actf, keys, t0, n, goff) in ([(act_o(t0, n), ['hTo'], t0, n, t0) for (t0, n) in own_tiles] +
                                          [(act_p(t0, n), ['hTp'], t0, n, NT + t0) for (t0, n) in own_tiles] +
                                          [(act_c(0, NCX), ['hTc'], 0, NCX, 2 * NT)]):
            b = mm16(s_, actf, n, keys)
            for g0 in (0, 64):
                S.op('act', lambda e: e.activation(out=sig[g0:g0 + 32, 0:n], in_=pb[b][g0:g0 + 32, 0:n], func=AF.Exp,
                                                   bias=prm[g0:g0 + 32, gp + 1:gp + 2], scale=1.0), reads=[pk(b), 'prm'], writes=['sig'])
                S.op('act', lambda e: e.activation(out=sig[g0:g0 + 32, 0:n], in_=sig[g0:g0 + 32, 0:n], func=AF.Ln,
                                                   bias=epsc[g0:g0 + 32, 1:2], scale=1.0), reads=['sig', 'epsc'], writes=['sig'])
                S.op('dve', lambda e: e.tensor_scalar(out=gT[g0:g0 + 32, goff:goff + n], in0=sig[g0:g0 + 32, 0:n],
                                                      scalar1=md[g0:g0 + 32, 160:161], scalar2=None, op0=ALU.mult),
                     reads=['sig', 'md'], writes=['gT'])
                S.op('act', lambda e: e.activation(out=gT[g0 + 32:g0 + 64, goff:goff + n], in_=pb[b][g0 + 32:g0 + 64, 0:n], func=AF.Sigmoid),
                     reads=[pk(b)], writes=['gT'])

        for j in range(32):
            s_ = load_w(win[C_BRC + j])
            si = next_stg()
            for (t0, n) in own_tiles:
                b = mm16(s_, act_o(t0, n), n, ['hTo'])
                S.op('act', lambda e: e.activation(out=stg[si][:, t0:t0 + n], in_=pb[b][:, 0:n], func=AF.Sigmoid), reads=[pk(b)], writes=['stg%d' % si])
            S.dma('sp', 'stgo%d' % si, lambda e: e.dma_start(out=BR[j], in_=stg[si][:]), reads=['stg%d' % si])
        if debug == 3:
            S.dma('sp', 'dbg', lambda e: e.dma_start(out=DBG[:, 0:2304], in_=gT[:]), reads=['gT'], writes=['DBG'])
        cvt_step(10 ** 6)
        cvt_state['bufs'] = None
        S.barrier()
        ph.close()

    if debug in (1, 2, 3):
        S.barrier()
        es.close()
        return nc, dbg_outs

    def r3(ap, inner):
        return ap.rearrange("p (a b) -> p a b", b=inner)

    def pbb(b):
        return pb[b][:, :].bitcast(BF16)

    phDE = ExitStack()
    OT = sb(phDE, "OT", [128, 16, NT], BF16)

    with ExitStack() as ph:
        Sf = sb(ph, "Sf", [128, 16, 128], F32)
        Sb = sb(ph, "Sb", [128, 16, 128], BF16)
        kblk = sb(ph, "kblk", [128, 16, 256], BF16)
        vblk = sb(ph, "vblk", [128, 16, 256], BF16)
        qblk = sb(ph, "qblk", [128, 16, 256], BF16)
        gtm = sb(ph, "gtm", [64, 128], F32)
        sm = sb(ph, "sm", [64, 96], F32)
        egl = sb(ph, "egl", [128, 16], F32)
        Gm = sb(ph, "Gm", [64, 1024], F32)
        gb = sb(ph, "gb", [64, 1024], F32)
        dl = sb(ph, "dl", [64, 512], F32)
        du = sb(ph, "du", [64, 512], F32)
        decS = [sb(ph, "decS%d" % i, [64, 512], F32) for i in range(2)]
        decT = [sb(ph, "decT%d" % i, [64, 512], F32) for i in range(2)]
        tN = sb(ph, "tN", [64, 512], F32)
        Nm = [sb(ph, "Nm%d" % i, [64, 512], F32) for i in range(2)]
        Qm = [sb(ph, "Qm%d" % i, [64, 512], F32) for i in range(2)]
        Nn = [[sb(ph, "Nn%d_%d" % (i, k), [64, 512], F32) for k in range(2)] for i in range(2)]
        Qn = [[sb(ph, "Qn%d_%d" % (i, k), [64, 512], F32) for k in range(2)] for i in range(2)]
        N2I = [sb(ph, "N2I%d" % i, [64, 512], F32) for i in range(2)]
        Rm = [sb(ph, "Rm%d" % i, [64, 512], F32) for i in range(2)]
        TinvT = [sb(ph, "TinvT%d" % i, [64, 512], BF16) for i in range(2)]
        kd = [sb(ph, "kd%d" % i, [64, 1024], BF16) for i in range(2)]
        vb = [sb(ph, "vb%d" % i, [64, 1024], F32) for i in range(2)]
        attnT = [sb(ph, "attnT%d" % i, [64, 512], BF16) for i in range(2)]
        Ed = sb(ph, "Ed", [64, 1024], F32)
        qs = [sb(ph, "qs%d" % i, [128, 8, 64], BF16) for i in range(2)]
        tr = [sb(ph, "tr%d" % i, [64, 512], F32) for i in range(2)]
        rr = [sb(ph, "rr%d" % i, [64, 512], BF16) for i in range(2)]
        vn = [sb(ph, "vn%d" % i, [64, 512], BF16) for i in range(2)]
        rk = sb(ph, "rk", [128, 16, 64], BF16)
        rv = sb(ph, "rv", [128, 16, 64], BF16)
        rq = sb(ph, "rq", [128, 16, 64], BF16)
        rg = sb(ph, "rg", [128, 64], F32)

        gcum_sb, egc, dd, ekd, cc_, nbeta = (sm[:, 0:16], sm[:, 16:32], sm[:, 32:48], sm[:, 48:64], sm[:, 64:80], sm[:, 80:96])

        def bc(ap, axis, shape):
            return ap.unsqueeze(axis).to_broadcast(shape)

        def dn_chunk(kT, vT, qT, qT8, gview, goff, boff, omode, otv, keys=('kblk', 'vblk', 'qblk', 'gT')):
            KK_, VK_, QK_, GK_ = keys
            if dn_stop <= 0:
                return
            b = nb()
            S.op('pe', lambda e: e.transpose(out=pb[b][0:64, 0:128], in_=gview, identity=ident), reads=[GK_, 'cs'], writes=[pk(b)])
            S.op('act', lambda e: e.activation(out=gtm[:], in_=pb[b][0:64, 0:128], func=AF.Copy), reads=[pk(b)], writes=['gtm'])
            g = gtm[:, goff:goff + 16]
            beta = gtm[:, boff:boff + 16]
            b = nb()
            S.op('pe', lambda e: e.matmul(pb[b][0:64, 0:16], lhsT=tri, rhs=g, start=True, stop=True), reads=['gtm', 'cs'], writes=[pk(b)])
            S.op('pe', lambda e: e.matmul(pb[b][0:64, 16:32], lhsT=ones[0:64, 0:64], rhs=g, start=True, stop=True), reads=['gtm', 'cs'], writes=[pk(b)])
            S.op('pe', lambda e: e.matmul(pb[b][:, 32:48], lhsT=ones[0:64, :], rhs=g, start=True, stop=True), reads=['gtm', 'cs'], writes=[pk(b)])
            S.op('act', lambda e: e.activation(out=gcum_sb, in_=pb[b][0:64, 0:16], func=AF.Copy), reads=[pk(b)], writes=['sm'])
            S.op('act', lambda e: e.activation(out=egc, in_=pb[b][0:64, 0:16], func=AF.Exp), reads=[pk(b)], writes=['sm'])
            S.op('act', lambda e: e.activation(out=dd, in_=pb[b][0:64, 16:32], func=AF.Copy), reads=[pk(b)], writes=['sm'])
            S.op('dve', lambda e: e.tensor_tensor(out=dd, in0=dd, in1=gcum_sb, op=ALU.subtract), reads=['sm'], writes=['sm'])
            S.op('act', lambda e: e.activation(out=ekd, in_=dd, func=AF.Exp), reads=['sm'], writes=['sm'])
            S.op('act', lambda e: e.activation(out=egl[:], in_=pb[b][:, 32:48], func=AF.Exp), reads=[pk(b)], writes=['egl'])
            S.op('dve', lambda e: e.scalar_tensor_tensor(out=cc_, in0=beta, scalar=-1.0, in1=egc, op0=ALU.mult, op1=ALU.mult),
                 reads=['gtm', 'sm'], writes=['sm'])
            S.op('dve', lambda e: e.tensor_scalar(out=nbeta, in0=beta, scalar1=-1.0, scalar2=None, op0=ALU.mult), reads=['gtm'], writes=['sm'])
            if dn_stop <= 1:
                return
            S.op('dve', lambda e: e.tensor_tensor(out=r3(Gm[:], 64), in0=bc(tri, 1, [64, 16, 64]), in1=bc(g, 2, [64, 16, 64]), op=ALU.mult),
                 reads=['gtm', 'cs'], writes=['Gm'])
            S.op('dve', lambda e: e.tensor_copy(out=r3(gb[:], 64), in_=bc(g, 2, [64, 16, 64])), reads=['gtm'], writes=['gb'])
            for hh in range(2):
                b = nb()
                S.op('pe', lambda e: e.matmul(pb[b][0:64, :], lhsT=tri, rhs=gb[:, hh * 512:(hh + 1) * 512], start=True, stop=False),
                     reads=['gb', 'cs'], writes=[pk(b)])
                S.op('pe', lambda e: e.matmul(pb[b][0:64, :], lhsT=negones, rhs=Gm[:, hh * 512:(hh + 1) * 512], start=False, stop=True),
                     reads=['Gm', 'cs'], writes=[pk(b)])
                S.op('dve', lambda e: e.tensor_tensor(out=r3(dl[:], 64), in0=r3(pb[b][0:64, :], 64), in1=bc(nmL, 1, [64, 8, 64]), op=ALU.add),
                     reads=[pk(b), 'cs'], writes=['dl'])
                S.op('act', lambda e: e.activation(out=decS[hh][:], in_=dl[:], func=AF.Exp), reads=['dl'], writes=['decS%d' % hh])
                S.op('dve', lambda e: e.scalar_tensor_tensor(out=r3(du[:], 64), in0=r3(pb[b][0:64, :], 64), scalar=-1.0, in1=bc(nmU, 1, [64, 8, 64]),
                                                             op0=ALU.mult, op1=ALU.add), reads=[pk(b), 'cs'], writes=['du'])
                S.op('act', lambda e: e.activation(out=decT[hh][:], in_=du[:], func=AF.Exp), reads=['du'], writes=['decT%d' % hh])
            if dn_stop <= 2:
                return
            for hh in range(2):
                b = nb()
                for hl in range(8):
                    h = hh * 8 + hl
                    S.op('pe', lambda e: e.matmul(pb[b][0:64, hl * 64:(hl + 1) * 64], lhsT=kT(h), rhs=kT(h), start=True, stop=True),
                         reads=[KK_], writes=[pk(b)])
                S.op('dve', lambda e: e.tensor_tensor(out=tN[:], in0=pb[b][0:64, :], in1=decS[hh][:], op=ALU.mult),
                     reads=[pk(b), 'decS%d' % hh], writes=['tN'])
                S.op('dve', lambda e: e.tensor_tensor(out=r3(Nm[hh][:], 64), in0=r3(tN[:], 64), in1=bc(nbeta[:, hh * 8:hh * 8 + 8], 2, [64, 8, 64]), op=ALU.mult),
                     reads=['tN', 'sm'], writes=['Nm%d' % hh])
                if dn_stop <= 2.3:
                    continue
                b2 = nb()
                for hl in range(8):
                    S.op('pe', lambda e: e.transpose(out=pb[b2][0:64, hl * 64:(hl + 1) * 64], in_=Nm[hh][:, hl * 64:(hl + 1) * 64], identity=eye64),
                         reads=['Nm%d' % hh, 'cs'], writes=[pk(b2)])
                if dn_stop <= 2.6:
                    continue
                S.op('act', lambda e: e.activation(out=Qm[hh][:], in_=pb[b2][0:64, :], func=AF.Copy), reads=[pk(b2)], writes=['Qm%d' % hh])
                if dn_stop <= 2.7:
                    continue
                S.op('dve', lambda e: e.tensor_tensor(out=r3(Rm[hh][:], 64), in0=r3(Qm[hh][:], 64), in1=bc(eye64, 1, [64, 8, 64]), op=ALU.add),
                     reads=['Qm%d' % hh, 'cs'], writes=['Rm%d' % hh])
            if dn_stop <= 3:
                return
            cur = [(Nm[0], 'Nm0', Qm[0], 'Qm0'), (Nm[1], 'Nm1', Qm[1], 'Qm1')]
            for lvl in range(5):
                bNs, bQs = [], []
                for hh in range(2):
                    Nc, Nk, Qc, Qk = cur[hh]
                    bN = nb()
                    for hl in range(8):
                        sl = slice(hl * 64, (hl + 1) * 64)
                        S.op('pe', lambda e: e.matmul(pb[bN][0:64, sl], lhsT=Qc[:, sl], rhs=Nc[:, sl], start=True, stop=True),
                             reads=[Nk, Qk], writes=[pk(bN)])
                    bNs.append(bN)
                    if lvl < 4:
                        bQ = nb()
                        for hl in range(8):
                            sl = slice(hl * 64, (hl + 1) * 64)
                            S.op('pe', lambda e: e.matmul(pb[bQ][0:64, sl], lhsT=Nc[:, sl], rhs=Qc[:, sl], start=True, stop=True),
                                 reads=[Nk, Qk], writes=[pk(bQ)])
                        bQs.append(bQ)
                for hh in range(2):
                    bN = bNs[hh]
                    nk, qk = 'Nn%d_%d' % (hh, lvl % 2), 'Qn%d_%d' % (hh, lvl % 2)
                    S.op('act', lambda e: e.activation(out=Nn[hh][lvl % 2][:], in_=pb[bN][0:64, :], func=AF.Copy), reads=[pk(bN)], writes=[nk])
                    S.op('dve', lambda e: e.tensor_tensor(out=r3(N2I[hh][:], 64), in0=r3(Nn[hh][lvl % 2][:], 64), in1=bc(eye64, 1, [64, 8, 64]), op=ALU.add),
                         reads=[nk, 'cs'], writes=['N2I%d' % hh])
                    if lvl < 4:
                        S.op('act', lambda e: e.activation(out=Qn[hh][lvl % 2][:], in_=pb[bQs[hh]][0:64, :], func=AF.Copy), reads=[pk(bQs[hh])], writes=[qk])
                        cur[hh] = (Nn[hh][lvl % 2], nk, Qn[hh][lvl % 2], qk)
                for hh in range(2):
                    bR = nb()
                    for hl in range(8):
                        sl = slice(hl * 64, (hl + 1) * 64)
                        S.op('pe', lambda e: e.matmul(pb[bR][0:64, sl], lhsT=N2I[hh][:, sl], rhs=Rm[hh][:, sl], start=True, stop=True),
                             reads=['N2I%d' % hh, 'Rm%d' % hh], writes=[pk(bR)])
                    if lvl < 4:
                        S.op('act', lambda e: e.activation(out=Rm[hh][:], in_=pb[bR][0:64, :], func=AF.Copy), reads=[pk(bR)], writes=['Rm%d' % hh])
                    else:
                        S.op('act', lambda e: e.activation(out=TinvT[hh][:], in_=pb[bR][0:64, :], func=AF.Copy), reads=[pk(bR)], writes=['TinvT%d' % hh])
            if dn_stop <= 4:
                return
            for hh in range(2):
                b = nb()
                for hl in range(8):
                    S.op('pe', lambda e: e.transpose(out=pbb(b)[0:64, hl * 128:(hl + 1) * 128], in_=kT(hh * 8 + hl), identity=identb[:]),
                         reads=[KK_, 'identb'], writes=[pk(b)])
                S.op('dve', lambda e: e.tensor_tensor(out=r3(kd[hh][:], 128), in0=r3(pbb(b)[0:64, :], 128), in1=bc(ekd[:, hh * 8:hh * 8 + 8], 2, [64, 8, 128]), op=ALU.mult),
                     reads=[pk(b), 'sm'], writes=['kd%d' % hh])
                b = nb()
                for hl in range(8):
                    S.op('pe', lambda e: e.transpose(out=pbb(b)[0:64, hl * 128:(hl + 1) * 128], in_=vT(hh * 8 + hl), identity=identb[:]),
                         reads=[VK_, 'identb'], writes=[pk(b)])
                S.op('dve', lambda e: e.tensor_tensor(out=r3(vb[hh][:], 128), in0=r3(pbb(b)[0:64, :], 128), in1=bc(beta[:, hh * 8:hh * 8 + 8], 2, [64, 8, 128]), op=ALU.mult),
                     reads=[pk(b), 'gtm'], writes=['vb%d' % hh])
            if dn_stop <= 5:
                return
            if omode:
                S.op('dve', lambda e: e.tensor_tensor(out=r3(Ed[:], 64), in0=bc(eye64, 1, [64, 16, 64]), in1=bc(egc, 2, [64, 16, 64]), op=ALU.mult),
                     reads=['sm', 'cs'], writes=['Ed'])
                for hh in range(2):
                    b = nb()
                    for hl in range(8):
                        h = hh * 8 + hl
                        S.op('pe', lambda e: e.matmul(pb[b][0:64, hl * 64:(hl + 1) * 64], lhsT=kT(h), rhs=qT(h), start=True, stop=True),
                             reads=[KK_, QK_], writes=[pk(b)])
                    S.op('dve', lambda e: e.tensor_tensor(out=attnT[hh][:], in0=pb[b][0:64, :], in1=decT[hh][:], op=ALU.mult),
                         reads=[pk(b), 'decT%d' % hh], writes=['attnT%d' % hh])
                    b = nb()
                    S.op('pe', lambda e: e.matmul(pb[b][:, :], lhsT=ones[0:64, :], rhs=Ed[:, hh * 512:(hh + 1) * 512], start=True, stop=True),
                         reads=['Ed', 'cs'], writes=[pk(b)])
                    S.op('dve', lambda e: e.tensor_tensor(out=qs[hh][:], in0=qT8(hh), in1=r3(pb[b][:, :], 64), op=ALU.mult),
                         reads=[QK_, pk(b)], writes=['qs%d' % hh])
            if dn_stop <= 6:
                return
            for qd in range(4):
                hh, hb, par = qd // 2, (qd % 2) * 4, qd % 2
                b = nb()
                for hl in range(4):
                    h = 4 * qd + hl
                    S.op('pe', lambda e: e.matmul(pb[b][0:64, hl * 128:(hl + 1) * 128], lhsT=kT(h), rhs=Sb[:, h, :], start=True, stop=True),
                         reads=[KK_, 'Sb%d' % qd], writes=[pk(b)])
                S.op('dve', lambda e: e.tensor_tensor(out=r3(tr[par][:], 128), in0=r3(pb[b][0:64, :], 128), in1=bc(cc_[:, 4 * qd:4 * qd + 4], 2, [64, 4, 128]), op=ALU.mult),
                     reads=[pk(b), 'sm'], writes=['tr%d' % par])
                S.op('dve', lambda e: e.tensor_tensor(out=rr[par][:], in0=tr[par][:], in1=vb[hh][:, hb * 128:(hb + 4) * 128], op=ALU.add),
                     reads=['tr%d' % par, 'vb%d' % hh], writes=['rr%d' % par])
                b2 = nb()
                for hl in range(4):
                    S.op('pe', lambda e: e.matmul(pb[b2][0:64, hl * 128:(hl + 1) * 128], lhsT=TinvT[hh][:, (hb + hl) * 64:(hb + hl + 1) * 64],
                                                  rhs=rr[par][:, hl * 128:(hl + 1) * 128], start=True, stop=True),
                         reads=['TinvT%d' % hh, 'rr%d' % par], writes=[pk(b2)])
                S.op('act', lambda e: e.activation(out=vn[par][:], in_=pb[b2][0:64, :], func=AF.Copy), reads=[pk(b2)], writes=['vn%d' % par])
                if omode:
                    b3 = nb()
                    for hl in range(4):
                        h = 4 * qd + hl
                        S.op('pe', lambda e: e.matmul(pb[b3][:, hl * 64:(hl + 1) * 64], lhsT=Sb[:, h, :], rhs=qs[hh][:, hb + hl, :], start=True, stop=False),
                             reads=['Sb%d' % qd, 'qs%d' % hh], writes=[pk(b3)])
                        S.op('pe', lambda e: e.matmul(pb[b3][:, hl * 64:(hl + 1) * 64], lhsT=vn[par][:, hl * 128:(hl + 1) * 128],
                                                      rhs=attnT[hh][:, (hb + hl) * 64:(hb + hl + 1) * 64], start=False, stop=True),
                             reads=['vn%d' % par, 'attnT%d' % hh], writes=[pk(b3)])
                    if omode == 'set':
                        S.op('act', lambda e: e.activation(out=otv(qd), in_=r3(pb[b3][:, 0:256], 64), func=AF.Copy), reads=[pk(b3)], writes=['OT'])
                    else:
                        S.op('dve', lambda e: e.tensor_tensor(out=otv(qd), in0=r3(pb[b3][:, 0:256], 64), in1=otv(qd), op=ALU.add),
                             reads=[pk(b3), 'OT'], writes=['OT'])
                b4 = nb()
                for hl in range(4):
                    S.op('pe', lambda e: e.matmul(pb[b4][:, hl * 128:(hl + 1) * 128], lhsT=kd[hh][:, (hb + hl) * 128:(hb + hl + 1) * 128],
                                                  rhs=vn[par][:, hl * 128:(hl + 1) * 128], start=True, stop=True),
                         reads=['kd%d' % hh, 'vn%d' % par], writes=[pk(b4)])
                for hl in range(4):
                    h = 4 * qd + hl
                    S.op('dve', lambda e: e.scalar_tensor_tensor(out=Sf[:, h, :], in0=Sf[:, h, :], scalar=egl[:, h:h + 1], in1=pb[b4][:, hl * 128:(hl + 1) * 128],
                                                                 op0=ALU.mult, op1=ALU.add), reads=['Sf%d' % qd, 'egl', pk(b4)], writes=['Sf%d' % qd])
                S.op('act', lambda e: e.activation(out=Sb[:, 4 * qd:4 * qd + 4, :], in_=Sf[:, 4 * qd:4 * qd + 4, :], func=AF.Copy),
                     reads=['Sf%d' % qd], writes=['Sb%d' % qd])

        def fwd(t, h, c0):
            return t[:, h, c0:c0 + 64]

        def rev(t, h, c0):
            if c0 == 0:
                return t[:, h, 63::-1]
            return t[:, h, c0 + 63:c0 - 1:-1]

        def gfwd(c0):
            return gT[:, c0:c0 + 64]

        def grev(c0):
            if c0 == 0:
                return gT[:, 63::-1]
            return gT[:, c0 + 63:c0 - 1:-1]

        def reset_state():
            for qd in range(4):
                S.op('dve', lambda e: e.memset(Sf[:, 4 * qd:4 * qd + 4, :], 0.0), writes=['Sf%d' % qd])
                S.op('dve', lambda e: e.memset(Sb[:, 4 * qd:4 * qd + 4, :], 0.0), writes=['Sb%d' % qd])

        def load_blk(Ksrc, Vsrc, Qsrc, t0, n):
            S.dma('sp', 'kblk', lambda e: e.dma_start(out=kblk[:, :, 0:n], in_=Ksrc.rearrange("h p t -> p h t")[:, :, t0:t0 + n]), writes=['kblk'])
            S.dma('sp', 'vblk', lambda e: e.dma_start(out=vblk[:, :, 0:n], in_=Vsrc.rearrange("h p t -> p h t")[:, :, t0:t0 + n]), writes=['vblk'])
            if Qsrc is not None:
                S.dma('sp', 'qblk', lambda e: e.dma_start(out=qblk[:, :, 0:n], in_=Qsrc.rearrange("h p t -> p h t")[:, :, t0:t0 + n]), writes=['qblk'])

        def run_stream(Ksrc, Vsrc, Qsrc, nblk, gbase, goff, boff, reverse, omode):
            blks = range(nblk - 1, -1, -1) if reverse else range(nblk)
            for bi in blks:
                load_blk(Ksrc, Vsrc, Qsrc, bi * 256, 256)
                chs = range(3, -1, -1) if reverse else range(4)
                for ci in chs:
                    c0 = ci * 64
                    view = rev if reverse else fwd
                    gv = (grev if reverse else gfwd)(gbase + bi * 256 + c0)
                    tok = bi * 256 + c0

                    def otv(qd, tok=tok):
                        if reverse:
                            if tok == 0:
                                return OT[:, 4 * qd:4 * qd + 4, 63::-1]
                            return OT[:, 4 * qd:4 * qd + 4, tok + 63:tok - 1:-1]
                        return OT[:, 4 * qd:4 * qd + 4, tok:tok + 64]

                    def qT8(hh, c0=c0):
                        if reverse:
                            if c0 == 0:
                                return qblk[:, hh * 8:hh * 8 + 8, 63::-1]
                            return qblk[:, hh * 8:hh * 8 + 8, c0 + 63:c0 - 1:-1]
                        return qblk[:, hh * 8:hh * 8 + 8, c0:c0 + 64]
                    if reverse:
                        def rsl(t, c0=c0):
                            if c0 == 0:
                                return t[:, :, 63::-1]
                            return t[:, :, c0 + 63:c0 - 1:-1]
                        S.op('act', lambda e: e.activation(out=rk[:], in_=rsl(kblk), func=AF.Copy), reads=['kblk'], writes=['rk'])
                        S.op('dve', lambda e: e.tensor_copy(out=rv[:], in_=rsl(vblk)), reads=['vblk'], writes=['rv'])
                        if omode:
                            S.op('act', lambda e: e.activation(out=rq[:], in_=rsl(qblk), func=AF.Copy), reads=['qblk'], writes=['rq'])
                        S.op('dve', lambda e: e.tensor_copy(out=rg[:], in_=gv), reads=['gT'], writes=['rg'])
                        dn_chunk(lambda h: rk[:, h, :], lambda h: rv[:, h, :], lambda h: rq[:, h, :],
                                 lambda hh: rq[:, hh * 8:hh * 8 + 8, :], rg[:], goff, boff, omode, otv,
                                 keys=('rk', 'rv', 'rq', 'rg'))
                    else:
                        dn_chunk(lambda h, c0=c0: view(kblk, h, c0), lambda h, c0=c0: view(vblk, h, c0),
                                 lambda h, c0=c0: view(qblk, h, c0), qT8, gv, goff, boff, omode, otv)

        NDB = DN_BLOCKS
        reset_state()
        run_stream(KTc, VTc, None, 1, 2 * NT, 0, 32, False, None)
        S.barrier()
        run_stream(KTo, VTo, QTo, NDB, 0, 0, 32, False, 'set')
        S.barrier()
        def dn_dump():
            def san(dst, src, rk_, wk_):
                S.op('dve', lambda e: e.tensor_scalar(out=dst, in0=src, scalar1=1e30, scalar2=-1e30, op0=ALU.min, op1=ALU.max), reads=rk_, writes=wk_)
            S.barrier()
            san(OT[:], OT[:], ['OT'], ['OT'])
            S.dma('sp', 'dbg', lambda e: e.dma_start(out=OTD, in_=OT[:]), reads=['OT'], writes=['OTD'])
            san(Sf[:], Sf[:], ['Sf0'], ['Sf0'])
            S.dma('sp', 'dbg', lambda e: e.dma_start(out=DBG[:, 1024:3072], in_=Sf[:].rearrange("p h v -> p (h v)")), reads=['Sf0'], writes=['DBG'])
            for (src, c0, n) in [(decS[0][:], 0, 512), (Nm[0][:], 512, 512), (TinvT[0][:], 3072, 512), (vb[0][:, 0:512], 3584, 512)]:
                san(Gm[:, 0:n], src, [], ['Gm'])
                S.dma('sp', 'dbg', lambda e: e.dma_start(out=DBG[0:64, c0:c0 + n], in_=Gm[:, 0:n]), reads=['Gm'], writes=['DBG'])
            for (src, c0, n) in [(sm[:], 0, 96), (gtm[:], 128, 128), (kd[0][:, 0:512], 256, 512), (vn[0][:], 768, 512)]:
                san(gb[:, 0:n], src, [], ['gb'])
                S.dma('sp', 'dbg', lambda e: e.dma_start(out=DBG[64:128, c0:c0 + n], in_=gb[:, 0:n]), reads=['gb'], writes=['DBG'])
            S.barrier()
        if debug == 8:
            dn_dump()
        for _ in ([0] if debug != 8 else []):
          reset_state()
          run_stream(KTc, VTc, None, 1, 2 * NT, 64, 96, True, None)
          S.barrier()
          if NDB == 4:
              run_stream(KTp, VTp, None, 4, NT, 64, 96, False, None)
              S.barrier()
          run_stream(KTo, VTo, QTo, NDB, 0, 64, 96, True, 'add')
          S.barrier()
        if debug in (4, 7):
            dn_dump()

    if debug in (4, 7, 8):
        S.barrier()
        phDE.close()
        es.close()
        return nc, dbg_outs

    own_tiles = [(0, 512), (512, 512)]
    with ExitStack() as ph:
        convact = sb(ph, "convact", [128, 16, NT], BF16)
        mT = sb(ph, "mT", [128, 16, NT], BF16)
        zt = [sb(ph, "zt%d" % i, [128, NT], BF16) for i in range(2)]
        osq = [sb(ph, "osq%d" % i, [128, 512], BF16) for i in range(2)]
        lt = sb(ph, "lt", [128, 4, 512], F32)
        xj = [sb(ph, "xj%d" % i, [128, NT], F32) for i in range(2)]
        for h in range(16):
            s_ = h % 2
            S.dma('sp', 'zt%d' % s_, lambda e: e.dma_start(out=zt[s_][:], in_=ZT[h]), writes=['zt%d' % s_])
            for (t0, n) in own_tiles:
                S.op('act', lambda e: e.activation(out=osq[0][:], in_=OT[:, h, t0:t0 + n], func=AF.Square), reads=['OT'], writes=['osq0'])
                b = nb()
                S.op('pe', lambda e: e.matmul(pb[b][:, :], lhsT=onesb[:], rhs=osq[0][:], start=True, stop=True), reads=['osq0', 'onesb'], writes=[pk(b)])
                S.op('act', lambda e: e.activation(out=fm_rs[:], in_=pb[b][:, :], func=AF.Sqrt, scale=1.0 / 128, bias=epsc[:, 0:1]),
                     reads=[pk(b), 'epsc'], writes=['fm_rs'])
                S.op('dve', lambda e: e.reciprocal(out=fm_rs[:], in_=fm_rs[:]), reads=['fm_rs'], writes=['fm_rs'])
                S.op('dve', lambda e: e.tensor_tensor(out=fm_t[:], in0=OT[:, h, t0:t0 + n], in1=fm_rs[:], op=ALU.mult), reads=['OT', 'fm_rs'], writes=['fm_t'])
                S.op('dve', lambda e: e.scalar_tensor_tensor(out=OT[:, h, t0:t0 + n], in0=fm_t[:], scalar=pcol('dnng', 0), in1=zt[s_][:, t0:t0 + n],
                                                             op0=ALU.mult, op1=ALU.mult), reads=['fm_t', 'prm', 'zt%d' % s_], writes=['OT'])
        for kc in range(16):
            S.dma('sp', 'cact%d' % (kc % 4), lambda e: e.dma_start(out=convact[:, kc, :], in_=UC[kc]), writes=['convact'])
        for (t0, n) in own_tiles:
            bs, bq = nb(), nb()
            for kc in range(16):
                S.op('pe', lambda e: e.matmul(pb[bs][:, :], lhsT=onesb[:], rhs=convact[:, kc, t0:t0 + n], start=(kc == 0), stop=(kc == 15)),
                     reads=['convact', 'onesb'], writes=[pk(bs)])
                S.op('act', lambda e: e.activation(out=osq[kc % 2][:], in_=convact[:, kc, t0:t0 + n], func=AF.Square), reads=['convact'], writes=['osq%d' % (kc % 2)])
                S.op('pe', lambda e: e.matmul(pb[bq][:, :], lhsT=onesb[:], rhs=osq[kc % 2][:], start=(kc == 0), stop=(kc == 15)),
                     reads=['osq%d' % (kc % 2), 'onesb'], writes=[pk(bq)])
            mean, msq, rs_, nmr = lt[:, 0, :], lt[:, 1, :], lt[:, 2, :], lt[:, 3, :]
            S.op('act', lambda e: e.activation(out=mean, in_=pb[bs][:, :], func=AF.Copy, scale=1.0 / D), reads=[pk(bs)], writes=['lt'])
            S.op('dve', lambda e: e.tensor_tensor(out=msq, in0=mean, in1=mean, op=ALU.mult), reads=['lt'], writes=['lt'])
            S.op('dve', lambda e: e.scalar_tensor_tensor(out=rs_, in0=pb[bq][:, :], scalar=1.0 / D, in1=msq, op0=ALU.mult, op1=ALU.subtract),
                 reads=[pk(bq), 'lt'], writes=['lt'])
            S.op('act', lambda e: e.activation(out=rs_, in_=rs_, func=AF.Sqrt, scale=1.0, bias=epsc[:, 0:1]), reads=['lt', 'epsc'], writes=['lt'])
            S.op('dve', lambda e: e.reciprocal(out=rs_, in_=rs_), reads=['lt'], writes=['lt'])
            S.op('dve', lambda e: e.scalar_tensor_tensor(out=nmr, in0=mean, scalar=-1.0, in1=rs_, op0=ALU.mult, op1=ALU.mult), reads=['lt'], writes=['lt'])
            for kc in range(16):
                S.op('dve', lambda e: e.tensor_tensor(out=fm_t[:], in0=convact[:, kc, t0:t0 + n], in1=rs_, op=ALU.mult), reads=['convact', 'lt'], writes=['fm_t'])
                S.op('dve', lambda e: e.tensor_tensor(out=fm_t[:], in0=fm_t[:], in1=nmr, op=ALU.add), reads=['fm_t', 'lt'], writes=['fm_t'])
                S.op('act', lambda e: e.activation(out=convact[:, kc, t0:t0 + n], in_=fm_t[:], func=AF.Silu, scale=pcol('lng', kc), bias=pcol('lnb', kc)),
                     reads=['fm_t', 'prm'], writes=['convact'])
        for j in range(16):
            s_ = load_w(wco[j])
            z_ = j % 2
            S.dma('sp', 'zt%d' % z_, lambda e: e.dma_start(out=zt[z_][:], in_=BR[j]), writes=['zt%d' % z_])
            for (t0, n) in own_tiles:
                b = mm16(s_, lambda kc: convact[:, kc, t0:t0 + n], n, ['convact'])
                S.op('dve', lambda e: e.tensor_tensor(out=mT[:, j, t0:t0 + n], in0=pb[b][:, 0:n], in1=zt[z_][:, t0:t0 + n], op=ALU.mult),
                     reads=[pk(b), 'zt%d' % z_], writes=['mT'])
        for j in range(16):
            s_ = load_w(wdn[j])
            z_ = j % 2
            S.dma('sp', 'zt%d' % z_, lambda e: e.dma_start(out=zt[z_][:], in_=BR[16 + j]), writes=['zt%d' % z_])
            for (t0, n) in own_tiles:
                b = mm16(s_, lambda kc: OT[:, kc, t0:t0 + n], n, ['OT'])
                S.op('dve', lambda e: e.tensor_tensor(out=fm_t[:], in0=pb[b][:, 0:n], in1=zt[z_][:, t0:t0 + n], op=ALU.mult),
                     reads=[pk(b), 'zt%d' % z_], writes=['fm_t'])
                S.op('dve', lambda e: e.tensor_tensor(out=mT[:, j, t0:t0 + n], in0=fm_t[:], in1=mT[:, j, t0:t0 + n], op=ALU.add),
                     reads=['fm_t', 'mT'], writes=['mT'])
        for j in range(16):
            s_ = load_w(wout[j])
            z_ = j % 2
            S.dma('sp', 'xj%d' % z_, lambda e: e.dma_start(out=xj[z_][:], in_=XT[j]), writes=['xj%d' % z_])
            for (t0, n) in own_tiles:
                b = mm16(s_, lambda kc: mT[:, kc, t0:t0 + n], n, ['mT'])
                S.op('dve', lambda e: e.scalar_tensor_tensor(out=xj[z_][:, t0:t0 + n], in0=pb[b][:, 0:n], scalar=mdc('G2', j), in1=xj[z_][:, t0:t0 + n],
                                                             op0=ALU.mult, op1=ALU.add), reads=[pk(b), 'md', 'xj%d' % z_], writes=['xj%d' % z_])
            S.dma('sp', 'xjo%d' % z_, lambda e: e.dma_start(out=X1[j], in_=xj[z_][:]), reads=['xj%d' % z_], writes=['X1'])
        S.barrier()
    phDE.close()
    if debug == 5:
        S.barrier()
        es.close()
        return nc, dbg_outs

    S.barrier()
    with ExitStack() as ph:
        h2T = sb(ph, "h2T", [128, 16, NT], BF16)
        qpT = sb(ph, "qpT", [128, 16, NT], BF16)
        with ExitStack() as ph2:
            x1t = sb(ph2, "x1t", [128, 16, 512], F32)
            sqt2 = sb(ph2, "sqt2", [128, 16, 512], BF16)

            class _V2:
                def __init__(self, t, o):
                    self.t, self.o = t, o

                def __getitem__(self, key):
                    p, kc, sl = key
                    return self.t[p, kc, self.o + sl.start:self.o + sl.stop]
            for (t0, n) in own_tiles:
                S.dma('sp', 'x1t', lambda e: e.dma_start(out=x1t[:], in_=X1.rearrange("c p t -> p c t")[:, :, t0:t0 + n]), writes=['x1t'])
                fm_norm(ph2, x1t, n, lambda kc: mdc('A2', kc), lambda kc: mdc('B2', kc), _V2(h2T, t0), 'h2T', 'x1t', sqt2, 'sqt2')
            S.barrier()
        for j in range(16):
            s_ = load_w(wq[j])
            for (t0, n) in own_tiles:
                b = mm16(s_, lambda kc: h2T[:, kc, t0:t0 + n], n, ['h2T'])
                S.op('act', lambda e: e.activation(out=qpT[:, j, t0:t0 + n], in_=pb[b][:, 0:n], func=AF.Copy), reads=[pk(b)], writes=['qpT'])
        kT12 = sb(ph, "kT12", [128, 16, 128], BF16)
        ktmp = [sb(ph, "ktmp%d" % i, [128, 128], F32) for i in range(2)]
        for i in range(16):
            src = (pk1 if i < 8 else pk2)[i % 8]
            s_ = i % 2
            S.dma('sp', 'ktmp%d' % s_, lambda e: e.dma_start(out=ktmp[s_][:], in_=src), writes=['ktmp%d' % s_])
            b = nb()
            S.op('pe', lambda e: e.transpose(out=pb[b][:, 0:128], in_=ktmp[s_][:], identity=ident), reads=['ktmp%d' % s_, 'cs'], writes=[pk(b)])
            S.op('act', lambda e: e.activation(out=kT12[:, i, :], in_=pb[b][:, 0:128], func=AF.Copy), reads=[pk(b)], writes=['kT12'])

        h2tm = sb(ph, "h2tm", [128, D], BF16)
        gsl = [sb(ph, "gsl%d" % i, [128, D], BF16) for i in range(10)]
        ysb = sb(ph, "ysb", [128, D], F32)
        x1tile = sb(ph, "x1tile", [128, 16, 128], F32)
        xnt = sb(ph, "xnt", [128, 16, 128], F32)
        sq3 = sb(ph, "sq3", [128, 16, 128], BF16)
        junkb2 = [sb(ph, "junkb%d" % i, [128, D], BF16) for i in range(2)]
        v12 = sb(ph, "v12", [128, 32], F32)
        i12 = sb(ph, "i12", [128, 32], U32)
        i12f = sb(ph, "i12f", [128, 32], F32)
        scr = sb(ph, "scr", [128, 128], F32)
        cand = sb(ph, "cand", [128, 256], F32)
        cidx = sb(ph, "cidx", [128, 256], F32)
        scr2 = sb(ph, "scr2", [128, 256], F32)
        junk = sb(ph, "junk", [128, 256], F32)
        best = sb(ph, "best", [128, 16], F32)
        e16 = sb(ph, "e16", [128, 16], F32)
        sml = sb(ph, "sml", [128, 4], F32)
        idxf = sb(ph, "idxf", [128, 128], F32)
        idxi = sb(ph, "idxi", [128, 128], I32)
        wgt = sb(ph, "wgt", [128, 128], F32)
        pre = sb(ph, "pre", [128, 128], F32)
        gtmp = sb(ph, "gtmp", [128, 3, 128], F32)
        coef = sb(ph, "coef", [128, 128], F32)
        dgs = [sb(ph, "dgs%d" % i, [128, 128], BF16) for i in range(2)]
        bank_pool[0] = [4, 5, 6, 7]
        yb = [0, 1, 2, 3]
        gctr = [0]

        for tt in range(PEER_TILES):
            tok = tt * 128
            for q4 in range(4):
                b = nb()
                for i in range(4):
                    kc = q4 * 4 + i
                    S.op('pe', lambda e: e.transpose(out=pbb(b)[:, i * 128:(i + 1) * 128], in_=h2T[:, kc, tok:tok + 128], identity=identb[:]),
                         reads=['h2T', 'identb'], writes=[pk(b)])
                S.op('act', lambda e: e.activation(out=h2tm[:, q4 * 512:(q4 + 1) * 512], in_=pbb(b)[:, 0:512], func=AF.Copy), reads=[pk(b)], writes=['h2tm'])
            for h in range(8):
                b = nb()
                S.op('pe', lambda e: e.matmul(pb[b][:, 0:128], lhsT=qpT[:, 2 * h, tok:tok + 128], rhs=kT12[:, h, :], start=True, stop=True),
                     reads=['qpT', 'kT12'], writes=[pk(b)])
                S.op('pe', lambda e: e.matmul(pb[b][:, 128:256], lhsT=qpT[:, 2 * h + 1, tok:tok + 128], rhs=kT12[:, 8 + h, :], start=True, stop=True),
                     reads=['qpT', 'kT12'], writes=[pk(b)])
                for (c0, vo) in ((0, 0), (128, 16)):
                    src = pb[b][:, c0:c0 + 128]
                    S.op('dve', lambda e: e.max(out=v12[:, vo:vo + 8], in_=src), reads=[pk(b)], writes=['v12'])
                    S.op('dve', lambda e: e.max_index(out=i12[:, vo:vo + 8], in_max=v12[:, vo:vo + 8], in_values=src), reads=[pk(b), 'v12'], writes=['i12'])
                    S.op('dve', lambda e: e.match_replace(out=scr[:], in_to_replace=v12[:, vo:vo + 8], in_values=src, imm_value=-1e30),
                         reads=[pk(b), 'v12'], writes=['scr'])
                    S.op('dve', lambda e: e.max(out=v12[:, vo + 8:vo + 16], in_=scr[:]), reads=['scr'], writes=['v12'])
                    S.op('dve', lambda e: e.max_index(out=i12[:, vo + 8:vo + 16], in_max=v12[:, vo + 8:vo + 16], in_values=scr[:]),
                         reads=['scr', 'v12'], writes=['i12'])
                S.op('dve', lambda e: e.tensor_copy(out=i12f[:], in_=i12[:]), reads=['i12'], writes=['i12f'])
                S.op('dve', lambda e: e.tensor_tensor(out=r3(cand[:], 16), in0=bc(v12[:, 0:16], 2, [128, 16, 16]), in1=bc(v12[:, 16:32], 1, [128, 16, 16]), op=ALU.add),
                     reads=['v12'], writes=['cand'])
                S.op('dve', lambda e: e.scalar_tensor_tensor(out=r3(cidx[:], 16), in0=bc(i12f[:, 0:16], 2, [128, 16, 16]), scalar=128.0,
                                                             in1=bc(i12f[:, 16:32], 1, [128, 16, 16]), op0=ALU.mult, op1=ALU.add),
                     reads=['i12f'], writes=['cidx'])
                S.op('dve', lambda e: e.max(out=best[:, 0:8], in_=cand[:]), reads=['cand'], writes=['best'])
                S.op('dve', lambda e: e.match_replace(out=scr2[:], in_to_replace=best[:, 0:8], in_values=cand[:], imm_value=-1e30),
                     reads=['cand', 'best'], writes=['scr2'])
                S.op('dve', lambda e: e.max(out=best[:, 8:16], in_=scr2[:]), reads=['scr2'], writes=['best'])
                for k in range(16):
                    S.op('dve', lambda e: e.scalar_tensor_tensor(out=junk[:], in0=cand[:], scalar=best[:, k:k + 1], in1=cidx[:], op0=ALU.is_equal, op1=ALU.mult,
                                                                 accum_out=idxf[:, h * 16 + k:h * 16 + k + 1]),
                         reads=['cand', 'best', 'cidx'], writes=['junk', 'idxf'])
                S.op('dve', lambda e: e.tensor_scalar(out=sml[:, 0:1], in0=best[:, 0:1], scalar1=-1.0, scalar2=None, op0=ALU.mult), reads=['best'], writes=['sml'])
                S.op('act', lambda e: e.activation(out=e16[:], in_=best[:], func=AF.Exp, bias=sml[:, 0:1], scale=1.0, accum_out=sml[:, 1:2]),
                     reads=['best', 'sml'], writes=['e16', 'sml'])
                S.op('dve', lambda e: e.reciprocal(out=sml[:, 2:3], in_=sml[:, 1:2]), reads=['sml'], writes=['sml'])
                S.op('dve', lambda e: e.tensor_scalar(out=wgt[:, h * 16:(h + 1) * 16], in0=e16[:], scalar1=sml[:, 2:3], scalar2=None, op0=ALU.mult),
                     reads=['e16', 'sml'], writes=['wgt'])
            S.op('dve', lambda e: e.tensor_scalar(out=idxf[:], in0=idxf[:], scalar1=float(PUR - 1), scalar2=0.0, op0=ALU.min, op1=ALU.max),
                 reads=['idxf'], writes=['idxf'])
            S.op('dve', lambda e: e.tensor_copy(out=idxi[:], in_=idxf[:]), reads=['idxf'], writes=['idxi'])
            for s in range(128):
                g_ = gctr[0] % 10
                gctr[0] += 1
                S.dma('pool', 'gsl%d' % g_, lambda e: e.indirect_dma_start(out=gsl[g_][:], out_offset=None, in_=PUB,
                      in_offset=bass.IndirectOffsetOnAxis(ap=idxi[:, s:s + 1], axis=0)), reads=['idxi'], writes=['gsl%d' % g_])
                jb_ = s % 2
                S.op('dve', lambda e: e.tensor_tensor(out=junkb2[jb_][:], in0=gsl[g_][:], in1=h2tm[:], op=ALU.mult),
                     reads=['gsl%d' % g_, 'h2tm'], writes=['junkb%d' % jb_])
                S.op('act', lambda e: e.activation(out=junkb2[jb_][:], in_=junkb2[jb_][:], func=AF.Copy, accum_out=pre[:, s:s + 1]),
                     reads=['junkb%d' % jb_], writes=['junkb%d' % jb_, 'pre%d' % (s % 8)])
            S.op('dve', lambda e: e.tensor_tensor(out=gtmp[:, 0, :], in0=pre[:], in1=pre[:], op=ALU.mult), reads=['pre%d' % i for i in range(8)], writes=['gtmp'])
            S.op('dve', lambda e: e.tensor_scalar(out=gtmp[:, 0, :], in0=gtmp[:, 0, :], scalar1=0.044715, scalar2=1.0, op0=ALU.mult, op1=ALU.add),
                 reads=['gtmp'], writes=['gtmp'])
            S.op('dve', lambda e: e.tensor_tensor(out=gtmp[:, 1, :], in0=gtmp[:, 0, :], in1=pre[:], op=ALU.mult), reads=['gtmp'] + ['pre%d' % i for i in range(8)], writes=['gtmp'])
            S.op('act', lambda e: e.activation(out=gtmp[:, 2, :], in_=gtmp[:, 1, :], func=AF.Tanh, scale=0.7978845608028654), reads=['gtmp'], writes=['gtmp'])
            S.op('dve', lambda e: e.scalar_tensor_tensor(out=gtmp[:, 0, :], in0=gtmp[:, 2, :], scalar=1.0, in1=pre[:], op0=ALU.add, op1=ALU.mult),
                 reads=['gtmp'] + ['pre%d' % i for i in range(8)], writes=['gtmp'])
            S.op('dve', lambda e: e.scalar_tensor_tensor(out=coef[:], in0=gtmp[:, 0, :], scalar=0.5, in1=wgt[:], op0=ALU.mult, op1=ALU.mult),
                 reads=['gtmp', 'wgt'], writes=['coef'])
            for s in range(128):
                g_ = gctr[0] % 10
                gctr[0] += 1
                S.dma('pool', 'gsl%d' % g_, lambda e: e.indirect_dma_start(out=gsl[g_][:], out_offset=None, in_=PVB,
                      in_offset=bass.IndirectOffsetOnAxis(ap=idxi[:, s:s + 1], axis=0)), reads=['idxi'], writes=['gsl%d' % g_])
                d_ = s % 2
                S.op('dve', lambda e: e.tensor_scalar(out=dgs[d_][:], in0=identb[:], scalar1=coef[:, s:s + 1], scalar2=None, op0=ALU.mult),
                     reads=['identb', 'coef'], writes=['dgs%d' % d_])
                for n4 in range(4):
                    S.op('pe', lambda e: e.matmul(pb[yb[n4]][:, :], lhsT=dgs[d_][:], rhs=gsl[g_][:, n4 * 512:(n4 + 1) * 512], start=(s == 0), stop=(s == 127)),
                         reads=['dgs%d' % d_, 'gsl%d' % g_], writes=[pk(yb[n4])])
            for n4 in range(4):
                S.op('act', lambda e: e.activation(out=ysb[:, n4 * 512:(n4 + 1) * 512], in_=pb[yb[n4]][:, :], func=AF.Copy), reads=[pk(yb[n4])], writes=['ysb'])
            S.dma('sp', 'x1tile', lambda e: e.dma_start(out=x1tile[:], in_=X1.rearrange("c p t -> p c t")[:, :, tok:tok + 128]), writes=['x1tile'])
            for q4 in range(4):
                b = nb()
                for i in range(4):
                    kc = q4 * 4 + i
                    S.op('pe', lambda e: e.transpose(out=pb[b][:, i * 128:(i + 1) * 128], in_=ysb[:, kc * 128:(kc + 1) * 128], identity=ident),
                         reads=['ysb', 'cs'], writes=[pk(b)])
                for i in range(4):
                    kc = q4 * 4 + i
                    S.op('dve', lambda e: e.scalar_tensor_tensor(out=x1tile[:, kc, :], in0=pb[b][:, i * 128:(i + 1) * 128], scalar=mdc('G5', kc), in1=x1tile[:, kc, :],
                                                                 op0=ALU.mult, op1=ALU.add), reads=[pk(b), 'md', 'x1tile'], writes=['x1tile'])
            fm_norm(ph, x1tile, 128, lambda kc: pcol('gf', kc), None, xnt, 'xnt', 'x1tile', sq3, 'sq3')
            for q4 in range(4):
                b = nb()
                for i in range(4):
                    kc = q4 * 4 + i
                    S.op('pe', lambda e: e.transpose(out=pb[b][:, i * 128:(i + 1) * 128], in_=xnt[:, kc, :], identity=ident), reads=['xnt', 'cs'], writes=[pk(b)])
                S.op('act', lambda e: e.activation(out=ysb[:, q4 * 512:(q4 + 1) * 512], in_=pb[b][:, :], func=AF.Copy), reads=[pk(b)], writes=['ysb'])
            S.dma('sp', 'outst', lambda e: e.dma_start(out=out[tok:tok + 128, :], in_=ysb[:]), reads=['ysb'], writes=['out'])
        if debug == 6:
            S.dma('sp', 'dbg', lambda e: e.dma_start(out=DBG[:, 0:128], in_=idxf[:]), reads=['idxf'], writes=['DBG'])
            S.dma('sp', 'dbg', lambda e: e.dma_start(out=DBG[:, 128:256], in_=wgt[:]), reads=['wgt'], writes=['DBG'])
            S.dma('sp', 'dbg', lambda e: e.dma_start(out=DBG[:, 256:384], in_=pre[:]), reads=['pre'], writes=['DBG'])
            S.dma('sp', 'dbg', lambda e: e.dma_start(out=DBG[:, 384:512], in_=coef[:]), reads=['coef'], writes=['DBG'])
        S.barrier()
    S.barrier()
    es.close()
    return nc, dbg_outs


def _prep_core(inp, core):
    b, half = core // 2, core % 2
    f = np.ascontiguousarray
    x = inp['x'][b]
    ctx = inp['ctx'][b]
    if half == 0:
        xo, xp, xh, cxx = x[0:NT], x[2047:1023:-1], x[NT:NT + 2], ctx
        p1, p2 = 0, 1
    else:
        xo, xp, xh, cxx = x[2047:1023:-1], x[0:NT], x[1023:1021:-1], ctx[::-1]
        p1, p2 = 1, 0

    def colform(v):
        return f(v.reshape(-1, 128).T)

    ccol = np.stack([colform(inp['c'][b]), colform(inp['c_ctx'])], axis=-1)
    wm = inp['w_mod'][0].reshape(16, 128, 48, 256).transpose(2, 1, 0, 3)
    w_in = inp['w_in'][0]
    ab = w_in[:, 12288:12352]
    abp = np.zeros((D, 128), np.float32)
    for gi, d in enumerate((p1, p2)):
        abp[:, gi * 64:gi * 64 + 16] = ab[:, 32 * d:32 * d + 16]
        abp[:, gi * 64 + 32:gi * 64 + 48] = ab[:, 32 * d + 16:32 * d + 32]
    wsel = np.concatenate([w_in[:, 0:12288], abp, w_in[:, 12352:16448]], axis=1)

    def chunked(w):
        n = w.shape[1] // 128
        return f(w.reshape(16, 128, n, 128).transpose(2, 1, 0, 3))

    cw = inp['conv_w'][0]
    sw = inp['dn_short_w'][0]
    if half == 1:
        cw = cw[::-1]
        sw = sw[::-1]
    convw = f(cw.reshape(31, 16, 128).transpose(2, 1, 0))
    shw = f(sw.reshape(5, 48, 128).transpose(2, 1, 0))
    shwf = f(sw[::-1].reshape(5, 48, 128).transpose(2, 1, 0))
    gpar = np.zeros((128, 2), np.float32)
    for gi, d in enumerate((p1, p2)):
        gpar[gi * 64:gi * 64 + 16, 0] = inp['dn_a_log'][0, d]
        gpar[gi * 64:gi * 64 + 16, 1] = inp['dn_dt_bias'][0, d]
    cst = np.zeros((128, 1024), np.float32)
    cst[:, 0:128] = np.eye(128)
    cst[:, 128:256] = 1.0
    i_ = np.arange(64)
    cst[0:64, 256:320] = (i_[:, None] <= i_[None, :])
    cst[0:64, 320:384] = -1.0
    cst[0:64, 384:448] = np.where(i_[:, None] > i_[None, :], 0.0, NEG)
    cst[0:64, 448:512] = np.where(i_[:, None] <= i_[None, :], 0.0, NEG)
    m = dict(
        xo=f(xo), xp=f(xp), xh=f(xh), cx=f(cxx), ccol=f(ccol), wmod=f(wm), bmod=colform(inp['b_mod'][0]),
        g1=colform(inp['norm1_g'][0]), g2=colform(inp['norm2_g'][0]), gf=colform(inp['final_g']),
        win=chunked(wsel), convw=convw, convb=colform(inp['conv_b'][0]), lng=colform(inp['conv_ln_g'][0]),
        lnb=colform(inp['conv_ln_b'][0]), wco=chunked(inp['w_conv_out'][0]), wdn=chunked(inp['w_dn_out'][0]),
        wout=chunked(inp['w_out'][0]), wq=chunked(inp['peer_w_q'][0]), shw=shw, shwf=shwf, gpar=gpar,
        dnng=f(inp['dn_norm_g'][0].reshape(128, 1)), pk1=f(inp['peer_key1'][0]), pk2=f(inp['peer_key2'][0]),
        pu=f(inp['peer_u'][0]), pv=f(inp['peer_v'][0]), cst=cst,
    )
    return {k: np.ascontiguousarray(v, dtype=np.float32) for k, v in m.items()}


def kernel(**inputs):
    inp = {k: np.asarray(v) for k, v in inputs.items()}
    nc, _ = build_program(0)
    in_maps = [_prep_core(inp, c) for c in range(8)]
    res = run_bass_kernel_spmd(nc, in_maps, core_ids=list(range(8)))
    outp = np.zeros((4, 2048, D), np.float32)
    for c in range(8):
        b, half = c // 2, c % 2
        o = np.asarray(res.results[c]['out'])
        if half == 0:
            outp[b, 0:NT] = o
        else:
            outp[b, NT:] = o[::-1]
    return outp
```

```python
import os
from contextlib import ExitStack

import numpy as np
import concourse.bass as bass
import concourse.mybir as mybir
from concourse.bass_utils import run_bass_kernel_spmd

F32 = mybir.dt.float32
BF16 = mybir.dt.bfloat16
I32 = mybir.dt.int32
U32 = mybir.dt.uint32
AF = mybir.ActivationFunctionType
ALU = mybir.AluOpType

D = 2048
NT = 1024
NCX = 256
EPS = 1e-6
NEG = -30000.0


class Sched:
    def __init__(self, nc):
        self.nc = nc
        self.engs = {}
        self.state = {}
        self.dma_sems = {}
        self._ctx = []
        for name, obj in [('pe', nc.tensor), ('act', nc.scalar), ('dve', nc.vector),
                          ('pool', nc.gpsimd), ('sp', nc.sync)]:
            cm = nc.semaphore('s_' + name)
            sem = cm.__enter__()
            self._ctx.append(cm)
            self.engs[name] = dict(name=name, obj=obj, sem=sem, count=0, known={})

    def _st(self, k):
        if k not in self.state:
            self.state[k] = dict(w=None, r=[])
        return self.state[k]

    def _wait(self, eng, need):
        for key, (sem, val) in need.items():
            if sem is eng['sem'] and eng['name'] == 'pe':
                continue
            if eng['known'].get(key, 0) < val:
                eng['obj'].wait_ge(sem, val)
                eng['known'][key] = val

    def _deps(self, eng, reads, writes):
        need = {}

        def add(tok):
            if tok is None:
                return
            sem, val = tok
            key = id(sem)
            if key not in need or need[key][1] < val:
                need[key] = (sem, val)
        for k in reads:
            add(self._st(k)['w'])
        for k in writes:
            st = self._st(k)
            add(st['w'])
            for r in st['r']:
                add(r)
        self._wait(eng, need)

    def _mark(self, tok, reads, writes):
        for k in reads:
            st = self._st(k)
            st['r'] = [r for r in st['r'] if r[0] is not tok[0]] + [tok]
        for k in writes:
            st = self._st(k)
            st['w'] = tok
            st['r'] = []

    def op(self, engname, fn, reads=(), writes=()):
        eng = self.engs[engname]
        self._deps(eng, reads, writes)
        inst = fn(eng['obj'])
        eng['count'] += 1
        inst.then_inc(eng['sem'], 1)
        self._mark((eng['sem'], eng['count']), reads, writes)
        return inst

    def dma(self, engname, slot, fn, reads=(), writes=()):
        eng = self.engs[engname]
        self._deps(eng, reads, writes)
        if slot not in self.dma_sems:
            cm = self.nc.semaphore('d_' + slot)
            sem = cm.__enter__()
            self._ctx.append(cm)
            self.dma_sems[slot] = [sem, 0]
        ent = self.dma_sems[slot]
        inst = fn(eng['obj'])
        ent[1] += 16
        inst.then_inc(ent[0], 16)
        self._mark((ent[0], ent[1]), reads, writes)
        return inst

    def barrier(self):
        need = {}
        for e in self.engs.values():
            if e['count'] > 0:
                need[id(e['sem'])] = (e['sem'], e['count'])
        for sem, val in self.dma_sems.values():
            if val > 0:
                need[id(sem)] = (sem, val)
        for e in self.engs.values():
            n2 = {k: v for k, v in need.items() if v[0] is not e['sem']}
            for key, (sem, val) in n2.items():
                if e['known'].get(key, 0) < val:
                    e['obj'].wait_ge(sem, val)
                    e['known'][key] = val
        self.state = {}
        self._gen = getattr(self, '_gen', 0) + 1
        for e in self.engs.values():
            if e['count'] > 2000:
                cm = self.nc.semaphore('s_%s_%d' % (e['name'], self._gen))
                e['sem'] = cm.__enter__()
                self._ctx.append(cm)
                e['count'] = 0


C_GLU_A, C_GLU_G, C_Q, C_K, C_V, C_Z, C_AB, C_BRC, C_BRD = 0, 16, 32, 48, 64, 80, 96, 97, 113
N_WIN_CH = 129


def build_program(debug=0, DN_BLOCKS=4, PEER_TILES=8, dn_stop=99):
    nc = bass.Bass("TRN2", target_bir_lowering=False)

    def di(name, shape, dt=F32):
        if debug == 7 and name != 'cst':
            shape = [1, 2]
        return nc.dram_tensor(name, list(shape), dt, kind="ExternalInput").ap()

    dbg_outs = []

    def scratch(name, shape, dt):
        if debug:
            dbg_outs.append(name)
            return nc.dram_tensor(name, list(shape), dt, kind="ExternalOutput").ap()
        return nc.dram_tensor(name, list(shape), dt, kind="Internal").ap()

    xo = di("xo", [NT, D]); xp = di("xp", [NT, D]); xh = di("xh", [2, D]); cx = di("cx", [NCX, D])
    ccol = di("ccol", [128, 16, 2])
    wmod = di("wmod", [48, 128, 16, 256]); bmod = di("bmod", [128, 96])
    g1 = di("g1", [128, 16]); g2 = di("g2", [128, 16]); gf = di("gf", [128, 16])
    win = di("win", [N_WIN_CH, 128, 16, 128])
    convw = di("convw", [128, 16, 31]); convb = di("convb", [128, 16])
    lng = di("lng", [128, 16]); lnb = di("lnb", [128, 16])
    wco = di("wco", [16, 128, 16, 128]); wdn = di("wdn", [16, 128, 16, 128])
    wout = di("wout", [16, 128, 16, 128]); wq = di("wq", [16, 128, 16, 128])
    shw = di("shw", [128, 48, 5]); shwf = di("shwf", [128, 48, 5])
    gpar = di("gpar", [128, 2])
    dnng = di("dnng", [128, 1])
    pk1 = di("pk1", [8, 128, 128]); pk2 = di("pk2", [8, 128, 128])
    PUR = 16384 if debug in (0, 6) else 128
    pu = di("pu", [PUR, D]); pv = di("pv", [PUR, D])
    cst = di("cst", [128, 1024])
    out = nc.dram_tensor("out", [NT, D], F32, kind="ExternalOutput").ap()

    XT = scratch("XT", [16, 128, NT], F32)
    X1 = scratch("X1", [16, 128, NT], F32)
    UC = scratch("UC", [16, 128, NT], BF16)
    QTo = scratch("QTo", [16, 128, NT], BF16)
    KTo = scratch("KTo", [16, 128, NT], BF16)
    VTo = scratch("VTo", [16, 128, NT], BF16)
    KTp = scratch("KTp", [16, 128, NT], BF16)
    VTp = scratch("VTp", [16, 128, NT], BF16)
    KTc = scratch("KTc", [16, 128, NCX], BF16)
    VTc = scratch("VTc", [16, 128, NCX], BF16)
    ZT = scratch("ZT", [16, 128, NT], BF16)
    BR = scratch("BR", [32, 128, NT], BF16)
    PUB = nc.dram_tensor("PUB", [PUR, D], BF16, kind="Internal").ap()
    PVB = nc.dram_tensor("PVB", [PUR, D], BF16, kind="Internal").ap()
    if debug:
        DBG = scratch("DBG", [128, 4096], F32)
        OTD = scratch("OTD", [128, 16, NT], BF16)

    S = Sched(nc)
    es = ExitStack()

    def sb(stack, name, shape, dt):
        return stack.enter_context(nc.sbuf_tensor(name, list(shape), dt))

    pb = [es.enter_context(nc.psum_tensor("pb%d" % i, [128, 512], F32)) for i in range(8)]
    bank_ctr = [0]
    bank_pool = [list(range(8))]

    def nb():
        pool = bank_pool[0]
        b = pool[bank_ctr[0] % len(pool)]
        bank_ctr[0] += 1
        return b

    def pk(b):
        return 'pb%d' % b

    cs = sb(es, "cs", [128, 1024], F32)
    S.dma('sp', 'cs', lambda e: e.dma_start(out=cs[:], in_=cst), writes=['cs'])
    ident = cs[:, 0:128]
    ones = cs[:, 128:256]
    tri = cs[0:64, 256:320]
    negones = cs[0:64, 320:384]
    nmL = cs[0:64, 384:448]
    nmU = cs[0:64, 448:512]
    eye64 = cs[0:64, 0:64]
    identb = sb(es, "identb", [128, 128], BF16)
    onesb = sb(es, "onesb", [128, 128], BF16)
    S.op('dve', lambda e: e.tensor_copy(out=identb[:], in_=ident), reads=['cs'], writes=['identb'])
    S.op('dve', lambda e: e.tensor_copy(out=onesb[:], in_=ones), reads=['cs'], writes=['onesb'])

    S_dma_prm = S.dma if debug != 7 else (lambda *a, **k: None)
    prm = sb(es, "prm", [128, 16 * 6 + 96 + 31 * 16 + 48 * 10 + 4], F32)
    o_ = [0]

    def prm_alloc(n):
        a = o_[0]
        o_[0] += n
        return a
    pofs = {}
    for nm_, src, n in [('g1', g1, 16), ('g2', g2, 16), ('gf', gf, 16), ('convb', convb, 16),
                        ('lng', lng, 16), ('lnb', lnb, 16), ('bmod', bmod, 96)]:
        a = prm_alloc(n)
        pofs[nm_] = a
        S_dma_prm('sp', 'prm_' + nm_, lambda e, a=a, n=n, src=src: e.dma_start(out=prm[:, a:a + n], in_=src),
              writes=['prm'])
    a = prm_alloc(31 * 16); pofs['convw'] = a
    S_dma_prm('sp', 'prm_convw', lambda e: e.dma_start(out=prm[:, a:a + 496], in_=convw.rearrange("p c k -> p (c k)")), writes=['prm'])
    a2 = prm_alloc(240); pofs['shw'] = a2
    S_dma_prm('sp', 'prm_shw', lambda e: e.dma_start(out=prm[:, a2:a2 + 240], in_=shw.rearrange("p c k -> p (c k)")), writes=['prm'])
    a3 = prm_alloc(240); pofs['shwf'] = a3
    S_dma_prm('sp', 'prm_shwf', lambda e: e.dma_start(out=prm[:, a3:a3 + 240], in_=shwf.rearrange("p c k -> p (c k)")), writes=['prm'])
    a4 = prm_alloc(2); pofs['gpar'] = a4
    S_dma_prm('sp', 'prm_gpar', lambda e: e.dma_start(out=prm[:, a4:a4 + 2], in_=gpar), writes=['prm'])
    a5 = prm_alloc(1); pofs['dnng'] = a5
    S_dma_prm('sp', 'prm_dnng', lambda e: e.dma_start(out=prm[:, a5:a5 + 1], in_=dnng), writes=['prm'])

    def pcol(name, j):
        a = pofs[name] + j
        return prm[:, a:a + 1]

    md = sb(es, "md", [128, 16 * 10 + 4], F32)
    MD = dict(A1=0, B1=16, A1c=32, B1c=48, A2=64, B2=80, G2=96, G5=112, NEA=160)

    def mdc(name, j):
        a = MD[name] + j
        return md[:, a:a + 1]

    wf = [sb(es, "wf%d" % i, [128, 16, 128], F32) for i in range(2)]
    wb = [sb(es, "wb%d" % i, [128, 16, 128], BF16) for i in range(2)]
    wctr = [0]

    cvt_jobs = []
    if debug != 7:
        for src_, dst_ in ((pu, PUB), (pv, PVB)):
            for r0 in range(0, PUR, 128):
                cvt_jobs.append((src_, dst_, r0))
    cvt_state = dict(call=0, bufs=None)

    def cvt_step(n=1):
        if cvt_state['bufs'] is None:
            return
        cvf, cvb = cvt_state['bufs']
        N = len(cvt_jobs)
        for _ in range(n):
            c = cvt_state['call']
            if 2 * c - 4 >= N:
                return
            cvt_state['call'] += 1
            for j in (2 * c, 2 * c + 1):
                if 0 <= j < N:
                    src_, dst_, r0 = cvt_jobs[j]
                    sl = j % 4
                    S.dma('sp', 'cvl%d' % sl, lambda e: e.dma_start(out=cvf[sl][:], in_=src_[r0:r0 + 128, :]), writes=['cvf%d' % sl])
            for j in (2 * c - 2, 2 * c - 1):
                if 0 <= j < N:
                    sl = j % 4
                    S.op('pool', lambda e: e.tensor_copy(out=cvb[sl][:], in_=cvf[sl][:]), reads=['cvf%d' % sl], writes=['cvb%d' % sl])
            for j in (2 * c - 4, 2 * c - 3):
                if 0 <= j < N:
                    src_, dst_, r0 = cvt_jobs[j]
                    sl = j % 4
                    S.dma('sp', 'cvs%d' % sl, lambda e: e.dma_start(out=dst_[r0:r0 + 128, :], in_=cvb[sl][:]), reads=['cvb%d' % sl])

    def load_w(chunk_ap):
        slot = wctr[0] % 2
        wctr[0] += 1
        S.dma('sp', 'wf%d' % slot, lambda e: e.dma_start(out=wf[slot][:], in_=chunk_ap), writes=['wf%d' % slot])
        S.op('pool', lambda e: e.tensor_copy(out=wb[slot][:], in_=wf[slot][:]), reads=['wf%d' % slot], writes=['wb%d' % slot])
        return slot

    def mm16(slot, actfn, N, actkeys):
        b = nb()
        for kc in range(16):
            S.op('pe', lambda e: e.matmul(pb[b][:, 0:N], lhsT=wb[slot][:, kc, :], rhs=actfn(kc),
                                          start=(kc == 0), stop=(kc == 15)),
                 reads=['wb%d' % slot] + list(actkeys), writes=[pk(b)])
        return b

    gT = sb(es, "gT", [128, 2304], F32)

    for ph in ([ExitStack()] if debug != 7 else []):
        cc = sb(ph, "cc", [128, 16, 2], F32)
        sc = sb(ph, "sc", [128, 16, 2], F32)
        wm = [sb(ph, "wm%d" % i, [128, 16, 256], F32) for i in range(2)]
        modT = sb(ph, "modT", [128, 96, 2], F32)
        S.dma('sp', 'cc', lambda e: e.dma_start(out=cc[:], in_=ccol), writes=['cc'])
        S.op('act', lambda e: e.activation(out=sc[:], in_=cc[:], func=AF.Silu), reads=['cc'], writes=['sc'])
        bm = nb()
        for blk in range(48):
            s_ = blk % 2
            S.dma('sp' if s_ == 0 else 'act', 'wm%d' % s_, lambda e: e.dma_start(out=wm[s_][:], in_=wmod[blk]), writes=['wm%d' % s_])
            for c2 in range(2):
                ci = blk * 2 + c2
                for kc in range(16):
                    S.op('pe', lambda e: e.matmul(pb[bm][:, ci * 2:ci * 2 + 2], lhsT=wm[s_][:, kc, c2 * 128:(c2 + 1) * 128],
                                                  rhs=sc[:, kc, :], start=(kc == 0), stop=(kc == 15)),
                         reads=['wm%d' % s_, 'sc'], writes=[pk(bm)])
        bmo = pofs['bmod']
        S.op('dve', lambda e: e.tensor_tensor(out=modT[:], in0=pb[bm][:, 0:192].rearrange("p (c t) -> p c t", t=2),
                                              in1=prm[:, bmo:bmo + 96].unsqueeze(2).to_broadcast([128, 96, 2]), op=ALU.add),
             reads=[pk(bm), 'prm'], writes=['modT'])
        for (An, Bn, gname, msc, msh, col) in [('A1', 'B1', 'g1', 1, 0, 0), ('A1c', 'B1c', 'g1', 1, 0, 1), ('A2', 'B2', 'g2', 4, 3, 0)]:
            ga = pofs[gname]
            S.op('dve', lambda e: e.scalar_tensor_tensor(out=md[:, MD[An]:MD[An] + 16], in0=modT[:, msc * 16:(msc + 1) * 16, col],
                                                         scalar=1.0, in1=prm[:, ga:ga + 16], op0=ALU.add, op1=ALU.mult),
                 reads=['modT', 'prm'], writes=['md'])
            S.op('dve', lambda e: e.tensor_copy(out=md[:, MD[Bn]:MD[Bn] + 16], in_=modT[:, msh * 16:(msh + 1) * 16, col]),
                 reads=['modT'], writes=['md'])
        S.op('dve', lambda e: e.tensor_copy(out=md[:, MD['G2']:MD['G2'] + 16], in_=modT[:, 32:48, 0]), reads=['modT'], writes=['md'])
        S.op('dve', lambda e: e.tensor_copy(out=md[:, MD['G5']:MD['G5'] + 16], in_=modT[:, 80:96, 0]), reads=['modT'], writes=['md'])
        gp = pofs['gpar']
        S.op('act', lambda e: e.activation(out=md[:, 160:161], in_=prm[:, gp:gp + 1], func=AF.Exp), reads=['prm'], writes=['md'])
        S.op('dve', lambda e: e.tensor_scalar(out=md[:, 160:161], in0=md[:, 160:161], scalar1=-1.0, scalar2=None, op0=ALU.mult),
             reads=['md'], writes=['md'])
        if debug == 1:
            S.dma('sp', 'dbg', lambda e: e.dma_start(out=DBG[:, 0:164], in_=md[:]), reads=['md'], writes=['DBG'])
        S.barrier()
        ph.close()

    def fm_norm(ph, srcT, N, Acol, Bcol, dst, dstkey, srckey, tmp, tmpkey, out_f32=False):
        b = nb()
        for kc in range(16):
            S.op('act', lambda e: e.activation(out=tmp[:, kc, 0:N], in_=srcT[:, kc, 0:N], func=AF.Square),
                 reads=[srckey], writes=[tmpkey + str(kc)])
            S.op('pe', lambda e: e.matmul(pb[b][:, 0:N], lhsT=onesb[:], rhs=tmp[:, kc, 0:N], start=(kc == 0), stop=(kc == 15)),
                 reads=[tmpkey + str(kc), 'onesb'], writes=[pk(b)])
        rs = fm_rs
        S.op('act', lambda e: e.activation(out=rs[:, 0:N], in_=pb[b][:, 0:N], func=AF.Sqrt, scale=1.0 / D, bias=epsc[:, 0:1]),
             reads=[pk(b), 'epsc'], writes=['fm_rs'])
        S.op('dve', lambda e: e.reciprocal(out=rs[:, 0:N], in_=rs[:, 0:N]), reads=['fm_rs'], writes=['fm_rs'])
        for kc in range(16):
            S.op('dve', lambda e: e.tensor_tensor(out=fm_t[:, 0:N], in0=srcT[:, kc, 0:N], in1=rs[:, 0:N], op=ALU.mult),
                 reads=[srckey, 'fm_rs'], writes=['fm_t'])
            if Bcol is not None:
                S.op('act', lambda e: e.activation(out=dst[:, kc, 0:N], in_=fm_t[:, 0:N], func=AF.Identity,
                                                   scale=Acol(kc), bias=Bcol(kc)),
                     reads=['fm_t', 'md', 'prm'], writes=[dstkey])
            else:
                S.op('act', lambda e: e.activation(out=dst[:, kc, 0:N], in_=fm_t[:, 0:N], func=AF.Identity, scale=Acol(kc), bias=epsc[:, 2:3]),
                     reads=['fm_t', 'md', 'prm', 'epsc'], writes=[dstkey])

    fm_rs = sb(es, "fm_rs", [128, 512], F32)
    fm_t = sb(es, "fm_t", [128, 512], F32)
    epsc = sb(es, "epsc", [128, 4], F32)
    S.op('dve', lambda e: e.memset(epsc[:, 0:1], EPS), writes=['epsc'])
    S.op('dve', lambda e: e.memset(epsc[:, 1:2], 1.0), writes=['epsc'])
    S.op('dve', lambda e: e.memset(epsc[:, 2:3], 0.0), writes=['epsc'])

    for ph in ([ExitStack()] if debug != 7 else []):
        hTo = sb(ph, "hTo", [128, 16, NT + 2], BF16)
        hTp = sb(ph, "hTp", [128, 16, NT], BF16)
        hTc = sb(ph, "hTc", [128, 16, NCX], BF16)
        phB = ExitStack()
        xtm = [sb(phB, "xtm%d" % i, [128, D], F32) for i in range(2)]
        xTt = sb(phB, "xTt", [128, 16, 512], F32)
        sqt = sb(phB, "sqt", [128, 16, 512], BF16)

        class _V:
            def __init__(self, t, o):
                self.t, self.o = t, o

            def __getitem__(self, key):
                p, kc, sl = key
                return self.t[p, kc, self.o + sl.start:self.o + sl.stop]

        def stageB2(src, ntok, dstT, dstkey, off, Aname, Bname, save_xt):
            t0 = 0
            while t0 < ntok:
                n = min(512, ntok - t0)
                for s0 in range(0, n, 128):
                    m = min(128, n - s0)
                    sl = ((t0 + s0) // 128) % 2
                    S.dma('sp', 'xtm%d' % sl, lambda e: e.dma_start(out=xtm[sl][0:m, :], in_=src[t0 + s0:t0 + s0 + m, :]),
                          writes=['xtm%d' % sl])
                    for q4 in range(4):
                        b = nb()
                        for i in range(4):
                            kc = q4 * 4 + i
                            S.op('pe', lambda e: e.transpose(out=pb[b][:, i * 128:i * 128 + m], in_=xtm[sl][0:m, kc * 128:(kc + 1) * 128],
                                                             identity=ident[0:m, 0:m]),
                                 reads=['xtm%d' % sl, 'cs'], writes=[pk(b)])
                        S.op('act', lambda e: e.activation(out=xTt[:, q4 * 4:q4 * 4 + 4, s0:s0 + m],
                                                           in_=pb[b][:, :].rearrange("p (i t) -> p i t", t=128)[:, :, 0:m], func=AF.Copy),
                             reads=[pk(b)], writes=['xTt'])
                if save_xt:
                    S.dma('sp', 'XTst', lambda e: e.dma_start(out=XT.rearrange("c p t -> p c t")[:, :, t0:t0 + n], in_=xTt[:, :, 0:n]),
                          reads=['xTt'], writes=['XT'])
                fm_norm(ph, xTt, n, lambda kc: mdc(Aname, kc), lambda kc: mdc(Bname, kc),
                        _V(dstT, off + t0), dstkey, 'xTt', sqt, 'sqt')
                t0 += n

        stageB2(xo, NT, hTo, 'hTo', 0, 'A1', 'B1', True)
        stageB2(xh, 2, hTo, 'hTo', NT, 'A1', 'B1', False)
        stageB2(xp, NT, hTp, 'hTp', 0, 'A1', 'B1', False)
        stageB2(cx, NCX, hTc, 'hTc', 0, 'A1c', 'B1c', False)
        if debug == 2:
            S.barrier()
            S.op('act', lambda e: e.activation(out=xTt[:, :, 0:64], in_=hTo[:, :, 0:64], func=AF.Copy), reads=['hTo'], writes=['xTt'])
            S.dma('sp', 'dbg', lambda e: e.dma_start(out=DBG[:, 0:1024], in_=xTt[:, :, 0:64]), reads=['xTt'], writes=['DBG'])

        S.barrier()
        phB.close()
        if cvt_jobs:
            cvt_state['bufs'] = ([sb(ph, "cvf%d" % i, [128, D], F32) for i in range(4)],
                                 [sb(ph, "cvb%d" % i, [128, D], BF16) for i in range(4)])
        upad = sb(ph, "upad", [128, 16, 94], BF16)
        S.op('dve', lambda e: e.memset(upad[:], 0.0), writes=['upad'])
        dg = sb(ph, "dg", [128, 31, 128], BF16)
        dg5 = sb(ph, "dg5", [128, 5, 128], BF16)
        dg5f = sb(ph, "dg5f", [128, 5, 128], BF16)
        po = sb(ph, "po", [128, NT + 6], BF16)
        pp = sb(ph, "pp", [128, NT + 4], BF16)
        pc = sb(ph, "pc", [128, NCX + 4], BF16)
        for t_, k_ in [(po, 'po'), (pp, 'pp'), (pc, 'pc')]:
            S.op('dve', lambda e: e.memset(t_[:], 0.0), writes=[k_])
        sig = sb(ph, "sig", [128, 512], F32)
        stg = [sb(ph, "stg%d" % i, [128, NT], BF16) for i in range(2)]
        stgc = [0]
        sl5s = [sb(ph, "sl5_%d" % i, [128, 512], F32) for i in range(2)]
        sq5s = [sb(ph, "sq5_%d" % i, [128, 512], BF16) for i in range(2)]
        rs5s = [sb(ph, "rs5_%d" % i, [128, 512], F32) for i in range(2)]
        c5ctr = [0]

        own_tiles = [(0, 512), (512, 512)]

        def act_o(t0, n):
            return lambda kc: hTo[:, kc, t0:t0 + n]

        def act_p(t0, n):
            return lambda kc: hTp[:, kc, t0:t0 + n]

        def act_c(t0, n):
            return lambda kc: hTc[:, kc, t0:t0 + n]

        def next_stg():
            i = stgc[0] % 2
            stgc[0] += 1
            return i

        for j in range(16):
            sa = load_w(win[C_GLU_A + j])
            bas = [mm16(sa, act_o(t0, n), n, ['hTo']) for (t0, n) in own_tiles]
            sg = load_w(win[C_GLU_G + j])
            for ti, (t0, n) in enumerate(own_tiles):
                bg = mm16(sg, act_o(t0, n), n, ['hTo'])
                S.op('act', lambda e: e.activation(out=sig[:, 0:n], in_=pb[bg][:, 0:n], func=AF.Sigmoid), reads=[pk(bg)], writes=['sig'])
                S.op('dve', lambda e: e.tensor_tensor(out=upad[:, 8 * ti:8 * ti + 8, 15:79],
                                                      in0=pb[bas[ti]][:, 0:512].rearrange("p (r c) -> p r c", c=64),
                                                      in1=sig[:, 0:512].rearrange("p (r c) -> p r c", c=64), op=ALU.mult),
                     reads=[pk(bas[ti]), 'sig'], writes=['upad'])
            cw = pofs['convw'] + j * 31
            S.op('dve', lambda e: e.tensor_tensor(out=dg[:], in0=identb[:].unsqueeze(1).to_broadcast([128, 31, 128]),
                                                  in1=prm[:, cw:cw + 31].unsqueeze(2).to_broadcast([128, 31, 128]), op=ALU.mult),
                 reads=['identb', 'prm'], writes=['dg'])
            si = next_stg()
            for hf in range(2):
                b = nb()
                for k in range(31):
                    S.op('pe', lambda e: e.matmul(pb[b][:, :], lhsT=dg[:, k, :], rhs=upad[:, 8 * hf:8 * hf + 8, k:k + 64],
                                                  start=(k == 0), stop=(k == 30)),
                         reads=['dg', 'upad'], writes=[pk(b)])
                S.op('act', lambda e: e.activation(out=stg[si][:, hf * 512:(hf + 1) * 512], in_=pb[b][:, :], func=AF.Identity,
                                                   bias=pcol('convb', j), scale=1.0),
                     reads=[pk(b), 'prm'], writes=['stg%d' % si])
            S.dma('sp', 'stgo%d' % si, lambda e: e.dma_start(out=UC[j], in_=stg[si][:]), reads=['stg%d' % si])

        def conv5_store(kind, j, pad, padkey, ntok, taps, dst, dstj):
            si = next_stg()
            t0 = 0
            while t0 < ntok:
                n = min(512, ntok - t0)
                pz = c5ctr[0] % 2
                c5ctr[0] += 1
                sl5, sq5, rs5 = sl5s[pz], sq5s[pz], rs5s[pz]
                b = nb()
                for k in range(5):
                    S.op('pe', lambda e: e.matmul(pb[b][:, 0:n], lhsT=taps[:, k, :], rhs=pad[:, t0 + k:t0 + k + n],
                                                  start=(k == 0), stop=(k == 4)),
                         reads=[padkey, 'dg5', 'dg5f'], writes=[pk(b)])
                if kind == 'v':
                    S.op('act', lambda e: e.activation(out=stg[si][:, t0:t0 + n], in_=pb[b][:, 0:n], func=AF.Silu),
                         reads=[pk(b)], writes=['stg%d' % si])
                else:
                    S.op('act', lambda e: e.activation(out=sl5[:, 0:n], in_=pb[b][:, 0:n], func=AF.Silu), reads=[pk(b)], writes=['sl5_%d' % pz])
                    S.op('act', lambda e: e.activation(out=sq5[:, 0:n], in_=sl5[:, 0:n], func=AF.Square), reads=['sl5_%d' % pz], writes=['sq5_%d' % pz])
                    b2 = nb()
                    S.op('pe', lambda e: e.matmul(pb[b2][:, 0:n], lhsT=onesb[:], rhs=sq5[:, 0:n], start=True, stop=True),
                         reads=['sq5_%d' % pz, 'onesb'], writes=[pk(b2)])
                    S.op('act', lambda e: e.activation(out=rs5[:, 0:n], in_=pb[b2][:, 0:n], func=AF.Sqrt, scale=1.0, bias=epsc[:, 0:1]),
                         reads=[pk(b2), 'epsc'], writes=['rs5_%d' % pz])
                    S.op('dve', lambda e: e.reciprocal(out=rs5[:, 0:n], in_=rs5[:, 0:n]), reads=['rs5_%d' % pz], writes=['rs5_%d' % pz])
                    sc_ = (128.0 ** -0.5) if kind == 'q' else 1.0
                    S.op('dve', lambda e: e.scalar_tensor_tensor(out=stg[si][:, t0:t0 + n], in0=sl5[:, 0:n], scalar=sc_, in1=rs5[:, 0:n],
                                                                 op0=ALU.mult, op1=ALU.mult),
                         reads=['sl5_%d' % pz, 'rs5_%d' % pz], writes=['stg%d' % si])
                t0 += n
            S.dma('sp', 'stgo%d' % si, lambda e: e.dma_start(out=dst[dstj][:, 0:ntok], in_=stg[si][:, 0:ntok]),
                  reads=['stg%d' % si])

        def build_taps(idx):
            a_ = pofs['shw'] + idx * 5
            f_ = pofs['shwf'] + idx * 5
            for (t_, o_k, key_) in ((dg5, a_, 'dg5'), (dg5f, f_, 'dg5f')):
                S.op('dve', lambda e: e.tensor_tensor(out=t_[:], in0=identb[:].unsqueeze(1).to_broadcast([128, 5, 128]),
                                                      in1=prm[:, o_k:o_k + 5].unsqueeze(2).to_broadcast([128, 5, 128]), op=ALU.mult),
                     reads=['identb', 'prm'], writes=[key_])

        for j in range(16):
            for kind, cbase, kidx in [('q', C_Q, 0), ('k', C_K, 16), ('v', C_V, 32)]:
                s_ = load_w(win[cbase + j])
                cvt_step(3)
                build_taps(kidx + j)
                for (t0, n) in own_tiles + [(NT, 2)]:
                    b = mm16(s_, act_o(t0, n), n, ['hTo'])
                    S.op('act', lambda e: e.activation(out=po[:, 2 + t0:2 + t0 + n], in_=pb[b][:, 0:n], func=AF.Copy), reads=[pk(b)], writes=['po'])
                if kind != 'q':
                    for (t0, n) in own_tiles:
                        b = mm16(s_, act_p(t0, n), n, ['hTp'])
                        S.op('act', lambda e: e.activation(out=pp[:, 2 + t0:2 + t0 + n], in_=pb[b][:, 0:n], func=AF.Copy), reads=[pk(b)], writes=['pp'])
                    S.op('act', lambda e: e.activation(out=pp[:, 2 + NT:3 + NT], in_=po[:, 2 + NT - 1:2 + NT], func=AF.Copy), reads=['po'], writes=['pp'])
                    S.op('act', lambda e: e.activation(out=pp[:, 3 + NT:4 + NT], in_=po[:, 2 + NT - 2:2 + NT - 1], func=AF.Copy), reads=['po'], writes=['pp'])
                    b = mm16(s_, act_c(0, NCX), NCX, ['hTc'])
                    S.op('act', lambda e: e.activation(out=pc[:, 2:2 + NCX], in_=pb[b][:, 0:NCX], func=AF.Copy), reads=[pk(b)], writes=['pc'])
                conv5_store(kind, j, po, 'po', NT, dg5, {'q': QTo, 'k': KTo, 'v': VTo}[kind], j)
                if kind != 'q':
                    conv5_store(kind, j, pp, 'pp', NT, dg5f, {'k': KTp, 'v': VTp}[kind], j)
                    conv5_store(kind, j, pc, 'pc', NCX, dg5, {'k': KTc, 'v': VTc}[kind], j)

        for j in range(16):
            s_ = load_w(win[C_Z + j])
            si = next_stg()
            for (t0, n) in own_tiles:
                b = mm16(s_, act_o(t0, n), n, ['hTo'])
                S.op('act', lambda e: e.activation(out=stg[si][:, t0:t0 + n], in_=pb[b][:, 0:n], func=AF.Silu), reads=[pk(b)], writes=['stg%d' % si])
            S.dma('sp', 'stgo%d' % si, lambda e: e.dma_start(out=ZT[j], in_=stg[si][:]), reads=['stg%d' % si])

        s_ = load_w(win[C_AB])
        gp = pofs['gpar']
        for (actf, keys, t0, n, goff) in ([(act_o(t0, n), ['hTo'], t0, n, t0) for (t0, n) in own_tiles] +
                                          [(act_p(t0, n), ['hTp'], t0, n, NT + t0) for (t0, n) in own_tiles] +
                                          [(act_c(0, NCX), ['hTc'], 0, NCX, 2 * NT)]):
            b = mm16(s_, actf, n, keys)
            for g0 in (0, 64):
                S.op('act', lambda e: e.activation(out=sig[g0:g0 + 32, 0:n], in_=pb[b][g0:g0 + 32, 0:n], func=AF.Exp,
                                                   bias=prm[g0:g0 + 32, gp + 1:gp + 2], scale=1.0), reads=[pk(b), 'prm'], writes=['sig'])
                S.op('act', lambda e: e.activation(out=sig[g0:g0 + 32, 0:n], in_=sig[g0:g0 + 32, 0:n], func=AF.Ln,
                                                   bias=epsc[g0:g0 + 32, 1:2], scale=1.0), reads=['sig', 'epsc'], writes=['sig'])
                S.op('dve', lambda e: e.tensor_scalar(out=gT[g0:g0 + 32, goff:goff + n], in0=sig[g0:g0 + 32, 0:n],
                                                      scalar1=md[g0:g0 + 32, 160:161], scalar2=None, op0=ALU.mult),
                     reads=['sig', 'md'], writes=['gT'])
                S.op('act', lambda e: e.activation(out=gT[g0 + 32:g0 + 64, goff:goff + n], in_=pb[b][g0 + 32:g0 + 64, 0:n], func=AF.Sigmoid),
                     reads=[pk(b)], writes=['gT'])

        for j in range(32):
            s_ = load_w(win[C_BRC + j])
            si = next_stg()
            for (t0, n) in own_tiles:
                b = mm16(s_, act_o(t0, n), n, ['hTo'])
                S.op('act', lambda e: e.activation(out=stg[si][:, t0:t0 + n], in_=pb[b][:, 0:n], func=AF.Sigmoid), reads=[pk(b)], writes=['stg%d' % si])
            S.dma('sp', 'stgo%d' % si, lambda e: e.dma_start(out=BR[j], in_=stg[si][:]), reads=['stg%d' % si])
        if debug == 3:
            S.dma('sp', 'dbg', lambda e: e.dma_start(out=DBG[:, 0:2304], in_=gT[:]), reads=['gT'], writes=['DBG'])
        cvt_step(10 ** 6)
        cvt_state['bufs'] = None
        S.barrier()
        ph.close()

    if debug in (1, 2, 3):
        S.barrier()
        es.close()
        return nc, dbg_outs

    def r3(ap, inner):
        return ap.rearrange("p (a b) -> p a b", b=inner)

    def pbb(b):
        return pb[b][:, :].bitcast(BF16)

    phDE = ExitStack()
    OT = sb(phDE, "OT", [128, 16, NT], BF16)

    with ExitStack() as ph:
        Sf = sb(ph, "Sf", [128, 16, 128], F32)
        Sb = sb(ph, "Sb", [128, 16, 128], BF16)
        kblk = sb(ph, "kblk", [128, 16, 256], BF16)
        vblk = sb(ph, "vblk", [128, 16, 256], BF16)
        qblk = sb(ph, "qblk", [128, 16, 256], BF16)
        gtm = sb(ph, "gtm", [64, 128], F32)
        sm = sb(ph, "sm", [64, 96], F32)
        egl = sb(ph, "egl", [128, 16], F32)
        Gm = sb(ph, "Gm", [64, 1024], F32)
        gb = sb(ph, "gb", [64, 1024], F32)
        dl = sb(ph, "dl", [64, 512], F32)
        du = sb(ph, "du", [64, 512], F32)
        decS = [sb(ph, "decS%d" % i, [64, 512], F32) for i in range(2)]
        decT = [sb(ph, "decT%d" % i, [64, 512], F32) for i in range(2)]
        tN = sb(ph, "tN", [64, 512], F32)
        Nm = [sb(ph, "Nm%d" % i, [64, 512], F32) for i in range(2)]
        Qm = [sb(ph, "Qm%d" % i, [64, 512], F32) for i in range(2)]
        Nn = [[sb(ph, "Nn%d_%d" % (i, k), [64, 512], F32) for k in range(2)] for i in range(2)]
        Qn = [[sb(ph, "Qn%d_%d" % (i, k), [64, 512], F32) for k in range(2)] for i in range(2)]
        N2I = [sb(ph, "N2I%d" % i, [64, 512], F32) for i in range(2)]
        Rm = [sb(ph, "Rm%d" % i, [64, 512], F32) for i in range(2)]
        TinvT = [sb(ph, "TinvT%d" % i, [64, 512], BF16) for i in range(2)]
        kd = [sb(ph, "kd%d" % i, [64, 1024], BF16) for i in range(2)]
        vb = [sb(ph, "vb%d" % i, [64, 1024], F32) for i in range(2)]
        attnT = [sb(ph, "attnT%d" % i, [64, 512], BF16) for i in range(2)]
        Ed = sb(ph, "Ed", [64, 1024], F32)
        qs = [sb(ph, "qs%d" % i, [128, 8, 64], BF16) for i in range(2)]
        tr = [sb(ph, "tr%d" % i, [64, 512], F32) for i in range(2)]
        rr = [sb(ph, "rr%d" % i, [64, 512], BF16) for i in range(2)]
        vn = [sb(ph, "vn%d" % i, [64, 512], BF16) for i in range(2)]
        rk = sb(ph, "rk", [128, 16, 64], BF16)
        rv = sb(ph, "rv", [128, 16, 64], BF16)
        rq = sb(ph, "rq", [128, 16, 64], BF16)
        rg = sb(ph, "rg", [128, 64], F32)

        gcum_sb, egc, dd, ekd, cc_, nbeta = (sm[:, 0:16], sm[:, 16:32], sm[:, 32:48], sm[:, 48:64], sm[:, 64:80], sm[:, 80:96])

        def bc(ap, axis, shape):
            return ap.unsqueeze(axis).to_broadcast(shape)

        def dn_chunk(kT, vT, qT, qT8, gview, goff, boff, omode, otv, keys=('kblk', 'vblk', 'qblk', 'gT')):
            KK_, VK_, QK_, GK_ = keys
            if dn_stop <= 0:
                return
            b = nb()
            S.op('pe', lambda e: e.transpose(out=pb[b][0:64, 0:128], in_=gview, identity=ident), reads=[GK_, 'cs'], writes=[pk(b)])
            S.op('act', lambda e: e.activation(out=gtm[:], in_=pb[b][0:64, 0:128], func=AF.Copy), reads=[pk(b)], writes=['gtm'])
            g = gtm[:, goff:goff + 16]
            beta = gtm[:, boff:boff + 16]
            b = nb()
            S.op('pe', lambda e: e.matmul(pb[b][0:64, 0:16], lhsT=tri, rhs=g, start=True, stop=True), reads=['gtm', 'cs'], writes=[pk(b)])
            S.op('pe', lambda e: e.matmul(pb[b][0:64, 16:32], lhsT=ones[0:64, 0:64], rhs=g, start=True, stop=True), reads=['gtm', 'cs'], writes=[pk(b)])
            S.op('pe', lambda e: e.matmul(pb[b][:, 32:48], lhsT=ones[0:64, :], rhs=g, start=True, stop=True), reads=['gtm', 'cs'], writes=[pk(b)])
            S.op('act', lambda e: e.activation(out=gcum_sb, in_=pb[b][0:64, 0:16], func=AF.Copy), reads=[pk(b)], writes=['sm'])
            S.op('act', lambda e: e.activation(out=egc, in_=pb[b][0:64, 0:16], func=AF.Exp), reads=[pk(b)], writes=['sm'])
            S.op('act', lambda e: e.activation(out=dd, in_=pb[b][0:64, 16:32], func=AF.Copy), reads=[pk(b)], writes=['sm'])
            S.op('dve', lambda e: e.tensor_tensor(out=dd, in0=dd, in1=gcum_sb, op=ALU.subtract), reads=['sm'], writes=['sm'])
            S.op('act', lambda e: e.activation(out=ekd, in_=dd, func=AF.Exp), reads=['sm'], writes=['sm'])
            S.op('act', lambda e: e.activation(out=egl[:], in_=pb[b][:, 32:48], func=AF.Exp), reads=[pk(b)], writes=['egl'])
            S.op('dve', lambda e: e.scalar_tensor_tensor(out=cc_, in0=beta, scalar=-1.0, in1=egc, op0=ALU.mult, op1=ALU.mult),
                 reads=['gtm', 'sm'], writes=['sm'])
            S.op('dve', lambda e: e.tensor_scalar(out=nbeta, in0=beta, scalar1=-1.0, scalar2=None, op0=ALU.mult), reads=['gtm'], writes=['sm'])
            if dn_stop <= 1:
                return
            S.op('dve', lambda e: e.tensor_tensor(out=r3(Gm[:], 64), in0=bc(tri, 1, [64, 16, 64]), in1=bc(g, 2, [64, 16, 64]), op=ALU.mult),
                 reads=['gtm', 'cs'], writes=['Gm'])
            S.op('dve', lambda e: e.tensor_copy(out=r3(gb[:], 64), in_=bc(g, 2, [64, 16, 64])), reads=['gtm'], writes=['gb'])
            for hh in range(2):
                b = nb()
                S.op('pe', lambda e: e.matmul(pb[b][0:64, :], lhsT=tri, rhs=gb[:, hh * 512:(hh + 1) * 512], start=True, stop=False),
                     reads=['gb', 'cs'], writes=[pk(b)])
                S.op('pe', lambda e: e.matmul(pb[b][0:64, :], lhsT=negones, rhs=Gm[:, hh * 512:(hh + 1) * 512], start=False, stop=True),
                     reads=['Gm', 'cs'], writes=[pk(b)])
                S.op('dve', lambda e: e.tensor_tensor(out=r3(dl[:], 64), in0=r3(pb[b][0:64, :], 64), in1=bc(nmL, 1, [64, 8, 64]), op=ALU.add),
                     reads=[pk(b), 'cs'], writes=['dl'])
                S.op('act', lambda e: e.activation(out=decS[hh][:], in_=dl[:], func=AF.Exp), reads=['dl'], writes=['decS%d' % hh])
                S.op('dve', lambda e: e.scalar_tensor_tensor(out=r3(du[:], 64), in0=r3(pb[b][0:64, :], 64), scalar=-1.0, in1=bc(nmU, 1, [64, 8, 64]),
                                                             op0=ALU.mult, op1=ALU.add), reads=[pk(b), 'cs'], writes=['du'])
                S.op('act', lambda e: e.activation(out=decT[hh][:], in_=du[:], func=AF.Exp), reads=['du'], writes=['decT%d' % hh])
            if dn_stop <= 2:
                return
            for hh in range(2):
                b = nb()
                for hl in range(8):
                    h = hh * 8 + hl
                    S.op('pe', lambda e: e.matmul(pb[b][0:64, hl * 64:(hl + 1) * 64], lhsT=kT(h), rhs=kT(h), start=True, stop=True),
                         reads=[KK_], writes=[pk(b)])
                S.op('dve', lambda e: e.tensor_tensor(out=tN[:], in0=pb[b][0:64, :], in1=decS[hh][:], op=ALU.mult),
                     reads=[pk(b), 'decS%d' % hh], writes=['tN'])
                S.op('dve', lambda e: e.tensor_tensor(out=r3(Nm[hh][:], 64), in0=r3(tN[:], 64), in1=bc(nbeta[:, hh * 8:hh * 8 + 8], 2, [64, 8, 64]), op=ALU.mult),
                     reads=['tN', 'sm'], writes=['Nm%d' % hh])
                if dn_stop <= 2.3:
                    continue
                b2 = nb()
                for hl in range(8):
                    S.op('pe', lambda e: e.transpose(out=pb[b2][0:64, hl * 64:(hl + 1) * 64], in_=Nm[hh][:, hl * 64:(hl + 1) * 64], identity=eye64),
                         reads=['Nm%d' % hh, 'cs'], writes=[pk(b2)])
                if dn_stop <= 2.6:
                    continue
                S.op('act', lambda e: e.activation(out=Qm[hh][:], in_=pb[b2][0:64, :], func=AF.Copy), reads=[pk(b2)], writes=['Qm%d' % hh])
                if dn_stop <= 2.7:
                    continue
                S.op('dve', lambda e: e.tensor_tensor(out=r3(Rm[hh][:], 64), in0=r3(Qm[hh][:], 64), in1=bc(eye64, 1, [64, 8, 64]), op=ALU.add),
                     reads=['Qm%d' % hh, 'cs'], writes=['Rm%d' % hh])
            if dn_stop <= 3:
                return
            cur = [(Nm[0], 'Nm0', Qm[0], 'Qm0'), (Nm[1], 'Nm1', Qm[1], 'Qm1')]
            for lvl in range(5):
                bNs, bQs = [], []
                for hh in range(2):
                    Nc, Nk, Qc, Qk = cur[hh]
                    bN = nb()
                    for hl in range(8):
                        sl = slice(hl * 64, (hl + 1) * 64)
                        S.op('pe', lambda e: e.matmul(pb[bN][0:64, sl], lhsT=Qc[:, sl], rhs=Nc[:, sl], start=True, stop=True),
                             reads=[Nk, Qk], writes=[pk(bN)])
                    bNs.append(bN)
                    if lvl < 4:
                        bQ = nb()
                        for hl in range(8):
                            sl = slice(hl * 64, (hl + 1) * 64)
                            S.op('pe', lambda e: e.matmul(pb[bQ][0:64, sl], lhsT=Nc[:, sl], rhs=Qc[:, sl], start=True, stop=True),
                                 reads=[Nk, Qk], writes=[pk(bQ)])
                        bQs.append(bQ)
                for hh in range(2):
                    bN = bNs[hh]
                    nk, qk = 'Nn%d_%d' % (hh, lvl % 2), 'Qn%d_%d' % (hh, lvl % 2)
                    S.op('act', lambda e: e.activation(out=Nn[hh][lvl % 2][:], in_=pb[bN][0:64, :], func=AF.Copy), reads=[pk(bN)], writes=[nk])
                    S.op('dve', lambda e: e.tensor_tensor(out=r3(N2I[hh][:], 64), in0=r3(Nn[hh][lvl % 2][:], 64), in1=bc(eye64, 1, [64, 8, 64]), op=ALU.add),
                         reads=[nk, 'cs'], writes=['N2I%d' % hh])
                    if lvl < 4:
                        S.op('act', lambda e: e.activation(out=Qn[hh][lvl % 2][:], in_=pb[bQs[hh]][0:64, :], func=AF.Copy), reads=[pk(bQs[hh])], writes=[qk])
                        cur[hh] = (Nn[hh][lvl % 2], nk, Qn[hh][lvl % 2], qk)
                for hh in range(2):
                    bR = nb()
                    for hl in range(8):
                        sl = slice(hl * 64, (hl + 1) * 64)
                        S.op('pe', lambda e: e.matmul(pb[bR][0:64, sl], lhsT=N2I[hh][:, sl], rhs=Rm[hh][:, sl], start=True, stop=True),
                             reads=['N2I%d' % hh, 'Rm%d' % hh], writes=[pk(bR)])
                    if lvl < 4:
                        S.op('act', lambda e: e.activation(out=Rm[hh][:], in_=pb[bR][0:64, :], func=AF.Copy), reads=[pk(bR)], writes=['Rm%d' % hh])
                    else:
                        S.op('act', lambda e: e.activation(out=TinvT[hh][:], in_=pb[bR][0:64, :], func=AF.Copy), reads=[pk(bR)], writes=['TinvT%d' % hh])
            if dn_stop <= 4:
                return
            for hh in range(2):
                b = nb()
                for hl in range(8):
                    S.op('pe', lambda e: e.transpose(out=pbb(b)[0:64, hl * 128:(hl + 1) * 128], in_=kT(hh * 8 + hl), identity=identb[:]),
                         reads=[KK_, 'identb'], writes=[pk(b)])
                S.op('dve', lambda e: e.tensor_tensor(out=r3(kd[hh][:], 128), in0=r3(pbb(b)[0:64, :], 128), in1=bc(ekd[:, hh * 8:hh * 8 + 8], 2, [64, 8, 128]), op=ALU.mult),
                     reads=[pk(b), 'sm'], writes=['kd%d' % hh])
                b = nb()
                for hl in range(8):
                    S.op('pe', lambda e: e.transpose(out=pbb(b)[0:64, hl * 128:(hl + 1) * 128], in_=vT(hh * 8 + hl), identity=identb[:]),
                         reads=[VK_, 'identb'], writes=[pk(b)])
                S.op('dve', lambda e: e.tensor_tensor(out=r3(vb[hh][:], 128), in0=r3(pbb(b)[0:64, :], 128), in1=bc(beta[:, hh * 8:hh * 8 + 8], 2, [64, 8, 128]), op=ALU.mult),
                     reads=[pk(b), 'gtm'], writes=['vb%d' % hh])
            if dn_stop <= 5:
                return
            if omode:
                S.op('dve', lambda e: e.tensor_tensor(out=r3(Ed[:], 64), in0=bc(eye64, 1, [64, 16, 64]), in1=bc(egc, 2, [64, 16, 64]), op=ALU.mult),
                     reads=['sm', 'cs'], writes=['Ed'])
                for hh in range(2):
                    b = nb()
                    for hl in range(8):
                        h = hh * 8 + hl
                        S.op('pe', lambda e: e.matmul(pb[b][0:64, hl * 64:(hl + 1) * 64], lhsT=kT(h), rhs=qT(h), start=True, stop=True),
                             reads=[KK_, QK_], writes=[pk(b)])
                    S.op('dve', lambda e: e.tensor_tensor(out=attnT[hh][:], in0=pb[b][0:64, :], in1=decT[hh][:], op=ALU.mult),
                         reads=[pk(b), 'decT%d' % hh], writes=['attnT%d' % hh])
                    b = nb()
                    S.op('pe', lambda e: e.matmul(pb[b][:, :], lhsT=ones[0:64, :], rhs=Ed[:, hh * 512:(hh + 1) * 512], start=True, stop=True),
                         reads=['Ed', 'cs'], writes=[pk(b)])
                    S.op('dve', lambda e: e.tensor_tensor(out=qs[hh][:], in0=qT8(hh), in1=r3(pb[b][:, :], 64), op=ALU.mult),
                         reads=[QK_, pk(b)], writes=['qs%d' % hh])
            if dn_stop <= 6:
                return
            for qd in range(4):
                hh, hb, par = qd // 2, (qd % 2) * 4, qd % 2
                b = nb()
                for hl in range(4):
                    h = 4 * qd + hl
                    S.op('pe', lambda e: e.matmul(pb[b][0:64, hl * 128:(hl + 1) * 128], lhsT=kT(h), rhs=Sb[:, h, :], start=True, stop=True),
                         reads=[KK_, 'Sb%d' % qd], writes=[pk(b)])
                S.op('dve', lambda e: e.tensor_tensor(out=r3(tr[par][:], 128), in0=r3(pb[b][0:64, :], 128), in1=bc(cc_[:, 4 * qd:4 * qd + 4], 2, [64, 4, 128]), op=ALU.mult),
                     reads=[pk(b), 'sm'], writes=['tr%d' % par])
                S.op('dve', lambda e: e.tensor_tensor(out=rr[par][:], in0=tr[par][:], in1=vb[hh][:, hb * 128:(hb + 4) * 128], op=ALU.add),
                     reads=['tr%d' % par, 'vb%d' % hh], writes=['rr%d' % par])
                b2 = nb()
                for hl in range(4):
                    S.op('pe', lambda e: e.matmul(pb[b2][0:64, hl * 128:(hl + 1) * 128], lhsT=TinvT[hh][:, (hb + hl) * 64:(hb + hl + 1) * 64],
                                                  rhs=rr[par][:, hl * 128:(hl + 1) * 128], start=True, stop=True),
                         reads=['TinvT%d' % hh, 'rr%d' % par], writes=[pk(b2)])
                S.op('act', lambda e: e.activation(out=vn[par][:], in_=pb[b2][0:64, :], func=AF.Copy), reads=[pk(b2)], writes=['vn%d' % par])
                if omode:
                    b3 = nb()
                    for hl in range(4):
                        h = 4 * qd + hl
                        S.op('pe', lambda e: e.matmul(pb[b3][:, hl * 64:(hl + 1) * 64], lhsT=Sb[:, h, :], rhs=qs[hh][:, hb + hl, :], start=True, stop=False),
                             reads=['Sb%d' % qd, 'qs%d' % hh], writes=[pk(b3)])
                        S.op('pe', lambda e: e.matmul(pb[b3][:, hl * 64:(hl + 1) * 64], lhsT=vn[par][:, hl * 128:(hl + 1) * 128],
                                                      rhs=attnT[hh][:, (hb + hl) * 64:(hb + hl + 1) * 64], start=False, stop=True),
                             reads=['vn%d' % par, 'attnT%d' % hh], writes=[pk(b3)])
                    if omode == 'set':
                        S.op('act', lambda e: e.activation(out=otv(qd), in_=r3(pb[b3][:, 0:256], 64), func=AF.Copy), reads=[pk(b3)], writes=['OT'])
                    else:
                        S.op('dve', lambda e: e.tensor_tensor(out=otv(qd), in0=r3(pb[b3][:, 0:256], 64), in1=otv(qd), op=ALU.add),
                             reads=[pk(b3), 'OT'], writes=['OT'])
                b4 = nb()
                for hl in range(4):
                    S.op('pe', lambda e: e.matmul(pb[b4][:, hl * 128:(hl + 1) * 128], lhsT=kd[hh][:, (hb + hl) * 128:(hb + hl + 1) * 128],
                                                  rhs=vn[par][:, hl * 128:(hl + 1) * 128], start=True, stop=True),
                         reads=['kd%d' % hh, 'vn%d' % par], writes=[pk(b4)])
                for hl in range(4):
                    h = 4 * qd + hl
                    S.op('dve', lambda e: e.scalar_tensor_tensor(out=Sf[:, h, :], in0=Sf[:, h, :], scalar=egl[:, h:h + 1], in1=pb[b4][:, hl * 128:(hl + 1) * 128],
                                                                 op0=ALU.mult, op1=ALU.add), reads=['Sf%d' % qd, 'egl', pk(b4)], writes=['Sf%d' % qd])
                S.op('act', lambda e: e.activation(out=Sb[:, 4 * qd:4 * qd + 4, :], in_=Sf[:, 4 * qd:4 * qd + 4, :], func=AF.Copy),
                     reads=['Sf%d' % qd], writes=['Sb%d' % qd])

        def fwd(t, h, c0):
            return t[:, h, c0:c0 + 64]

        def rev(t, h, c0):
            if c0 == 0:
                return t[:, h, 63::-1]
            return t[:, h, c0 + 63:c0 - 1:-1]

        def gfwd(c0):
            return gT[:, c0:c0 + 64]

        def grev(c0):
            if c0 == 0:
                return gT[:, 63::-1]
            return gT[:, c0 + 63:c0 - 1:-1]

        def reset_state():
            for qd in range(4):
                S.op('dve', lambda e: e.memset(Sf[:, 4 * qd:4 * qd + 4, :], 0.0), writes=['Sf%d' % qd])
                S.op('dve', lambda e: e.memset(Sb[:, 4 * qd:4 * qd + 4, :], 0.0), writes=['Sb%d' % qd])

        def load_blk(Ksrc, Vsrc, Qsrc, t0, n):
            S.dma('sp', 'kblk', lambda e: e.dma_start(out=kblk[:, :, 0:n], in_=Ksrc.rearrange("h p t -> p h t")[:, :, t0:t0 + n]), writes=['kblk'])
            S.dma('sp', 'vblk', lambda e: e.dma_start(out=vblk[:, :, 0:n], in_=Vsrc.rearrange("h p t -> p h t")[:, :, t0:t0 + n]), writes=['vblk'])
            if Qsrc is not None:
                S.dma('sp', 'qblk', lambda e: e.dma_start(out=qblk[:, :, 0:n], in_=Qsrc.rearrange("h p t -> p h t")[:, :, t0:t0 + n]), writes=['qblk'])

        def run_stream(Ksrc, Vsrc, Qsrc, nblk, gbase, goff, boff, reverse, omode):
            blks = range(nblk - 1, -1, -1) if reverse else range(nblk)
            for bi in blks:
                load_blk(Ksrc, Vsrc, Qsrc, bi * 256, 256)
                chs = range(3, -1, -1) if reverse else range(4)
                for ci in chs:
                    c0 = ci * 64
                    view = rev if reverse else fwd
                    gv = (grev if reverse else gfwd)(gbase + bi * 256 + c0)
                    tok = bi * 256 + c0

                    def otv(qd, tok=tok):
                        if reverse:
                            if tok == 0:
                                return OT[:, 4 * qd:4 * qd + 4, 63::-1]
                            return OT[:, 4 * qd:4 * qd + 4, tok + 63:tok - 1:-1]
                        return OT[:, 4 * qd:4 * qd + 4, tok:tok + 64]

                    def qT8(hh, c0=c0):
                        if reverse:
                            if c0 == 0:
                                return qblk[:, hh * 8:hh * 8 + 8, 63::-1]
                            return qblk[:, hh * 8:hh * 8 + 8, c0 + 63:c0 - 1:-1]
                        return qblk[:, hh * 8:hh * 8 + 8, c0:c0 + 64]
                    if reverse:
                        def rsl(t, c0=c0):
                            if c0 == 0:
                                return t[:, :, 63::-1]
                            return t[:, :, c0 + 63:c0 - 1:-1]
                        S.op('act', lambda e: e.activation(out=rk[:], in_=rsl(kblk), func=AF.Copy), reads=['kblk'], writes=['rk'])
                        S.op('dve', lambda e: e.tensor_copy(out=rv[:], in_=rsl(vblk)), reads=['vblk'], writes=['rv'])
                        if omode:
                            S.op('act', lambda e: e.activation(out=rq[:], in_=rsl(qblk), func=AF.Copy), reads=['qblk'], writes=['rq'])
                        S.op('dve', lambda e: e.tensor_copy(out=rg[:], in_=gv), reads=['gT'], writes=['rg'])
                        dn_chunk(lambda h: rk[:, h, :], lambda h: rv[:, h, :], lambda h: rq[:, h, :],
                                 lambda hh: rq[:, hh * 8:hh * 8 + 8, :], rg[:], goff, boff, omode, otv,
                                 keys=('rk', 'rv', 'rq', 'rg'))
                    else:
                        dn_chunk(lambda h, c0=c0: view(kblk, h, c0), lambda h, c0=c0: view(vblk, h, c0),
                                 lambda h, c0=c0: view(qblk, h, c0), qT8, gv, goff, boff, omode, otv)

        NDB = DN_BLOCKS
        reset_state()
        run_stream(KTc, VTc, None, 1, 2 * NT, 0, 32, False, None)
        S.barrier()
        run_stream(KTo, VTo, QTo, NDB, 0, 0, 32, False, 'set')
        S.barrier()
        def dn_dump():
            def san(dst, src, rk_, wk_):
                S.op('dve', lambda e: e.tensor_scalar(out=dst, in0=src, scalar1=1e30, scalar2=-1e30, op0=ALU.min, op1=ALU.max), reads=rk_, writes=wk_)
            S.barrier()
            san(OT[:], OT[:], ['OT'], ['OT'])
            S.dma('sp', 'dbg', lambda e: e.dma_start(out=OTD, in_=OT[:]), reads=['OT'], writes=['OTD'])
            san(Sf[:], Sf[:], ['Sf0'], ['Sf0'])
            S.dma('sp', 'dbg', lambda e: e.dma_start(out=DBG[:, 1024:3072], in_=Sf[:].rearrange("p h v -> p (h v)")), reads=['Sf0'], writes=['DBG'])
            for (src, c0, n) in [(decS[0][:], 0, 512), (Nm[0][:], 512, 512), (TinvT[0][:], 3072, 512), (vb[0][:, 0:512], 3584, 512)]:
                san(Gm[:, 0:n], src, [], ['Gm'])
                S.dma('sp', 'dbg', lambda e: e.dma_start(out=DBG[0:64, c0:c0 + n], in_=Gm[:, 0:n]), reads=['Gm'], writes=['DBG'])
            for (src, c0, n) in [(sm[:], 0, 96), (gtm[:], 128, 128), (kd[0][:, 0:512], 256, 512), (vn[0][:], 768, 512)]:
                san(gb[:, 0:n], src, [], ['gb'])
                S.dma('sp', 'dbg', lambda e: e.dma_start(out=DBG[64:128, c0:c0 + n], in_=gb[:, 0:n]), reads=['gb'], writes=['DBG'])
            S.barrier()
        if debug == 8:
            dn_dump()
        for _ in ([0] if debug != 8 else []):
          reset_state()
          run_stream(KTc, VTc, None, 1, 2 * NT, 64, 96, True, None)
          S.barrier()
          if NDB == 4:
              run_stream(KTp, VTp, None, 4, NT, 64, 96, False, None)
              S.barrier()
          run_stream(KTo, VTo, QTo, NDB, 0, 64, 96, True, 'add')
          S.barrier()
        if debug in (4, 7):
            dn_dump()

    if debug in (4, 7, 8):
        S.barrier()
        phDE.close()
        es.close()
        return nc, dbg_outs

    own_tiles = [(0, 512), (512, 512)]
    with ExitStack() as ph:
        convact = sb(ph, "convact", [128, 16, NT], BF16)
        mT = sb(ph, "mT", [128, 16, NT], BF16)
        zt = [sb(ph, "zt%d" % i, [128, NT], BF16) for i in range(2)]
        osq = [sb(ph, "osq%d" % i, [128, 512], BF16) for i in range(2)]
        lt = sb(ph, "lt", [128, 4, 512], F32)
        xj = [sb(ph, "xj%d" % i, [128, NT], F32) for i in range(2)]
        for h in range(16):
            s_ = h % 2
            S.dma('sp', 'zt%d' % s_, lambda e: e.dma_start(out=zt[s_][:], in_=ZT[h]), writes=['zt%d' % s_])
            for (t0, n) in own_tiles:
                S.op('act', lambda e: e.activation(out=osq[0][:], in_=OT[:, h, t0:t0 + n], func=AF.Square), reads=['OT'], writes=['osq0'])
                b = nb()
                S.op('pe', lambda e: e.matmul(pb[b][:, :], lhsT=onesb[:], rhs=osq[0][:], start=True, stop=True), reads=['osq0', 'onesb'], writes=[pk(b)])
                S.op('act', lambda e: e.activation(out=fm_rs[:], in_=pb[b][:, :], func=AF.Sqrt, scale=1.0 / 128, bias=epsc[:, 0:1]),
                     reads=[pk(b), 'epsc'], writes=['fm_rs'])
                S.op('dve', lambda e: e.reciprocal(out=fm_rs[:], in_=fm_rs[:]), reads=['fm_rs'], writes=['fm_rs'])
                S.op('dve', lambda e: e.tensor_tensor(out=fm_t[:], in0=OT[:, h, t0:t0 + n], in1=fm_rs[:], op=ALU.mult), reads=['OT', 'fm_rs'], writes=['fm_t'])
                S.op('dve', lambda e: e.scalar_tensor_tensor(out=OT[:, h, t0:t0 + n], in0=fm_t[:], scalar=pcol('dnng', 0), in1=zt[s_][:, t0:t0 + n],
                                                             op0=ALU.mult, op1=ALU.mult), reads=['fm_t', 'prm', 'zt%d' % s_], writes=['OT'])
        for kc in range(16):
            S.dma('sp', 'cact%d' % (kc % 4), lambda e: e.dma_start(out=convact[:, kc, :], in_=UC[kc]), writes=['convact'])
        for (t0, n) in own_tiles:
            bs, bq = nb(), nb()
            for kc in range(16):
                S.op('pe', lambda e: e.matmul(pb[bs][:, :], lhsT=onesb[:], rhs=convact[:, kc, t0:t0 + n], start=(kc == 0), stop=(kc == 15)),
                     reads=['convact', 'onesb'], writes=[pk(bs)])
                S.op('act', lambda e: e.activation(out=osq[kc % 2][:], in_=convact[:, kc, t0:t0 + n], func=AF.Square), reads=['convact'], writes=['osq%d' % (kc % 2)])
                S.op('pe', lambda e: e.matmul(pb[bq][:, :], lhsT=onesb[:], rhs=osq[kc % 2][:], start=(kc == 0), stop=(kc == 15)),
                     reads=['osq%d' % (kc % 2), 'onesb'], writes=[pk(bq)])
            mean, msq, rs_, nmr = lt[:, 0, :], lt[:, 1, :], lt[:, 2, :], lt[:, 3, :]
            S.op('act', lambda e: e.activation(out=mean, in_=pb[bs][:, :], func=AF.Copy, scale=1.0 / D), reads=[pk(bs)], writes=['lt'])
            S.op('dve', lambda e: e.tensor_tensor(out=msq, in0=mean, in1=mean, op=ALU.mult), reads=['lt'], writes=['lt'])
            S.op('dve', lambda e: e.scalar_tensor_tensor(out=rs_, in0=pb[bq][:, :], scalar=1.0 / D, in1=msq, op0=ALU.mult, op1=ALU.subtract),
                 reads=[pk(bq), 'lt'], writes=['lt'])
            S.op('act', lambda e: e.activation(out=rs_, in_=rs_, func=AF.Sqrt, scale=1.0, bias=epsc[:, 0:1]), reads=['lt', 'epsc'], writes=['lt'])
            S.op('dve', lambda e: e.reciprocal(out=rs_, in_=rs_), reads=['lt'], writes=['lt'])
            S.op('dve', lambda e: e.scalar_tensor_tensor(out=nmr, in0=mean, scalar=-1.0, in1=rs_, op0=ALU.mult, op1=ALU.mult), reads=['lt'], writes=['lt'])
            for kc in range(16):
                S.op('dve', lambda e: e.tensor_tensor(out=fm_t[:], in0=convact[:, kc, t0:t0 + n], in1=rs_, op=ALU.mult), reads=['convact', 'lt'], writes=['fm_t'])
                S.op('dve', lambda e: e.tensor_tensor(out=fm_t[:], in0=fm_t[:], in1=nmr, op=ALU.add), reads=['fm_t', 'lt'], writes=['fm_t'])
                S.op('act', lambda e: e.activation(out=convact[:, kc, t0:t0 + n], in_=fm_t[:], func=AF.Silu, scale=pcol('lng', kc), bias=pcol('lnb', kc)),
                     reads=['fm_t', 'prm'], writes=['convact'])
        for j in range(16):
            s_ = load_w(wco[j])
            z_ = j % 2
            S.dma('sp', 'zt%d' % z_, lambda e: e.dma_start(out=zt[z_][:], in_=BR[j]), writes=['zt%d' % z_])
            for (t0, n) in own_tiles:
                b = mm16(s_, lambda kc: convact[:, kc, t0:t0 + n], n, ['convact'])
                S.op('dve', lambda e: e.tensor_tensor(out=mT[:, j, t0:t0 + n], in0=pb[b][:, 0:n], in1=zt[z_][:, t0:t0 + n], op=ALU.mult),
                     reads=[pk(b), 'zt%d' % z_], writes=['mT'])
        for j in range(16):
            s_ = load_w(wdn[j])
            z_ = j % 2
            S.dma('sp', 'zt%d' % z_, lambda e: e.dma_start(out=zt[z_][:], in_=BR[16 + j]), writes=['zt%d' % z_])
            for (t0, n) in own_tiles:
                b = mm16(s_, lambda kc: OT[:, kc, t0:t0 + n], n, ['OT'])
                S.op('dve', lambda e: e.tensor_tensor(out=fm_t[:], in0=pb[b][:, 0:n], in1=zt[z_][:, t0:t0 + n], op=ALU.mult),
                     reads=[pk(b), 'zt%d' % z_], writes=['fm_t'])
                S.op('dve', lambda e: e.tensor_tensor(out=mT[:, j, t0:t0 + n], in0=fm_t[:], in1=mT[:, j, t0:t0 + n], op=ALU.add),
                     reads=['fm_t', 'mT'], writes=['mT'])
        for j in range(16):
            s_ = load_w(wout[j])
            z_ = j % 2
            S.dma('sp', 'xj%d' % z_, lambda e: e.dma_start(out=xj[z_][:], in_=XT[j]), writes=['xj%d' % z_])
            for (t0, n) in own_tiles:
                b = mm16(s_, lambda kc: mT[:, kc, t0:t0 + n], n, ['mT'])
                S.op('dve', lambda e: e.scalar_tensor_tensor(out=xj[z_][:, t0:t0 + n], in0=pb[b][:, 0:n], scalar=mdc('G2', j), in1=xj[z_][:, t0:t0 + n],
                                                             op0=ALU.mult, op1=ALU.add), reads=[pk(b), 'md', 'xj%d' % z_], writes=['xj%d' % z_])
            S.dma('sp', 'xjo%d' % z_, lambda e: e.dma_start(out=X1[j], in_=xj[z_][:]), reads=['xj%d' % z_], writes=['X1'])
        S.barrier()
    phDE.close()
    if debug == 5:
        S.barrier()
        es.close()
        return nc, dbg_outs

    S.barrier()
    with ExitStack() as ph:
        h2T = sb(ph, "h2T", [128, 16, NT], BF16)
        qpT = sb(ph, "qpT", [128, 16, NT], BF16)
        with ExitStack() as ph2:
            x1t = sb(ph2, "x1t", [128, 16, 512], F32)
            sqt2 = sb(ph2, "sqt2", [128, 16, 512], BF16)

            class _V2:
                def __init__(self, t, o):
                    self.t, self.o = t, o

                def __getitem__(self, key):
                    p, kc, sl = key
                    return self.t[p, kc, self.o + sl.start:self.o + sl.stop]
            for (t0, n) in own_tiles:
                S.dma('sp', 'x1t', lambda e: e.dma_start(out=x1t[:], in_=X1.rearrange("c p t -> p c t")[:, :, t0:t0 + n]), writes=['x1t'])
                fm_norm(ph2, x1t, n, lambda kc: mdc('A2', kc), lambda kc: mdc('B2', kc), _V2(h2T, t0), 'h2T', 'x1t', sqt2, 'sqt2')
            S.barrier()
        for j in range(16):
            s_ = load_w(wq[j])
            for (t0, n) in own_tiles:
                b = mm16(s_, lambda kc: h2T[:, kc, t0:t0 + n], n, ['h2T'])
                S.op('act', lambda e: e.activation(out=qpT[:, j, t0:t0 + n], in_=pb[b][:, 0:n], func=AF.Copy), reads=[pk(b)], writes=['qpT'])
        kT12 = sb(ph, "kT12", [128, 16, 128], BF16)
        ktmp = [sb(ph, "ktmp%d" % i, [128, 128], F32) for i in range(2)]
        for i in range(16):
            src = (pk1 if i < 8 else pk2)[i % 8]
            s_ = i % 2
            S.dma('sp', 'ktmp%d' % s_, lambda e: e.dma_start(out=ktmp[s_][:], in_=src), writes=['ktmp%d' % s_])
            b = nb()
            S.op('pe', lambda e: e.transpose(out=pb[b][:, 0:128], in_=ktmp[s_][:], identity=ident), reads=['ktmp%d' % s_, 'cs'], writes=[pk(b)])
            S.op('act', lambda e: e.activation(out=kT12[:, i, :], in_=pb[b][:, 0:128], func=AF.Copy), reads=[pk(b)], writes=['kT12'])

        h2tm = sb(ph, "h2tm", [128, D], BF16)
        gsl = [sb(ph, "gsl%d" % i, [128, D], BF16) for i in range(10)]
        ysb = sb(ph, "ysb", [128, D], F32)
        x1tile = sb(ph, "x1tile", [128, 16, 128], F32)
        xnt = sb(ph, "xnt", [128, 16, 128], F32)
        sq3 = sb(ph, "sq3", [128, 16, 128], BF16)
        junkb2 = [sb(ph, "junkb%d" % i, [128, D], BF16) for i in range(2)]
        v12 = sb(ph, "v12", [128, 32], F32)
        i12 = sb(ph, "i12", [128, 32], U32)
        i12f = sb(ph, "i12f", [128, 32], F32)
        scr = sb(ph, "scr", [128, 128], F32)
        cand = sb(ph, "cand", [128, 256], F32)
        cidx = sb(ph, "cidx", [128, 256], F32)
        scr2 = sb(ph, "scr2", [128, 256], F32)
        junk = sb(ph, "junk", [128, 256], F32)
        best = sb(ph, "best", [128, 16], F32)
        e16 = sb(ph, "e16", [128, 16], F32)
        sml = sb(ph, "sml", [128, 4], F32)
        idxf = sb(ph, "idxf", [128, 128], F32)
        idxi = sb(ph, "idxi", [128, 128], I32)
        wgt = sb(ph, "wgt", [128, 128], F32)
        pre = sb(ph, "pre", [128, 128], F32)
        gtmp = sb(ph, "gtmp", [128, 3, 128], F32)
        coef = sb(ph, "coef", [128, 128], F32)
        dgs = [sb(ph, "dgs%d" % i, [128, 128], BF16) for i in range(2)]
        bank_pool[0] = [4, 5, 6, 7]
        yb = [0, 1, 2, 3]
        gctr = [0]

        for tt in range(PEER_TILES):
            tok = tt * 128
            for q4 in range(4):
                b = nb()
                for i in range(4):
                    kc = q4 * 4 + i
                    S.op('pe', lambda e: e.transpose(out=pbb(b)[:, i * 128:(i + 1) * 128], in_=h2T[:, kc, tok:tok + 128], identity=identb[:]),
                         reads=['h2T', 'identb'], writes=[pk(b)])
                S.op('act', lambda e: e.activation(out=h2tm[:, q4 * 512:(q4 + 1) * 512], in_=pbb(b)[:, 0:512], func=AF.Copy), reads=[pk(b)], writes=['h2tm'])
            for h in range(8):
                b = nb()
                S.op('pe', lambda e: e.matmul(pb[b][:, 0:128], lhsT=qpT[:, 2 * h, tok:tok + 128], rhs=kT12[:, h, :], start=True, stop=True),
                     reads=['qpT', 'kT12'], writes=[pk(b)])
                S.op('pe', lambda e: e.matmul(pb[b][:, 128:256], lhsT=qpT[:, 2 * h + 1, tok:tok + 128], rhs=kT12[:, 8 + h, :], start=True, stop=True),
                     reads=['qpT', 'kT12'], writes=[pk(b)])
                for (c0, vo) in ((0, 0), (128, 16)):
                    src = pb[b][:, c0:c0 + 128]
                    S.op('dve', lambda e: e.max(out=v12[:, vo:vo + 8], in_=src), reads=[pk(b)], writes=['v12'])
                    S.op('dve', lambda e: e.max_index(out=i12[:, vo:vo + 8], in_max=v12[:, vo:vo + 8], in_values=src), reads=[pk(b), 'v12'], writes=['i12'])
                    S.op('dve', lambda e: e.match_replace(out=scr[:], in_to_replace=v12[:, vo:vo + 8], in_values=src, imm_value=-1e30),
                         reads=[pk(b), 'v12'], writes=['scr'])
                    S.op('dve', lambda e: e.max(out=v12[:, vo + 8:vo + 16], in_=scr[:]), reads=['scr'], writes=['v12'])
                    S.op('dve', lambda e: e.max_index(out=i12[:, vo + 8:vo + 16], in_max=v12[:, vo + 8:vo + 16], in_values=scr[:]),
                         reads=['scr', 'v12'], writes=['i12'])
                S.op('dve', lambda e: e.tensor_copy(out=i12f[:], in_=i12[:]), reads=['i12'], writes=['i12f'])
                S.op('dve', lambda e: e.tensor_tensor(out=r3(cand[:], 16), in0=bc(v12[:, 0:16], 2, [128, 16, 16]), in1=bc(v12[:, 16:32], 1, [128, 16, 16]), op=ALU.add),
                     reads=['v12'], writes=['cand'])
                S.op('dve', lambda e: e.scalar_tensor_tensor(out=r3(cidx[:], 16), in0=bc(i12f[:, 0:16], 2, [128, 16, 16]), scalar=128.0,
                                                             in1=bc(i12f[:, 16:32], 1, [128, 16, 16]), op0=ALU.mult, op1=ALU.add),
                     reads=['i12f'], writes=['cidx'])
                S.op('dve', lambda e: e.max(out=best[:, 0:8], in_=cand[:]), reads=['cand'], writes=['best'])
                S.op('dve', lambda e: e.match_replace(out=scr2[:], in_to_replace=best[:, 0:8], in_values=cand[:], imm_value=-1e30),
                     reads=['cand', 'best'], writes=['scr2'])
                S.op('dve', lambda e: e.max(out=best[:, 8:16], in_=scr2[:]), reads=['scr2'], writes=['best'])
                for k in range(16):
                    S.op('dve', lambda e: e.scalar_tensor_tensor(out=junk[:], in0=cand[:], scalar=best[:, k:k + 1], in1=cidx[:], op0=ALU.is_equal, op1=ALU.mult,
                                                                 accum_out=idxf[:, h * 16 + k:h * 16 + k + 1]),
                         reads=['cand', 'best', 'cidx'], writes=['junk', 'idxf'])
                S.op('dve', lambda e: e.tensor_scalar(out=sml[:, 0:1], in0=best[:, 0:1], scalar1=-1.0, scalar2=None, op0=ALU.mult), reads=['best'], writes=['sml'])
                S.op('act', lambda e: e.activation(out=e16[:], in_=best[:], func=AF.Exp, bias=sml[:, 0:1], scale=1.0, accum_out=sml[:, 1:2]),
                     reads=['best', 'sml'], writes=['e16', 'sml'])
                S.op('dve', lambda e: e.reciprocal(out=sml[:, 2:3], in_=sml[:, 1:2]), reads=['sml'], writes=['sml'])
                S.op('dve', lambda e: e.tensor_scalar(out=wgt[:, h * 16:(h + 1) * 16], in0=e16[:], scalar1=sml[:, 2:3], scalar2=None, op0=ALU.mult),
                     reads=['e16', 'sml'], writes=['wgt'])
            S.op('dve', lambda e: e.tensor_scalar(out=idxf[:], in0=idxf[:], scalar1=float(PUR - 1), scalar2=0.0, op0=ALU.min, op1=ALU.max),
                 reads=['idxf'], writes=['idxf'])
            S.op('dve', lambda e: e.tensor_copy(out=idxi[:], in_=idxf[:]), reads=['idxf'], writes=['idxi'])
            for s in range(128):
                g_ = gctr[0] % 10
                gctr[0] += 1
                S.dma('pool', 'gsl%d' % g_, lambda e: e.indirect_dma_start(out=gsl[g_][:], out_offset=None, in_=PUB,
                      in_offset=bass.IndirectOffsetOnAxis(ap=idxi[:, s:s + 1], axis=0)), reads=['idxi'], writes=['gsl%d' % g_])
                jb_ = s % 2
                S.op('dve', lambda e: e.tensor_tensor(out=junkb2[jb_][:], in0=gsl[g_][:], in1=h2tm[:], op=ALU.mult),
                     reads=['gsl%d' % g_, 'h2tm'], writes=['junkb%d' % jb_])
                S.op('act', lambda e: e.activation(out=junkb2[jb_][:], in_=junkb2[jb_][:], func=AF.Copy, accum_out=pre[:, s:s + 1]),
                     reads=['junkb%d' % jb_], writes=['junkb%d' % jb_, 'pre%d' % (s % 8)])
            S.op('dve', lambda e: e.tensor_tensor(out=gtmp[:, 0, :], in0=pre[:], in1=pre[:], op=ALU.mult), reads=['pre%d' % i for i in range(8)], writes=['gtmp'])
            S.op('dve', lambda e: e.tensor_scalar(out=gtmp[:, 0, :], in0=gtmp[:, 0, :], scalar1=0.044715, scalar2=1.0, op0=ALU.mult, op1=ALU.add),
                 reads=['gtmp'], writes=['gtmp'])
            S.op('dve', lambda e: e.tensor_tensor(out=gtmp[:, 1, :], in0=gtmp[:, 0, :], in1=pre[:], op=ALU.mult), reads=['gtmp'] + ['pre%d' % i for i in range(8)], writes=['gtmp'])
            S.op('act', lambda e: e.activation(out=gtmp[:, 2, :], in_=gtmp[:, 1, :], func=AF.Tanh, scale=0.7978845608028654), reads=['gtmp'], writes=['gtmp'])
            S.op('dve', lambda e: e.scalar_tensor_tensor(out=gtmp[:, 0, :], in0=gtmp[:, 2, :], scalar=1.0, in1=pre[:], op0=ALU.add, op1=ALU.mult),
                 reads=['gtmp'] + ['pre%d' % i for i in range(8)], writes=['gtmp'])
            S.op('dve', lambda e: e.scalar_tensor_tensor(out=coef[:], in0=gtmp[:, 0, :], scalar=0.5, in1=wgt[:], op0=ALU.mult, op1=ALU.mult),
                 reads=['gtmp', 'wgt'], writes=['coef'])
            for s in range(128):
                g_ = gctr[0] % 10
                gctr[0] += 1
                S.dma('pool', 'gsl%d' % g_, lambda e: e.indirect_dma_start(out=gsl[g_][:], out_offset=None, in_=PVB,
                      in_offset=bass.IndirectOffsetOnAxis(ap=idxi[:, s:s + 1], axis=0)), reads=['idxi'], writes=['gsl%d' % g_])
                d_ = s % 2
                S.op('dve', lambda e: e.tensor_scalar(out=dgs[d_][:], in0=identb[:], scalar1=coef[:, s:s + 1], scalar2=None, op0=ALU.mult),
                     reads=['identb', 'coef'], writes=['dgs%d' % d_])
                for n4 in range(4):
                    S.op('pe', lambda e: e.matmul(pb[yb[n4]][:, :], lhsT=dgs[d_][:], rhs=gsl[g_][:, n4 * 512:(n4 + 1) * 512], start=(s == 0), stop=(s == 127)),
                         reads=['dgs%d' % d_, 'gsl%d' % g_], writes=[pk(yb[n4])])
            for n4 in range(4):
                S.op('act', lambda e: e.activation(out=ysb[:, n4 * 512:(n4 + 1) * 512], in_=pb[yb[n4]][:, :], func=AF.Copy), reads=[pk(yb[n4])], writes=['ysb'])
            S.dma('sp', 'x1tile', lambda e: e.dma_start(out=x1tile[:], in_=X1.rearrange("c p t -> p c t")[:, :, tok:tok + 128]), writes=['x1tile'])
            for q4 in range(4):
                b = nb()
                for i in range(4):
                    kc = q4 * 4 + i
                    S.op('pe', lambda e: e.transpose(out=pb[b][:, i * 128:(i + 1) * 128], in_=ysb[:, kc * 128:(kc + 1) * 128], identity=ident),
                         reads=['ysb', 'cs'], writes=[pk(b)])
                for i in range(4):
                    kc = q4 * 4 + i
                    S.op('dve', lambda e: e.scalar_tensor_tensor(out=x1tile[:, kc, :], in0=pb[b][:, i * 128:(i + 1) * 128], scalar=mdc('G5', kc), in1=x1tile[:, kc, :],
                                                                 op0=ALU.mult, op1=ALU.add), reads=[pk(b), 'md', 'x1tile'], writes=['x1tile'])
            fm_norm(ph, x1tile, 128, lambda kc: pcol('gf', kc), None, xnt, 'xnt', 'x1tile', sq3, 'sq3')
            for q4 in range(4):
                b = nb()
                for i in range(4):
                    kc = q4 * 4 + i
                    S.op('pe', lambda e: e.transpose(out=pb[b][:, i * 128:(i + 1) * 128], in_=xnt[:, kc, :], identity=ident), reads=['xnt', 'cs'], writes=[pk(b)])
                S.op('act', lambda e: e.activation(out=ysb[:, q4 * 512:(q4 + 1) * 512], in_=pb[b][:, :], func=AF.Copy), reads=[pk(b)], writes=['ysb'])
            S.dma('sp', 'outst', lambda e: e.dma_start(out=out[tok:tok + 128, :], in_=ysb[:]), reads=['ysb'], writes=['out'])
        if debug == 6:
            S.dma('sp', 'dbg', lambda e: e.dma_start(out=DBG[:, 0:128], in_=idxf[:]), reads=['idxf'], writes=['DBG'])
            S.dma('sp', 'dbg', lambda e: e.dma_start(out=DBG[:, 128:256], in_=wgt[:]), reads=['wgt'], writes=['DBG'])
            S.dma('sp', 'dbg', lambda e: e.dma_start(out=DBG[:, 256:384], in_=pre[:]), reads=['pre'], writes=['DBG'])
            S.dma('sp', 'dbg', lambda e: e.dma_start(out=DBG[:, 384:512], in_=coef[:]), reads=['coef'], writes=['DBG'])
        S.barrier()
    S.barrier()
    es.close()
    return nc, dbg_outs


def _prep_core(inp, core):
    b, half = core // 2, core % 2
    f = np.ascontiguousarray
    x = inp['x'][b]
    ctx = inp['ctx'][b]
    if half == 0:
        xo, xp, xh, cxx = x[0:NT], x[2047:1023:-1], x[NT:NT + 2], ctx
        p1, p2 = 0, 1
    else:
        xo, xp, xh, cxx = x[2047:1023:-1], x[0:NT], x[1023:1021:-1], ctx[::-1]
        p1, p2 = 1, 0

    def colform(v):
        return f(v.reshape(-1, 128).T)

    ccol = np.stack([colform(inp['c'][b]), colform(inp['c_ctx'])], axis=-1)
    wm = inp['w_mod'][0].reshape(16, 128, 48, 256).transpose(2, 1, 0, 3)
    w_in = inp['w_in'][0]
    ab = w_in[:, 12288:12352]
    abp = np.zeros((D, 128), np.float32)
    for gi, d in enumerate((p1, p2)):
        abp[:, gi * 64:gi * 64 + 16] = ab[:, 32 * d:32 * d + 16]
        abp[:, gi * 64 + 32:gi * 64 + 48] = ab[:, 32 * d + 16:32 * d + 32]
    wsel = np.concatenate([w_in[:, 0:12288], abp, w_in[:, 12352:16448]], axis=1)

    def chunked(w):
        n = w.shape[1] // 128
        return f(w.reshape(16, 128, n, 128).transpose(2, 1, 0, 3))

    cw = inp['conv_w'][0]
    sw = inp['dn_short_w'][0]
    if half == 1:
        cw = cw[::-1]
        sw = sw[::-1]
    convw = f(cw.reshape(31, 16, 128).transpose(2, 1, 0))
    shw = f(sw.reshape(5, 48, 128).transpose(2, 1, 0))
    shwf = f(sw[::-1].reshape(5, 48, 128).transpose(2, 1, 0))
    gpar = np.zeros((128, 2), np.float32)
    for gi, d in enumerate((p1, p2)):
        gpar[gi * 64:gi * 64 + 16, 0] = inp['dn_a_log'][0, d]
        gpar[gi * 64:gi * 64 + 16, 1] = inp['dn_dt_bias'][0, d]
    cst = np.zeros((128, 1024), np.float32)
    cst[:, 0:128] = np.eye(128)
    cst[:, 128:256] = 1.0
    i_ = np.arange(64)
    cst[0:64, 256:320] = (i_[:, None] <= i_[None, :])
    cst[0:64, 320:384] = -1.0
    cst[0:64, 384:448] = np.where(i_[:, None] > i_[None, :], 0.0, NEG)
    cst[0:64, 448:512] = np.where(i_[:, None] <= i_[None, :], 0.0, NEG)
    m = dict(
        xo=f(xo), xp=f(xp), xh=f(xh), cx=f(cxx), ccol=f(ccol), wmod=f(wm), bmod=colform(inp['b_mod'][0]),
        g1=colform(inp['norm1_g'][0]), g2=colform(inp['norm2_g'][0]), gf=colform(inp['final_g']),
        win=chunked(wsel), convw=convw, convb=colform(inp['conv_b'][0]), lng=colform(inp['conv_ln_g'][0]),
        lnb=colform(inp['conv_ln_b'][0]), wco=chunked(inp['w_conv_out'][0]), wdn=chunked(inp['w_dn_out'][0]),
        wout=chunked(inp['w_out'][0]), wq=chunked(inp['peer_w_q'][0]), shw=shw, shwf=shwf, gpar=gpar,
        dnng=f(inp['dn_norm_g'][0].reshape(128, 1)), pk1=f(inp['peer_key1'][0]), pk2=f(inp['peer_key2'][0]),
        pu=f(inp['peer_u'][0]), pv=f(inp['peer_v'][0]), cst=cst,
    )
    return {k: np.ascontiguousarray(v, dtype=np.float32) for k, v in m.items()}


def kernel(**inputs):
    inp = {k: np.asarray(v) for k, v in inputs.items()}
    nc, _ = build_program(0)
    in_maps = [_prep_core(inp, c) for c in range(8)]
    res = run_bass_kernel_spmd(nc, in_maps, core_ids=list(range(8)))
    outp = np.zeros((4, 2048, D), np.float32)
    for c in range(8):
        b, half = c // 2, c % 2
        o = np.asarray(res.results[c]['out'])
        if half == 0:
            outp[b, 0:NT] = o
        else:
            outp[b, NT:] = o[::-1]
    return outp
```
